# Optimizing a Trainium2 kernel written in Bass

```python
import math
import jax
import jax.numpy as jnp
from jax import lax
import numpy as np

D_MODEL = 1024
BATCH = 8
SEQ = 4096
DEPTH = 2

GRID_W = 64
CTX_LEN = 256
N_EVEN = (DEPTH + 1) // 2
N_ODD = DEPTH // 2
MOD_CHUNKS = 6

DN_HEADS = 4
DN_DK = 128
DN_DV = 128
DN_SHORT_CONV = 5
DN_CHUNK = 64
QK_W = DN_HEADS * DN_DK
V_W = DN_HEADS * DN_DV
QKV_W = 2 * QK_W + V_W
AB_W = 2 * 2 * DN_HEADS
Z_W = V_W
DN_W = V_W
FN_GROUPS = 4
FN_GROUP_W = 128
FN_W = FN_GROUPS * FN_GROUP_W
HYB_IN_W = QKV_W + AB_W + Z_W + FN_W
MIX_W = DN_W + FN_W

CONF_D = D_MODEL
CONF_K = 31

MOE_EXPERTS = 32
MOE_GROUPS = 8
EXPERTS_PER_GROUP = MOE_EXPERTS // MOE_GROUPS
MOE_GROUP_SCORE_K = 2
MOE_TOP_K = 2
MOE_FF = 512

NORM_EPS = 1e-6
POS_BASE = 10000.0

kernel_name = 'hybrid_deltanet_fnet_conformer_moe_dit'


def rmsnorm(x, g):
    xf = x.astype(jnp.float32)
    y = xf * lax.rsqrt(jnp.mean(xf * xf, axis=-1, keepdims=True) + NORM_EPS)
    return (y * g.astype(jnp.float32)).astype(x.dtype)


def layernorm(x, g, b):
    xf = x.astype(jnp.float32)
    mu = jnp.mean(xf, axis=-1, keepdims=True)
    var = jnp.mean(jnp.square(xf - mu), axis=-1, keepdims=True)
    y = (xf - mu) * lax.rsqrt(var + NORM_EPS)
    return (y * g.astype(jnp.float32) + b.astype(jnp.float32)).astype(x.dtype)


def l2norm(t):
    tf = t.astype(jnp.float32)
    return tf * lax.rsqrt(jnp.sum(tf * tf, axis=-1, keepdims=True) + NORM_EPS)


def adaln(cond, w, b):
    return jnp.split(jax.nn.silu(cond) @ w + b, MOD_CHUNKS, axis=-1)


def grid_sincos(rows, cols, dim):
    quarter = dim // 4
    omega = 1.0 / jnp.power(POS_BASE, jnp.arange(quarter, dtype=jnp.float32) / quarter)

    def axis_emb(n):
        ang = jnp.arange(n, dtype=jnp.float32)[:, None] * omega[None, :]
        return jnp.concatenate([jnp.sin(ang), jnp.cos(ang)], axis=-1)

    er = jnp.broadcast_to(axis_emb(rows)[:, None, :], (rows, cols, dim // 2))
    ec = jnp.broadcast_to(axis_emb(cols)[None, :, :], (rows, cols, dim // 2))
    return jnp.concatenate([er, ec], axis=-1).reshape(rows * cols, dim)


def depthwise_conv(x, w):
    k = w.shape[0]
    return lax.conv_general_dilated(
        x, w[:, None, :].astype(x.dtype), window_strides=(1,),
        padding=[(k // 2, k - 1 - k // 2)],
        dimension_numbers=('NWC', 'WIO', 'NWC'),
        feature_group_count=x.shape[-1])


def chunk_gated_delta(q, k, v, g, beta, s0):
    b, seq_len, h, dk = k.shape
    dv = v.shape[-1]
    n = seq_len // DN_CHUNK
    f32 = jnp.float32

    def blocks(t):
        t = t.astype(f32).reshape((b, n, DN_CHUNK, h) + t.shape[3:])
        return jnp.moveaxis(jnp.moveaxis(t, 3, 2), 1, 0)

    k, v, g, beta = blocks(k), blocks(v), blocks(g), blocks(beta)
    cum_g = jnp.cumsum(g, axis=-1)
    pos = jnp.arange(DN_CHUNK)
    incl = pos[:, None] >= pos[None, :]
    strict = pos[:, None] > pos[None, :]
    decay = jnp.exp(jnp.where(incl, cum_g[..., :, None] - cum_g[..., None, :], -jnp.inf))
    k_beta = k * beta[..., None]
    a_mat = jnp.einsum('nbhcd,nbhmd->nbhcm', k_beta, k) * jnp.where(strict, decay, 0.0)
    rhs = jnp.concatenate([v * beta[..., None], k_beta * jnp.exp(cum_g)[..., None]], axis=-1)
    sol = lax.linalg.triangular_solve(a_mat + jnp.eye(DN_CHUNK, dtype=f32), rhs,
                                      left_side=True, lower=True, unit_diagonal=True)
    u, w = sol[..., :dv], sol[..., dv:]
    k_tail = k * jnp.exp(cum_g[..., -1:] - cum_g)[..., None]
    chunk_decay = jnp.exp(cum_g[..., -1])
    xs = (u, w, k_tail, chunk_decay)
    with_output = q is not None
    if with_output:
        q = blocks(q) * (dk ** -0.5)
        q_dec = q * jnp.exp(cum_g)[..., None]
        qk = jnp.einsum('nbhcd,nbhmd->nbhcm', q, k) * decay
        xs = xs + (q_dec, qk)

    def step(state, xc):
        v_new = xc[0] - jnp.einsum('bhcd,bhdv->bhcv', xc[1], state)
        new_state = state * xc[3][..., None, None] + jnp.einsum('bhcd,bhcv->bhdv', xc[2], v_new)
        if with_output:
            o_c = (jnp.einsum('bhcd,bhdv->bhcv', xc[4], state)
                   + jnp.einsum('bhcm,bhmv->bhcv', xc[5], v_new))
            return new_state, o_c
        return new_state, None

    s_final, o = lax.scan(step, s0.astype(f32), xs)
    if with_output:
        o = jnp.moveaxis(jnp.moveaxis(o, 0, 1), 2, 3).reshape(b, seq_len, h, dv)
    return o, s_final


def dn_gates(p_ab, a_log, dt_bias):
    ab = p_ab.astype(jnp.float32).reshape(p_ab.shape[:2] + (2, 2, DN_HEADS))
    g = -jnp.exp(a_log.astype(jnp.float32)) * jax.nn.softplus(ab[:, :, 0] + dt_bias.astype(jnp.float32))
    beta = jax.nn.sigmoid(ab[:, :, 1])
    return g, beta


def dn_bidir(q, k, v, g, beta, s0_fwd, s0_bwd):
    def flip(t):
        return None if t is None else jnp.flip(t, axis=1)
    o_f, s_f = chunk_gated_delta(q, k, v, g[:, :, 0], beta[:, :, 0], s0_fwd)
    o_b, s_b = chunk_gated_delta(flip(q), flip(k), flip(v), flip(g[:, :, 1]), flip(beta[:, :, 1]), s0_bwd)
    o = None if q is None else o_f + flip(o_b)
    return o, s_f, s_b


def hybrid_mixer(h, w_in, conv_w, a_log, dt_bias, onorm_g, w_out, s0_fwd, s0_bwd):
    b, seq_len, _ = h.shape
    p = h @ w_in
    qkv = jax.nn.silu(depthwise_conv(p[..., :QKV_W], conv_w))
    q = l2norm(qkv[..., :QK_W].reshape(b, seq_len, DN_HEADS, DN_DK))
    k = l2norm(qkv[..., QK_W:2 * QK_W].reshape(b, seq_len, DN_HEADS, DN_DK))
    v = qkv[..., 2 * QK_W:].reshape(b, seq_len, DN_HEADS, DN_DV)
    g, beta = dn_gates(p[..., QKV_W:QKV_W + AB_W], a_log, dt_bias)
    o, s_f, s_b = dn_bidir(q, k, v, g, beta, s0_fwd, s0_bwd)
    z_lo = QKV_W + AB_W
    f_lo = z_lo + Z_W
    z = p[..., z_lo:f_lo].reshape(b, seq_len, DN_HEADS, DN_DV).astype(jnp.float32)
    dn_out = (rmsnorm(o, onorm_g) * jax.nn.silu(z)).reshape(b, seq_len, DN_W).astype(h.dtype)
    f = p[..., f_lo:].reshape(b, seq_len, FN_GROUPS, FN_GROUP_W).astype(jnp.float32)
    fn_out = jnp.fft.fft2(f, axes=(1, 3), norm='ortho').real.reshape(b, seq_len, FN_W).astype(h.dtype)
    return jnp.concatenate([dn_out, fn_out], axis=-1) @ w_out, s_f, s_b


def dn_context_states(hc, w_in, conv_w, a_log, dt_bias):
    b, seq_len, _ = hc.shape
    p = hc @ w_in[:, QK_W:QKV_W + AB_W]
    kv = jax.nn.silu(depthwise_conv(p[..., :QK_W + V_W], conv_w[:, QK_W:]))
    k = l2norm(kv[..., :QK_W].reshape(b, seq_len, DN_HEADS, DN_DK))
    v = kv[..., QK_W:].reshape(b, seq_len, DN_HEADS, DN_DV)
    g, beta = dn_gates(p[..., QK_W + V_W:], a_log, dt_bias)
    zero = jnp.zeros((b, DN_HEADS, DN_DK, DN_DV), jnp.float32)
    _, s_f, s_b = dn_bidir(None, k, v, g, beta, zero, zero)
    return s_f, s_b


def conformer_conv(h, w1, b1, dw_w, dw_b, ln_g, ln_b, w2, b2):
    u = h @ w1 + b1
    val, gate = jnp.split(u, 2, axis=-1)
    u = val * jax.nn.sigmoid(gate)
    u = depthwise_conv(u, dw_w) + dw_b
    u = jax.nn.silu(layernorm(u, ln_g, ln_b))
    return u @ w2 + b2


def moe_ffn(h, router_w, router_bias, w_gate, w_up, w_down):
    b, seq_len, d = h.shape
    t = h.reshape(b * seq_len, d)
    scores = jax.nn.sigmoid(t.astype(jnp.float32) @ router_w.astype(jnp.float32))
    sel = scores + router_bias.astype(jnp.float32)
    group_score = jnp.sum(lax.top_k(sel.reshape(-1, MOE_GROUPS, EXPERTS_PER_GROUP), MOE_GROUP_SCORE_K)[0], axis=-1)
    best_group = jnp.argmax(group_score, axis=-1)
    in_group = (jnp.arange(MOE_EXPERTS) // EXPERTS_PER_GROUP)[None, :] == best_group[:, None]
    _, top_idx = lax.top_k(jnp.where(in_group, sel, -jnp.inf), MOE_TOP_K)
    top_w = jnp.take_along_axis(scores, top_idx, axis=-1)
    top_w = top_w / jnp.sum(top_w, axis=-1, keepdims=True)
    combine = jnp.sum(jax.nn.one_hot(top_idx, MOE_EXPERTS, dtype=jnp.float32) * top_w[..., None], axis=1)
    y = jnp.zeros((b * seq_len, d), jnp.float32)
    for e in range(MOE_EXPERTS):
        hid = jax.nn.silu(t @ w_gate[e]) * (t @ w_up[e])
        y = y + combine[:, e:e + 1] * (hid @ w_down[e])
    return y.astype(h.dtype).reshape(b, seq_len, d)


def setup_inputs(seed: int = 0) -> dict:
    key = jax.random.key(seed)
    ks = jax.random.split(key, 28)
    f32 = jnp.float32
    d = D_MODEL

    def nrm(k, shape, scale):
        return jax.random.normal(k, shape, f32) * scale

    dt = jnp.exp(jax.random.uniform(ks[11], (N_EVEN, 2, DN_HEADS), f32, math.log(1e-3), math.log(1e-1)))
    return {
        'x': nrm(ks[0], (BATCH, SEQ, d), 1.0),
        'c': nrm(ks[1], (BATCH, d), 1.0),
        'ctx': nrm(ks[2], (BATCH, CTX_LEN, d), 1.0),
        'c_ctx': nrm(ks[3], (d,), 1.0),
        'ada_w': nrm(ks[4], (DEPTH, d, MOD_CHUNKS * d), 0.5 * d ** -0.5),
        'ada_b': nrm(ks[5], (DEPTH, MOD_CHUNKS * d), 0.02),
        'norm1_g': 1.0 + nrm(ks[6], (DEPTH, d), 0.05),
        'norm2_g': 1.0 + nrm(ks[7], (DEPTH, d), 0.05),
        'hyb_w_in': nrm(ks[8], (N_EVEN, d, HYB_IN_W), d ** -0.5),
        'dn_conv_w': nrm(ks[9], (N_EVEN, DN_SHORT_CONV, QKV_W), DN_SHORT_CONV ** -0.5),
        'dn_a_log': jnp.log(jax.random.uniform(ks[10], (N_EVEN, 2, DN_HEADS), f32, 1.0, 16.0)),
        'dn_dt_bias': dt + jnp.log(-jnp.expm1(-dt)),
        'dn_onorm_g': 1.0 + nrm(ks[12], (N_EVEN, DN_DV), 0.05),
        'hyb_w_out': nrm(ks[13], (N_EVEN, MIX_W, d), MIX_W ** -0.5),
        'conf_w1': nrm(ks[14], (N_ODD, d, 2 * CONF_D), d ** -0.5),
        'conf_b1': nrm(ks[15], (N_ODD, 2 * CONF_D), 0.02),
        'conf_dw_w': nrm(ks[16], (N_ODD, CONF_K, CONF_D), CONF_K ** -0.5),
        'conf_dw_b': nrm(ks[17], (N_ODD, CONF_D), 0.02),
        'conf_ln_g': 1.0 + nrm(ks[18], (N_ODD, CONF_D), 0.05),
        'conf_ln_b': nrm(ks[19], (N_ODD, CONF_D), 0.02),
        'conf_w2': nrm(ks[20], (N_ODD, CONF_D, d), CONF_D ** -0.5),
        'conf_b2': nrm(ks[21], (N_ODD, d), 0.02),
        'router_w': nrm(ks[22], (d, MOE_EXPERTS), d ** -0.5),
        'router_bias': nrm(ks[23], (MOE_EXPERTS,), 0.01),
        'moe_w_gate': nrm(ks[24], (DEPTH, MOE_EXPERTS, d, MOE_FF), d ** -0.5),
        'moe_w_up': nrm(ks[25], (DEPTH, MOE_EXPERTS, d, MOE_FF), d ** -0.5),
        'moe_w_down': nrm(ks[26], (DEPTH, MOE_EXPERTS, MOE_FF, d), MOE_FF ** -0.5),
        'final_g': 1.0 + nrm(ks[27], (d,), 0.05),
    }


def reference(x, c, ctx, c_ctx, ada_w, ada_b, norm1_g, norm2_g, hyb_w_in, dn_conv_w, dn_a_log,
              dn_dt_bias, dn_onorm_g, hyb_w_out, conf_w1, conf_b1, conf_dw_w, conf_dw_b, conf_ln_g,
              conf_ln_b, conf_w2, conf_b2, router_w, router_bias, moe_w_gate, moe_w_up, moe_w_down,
              final_g):
    b, seq_len, d = x.shape
    rows = seq_len // GRID_W
    x = x + grid_sincos(rows, GRID_W, d).astype(x.dtype)[None]
    zero_state = jnp.zeros((b, DN_HEADS, DN_DK, DN_DV), jnp.float32)
    for l in range(DEPTH):
        i = l // 2
        even = l % 2 == 0
        ctx_continues = any(j % 2 == 0 for j in range(l + 1, DEPTH))
        sh1, sc1, g1, sh2, sc2, g2 = [m[:, None, :] for m in adaln(c, ada_w[l], ada_b[l])]
        h = rmsnorm(x, norm1_g[l]) * (1 + sc1) + sh1
        if even or ctx_continues:
            cm = adaln(c_ctx, ada_w[l], ada_b[l])
            hc = rmsnorm(ctx, norm1_g[l]) * (1 + cm[1]) + cm[0]
        if even:
            if ctx_continues:
                ctx_mix, s_f, s_b = hybrid_mixer(hc, hyb_w_in[i], dn_conv_w[i], dn_a_log[i], dn_dt_bias[i],
                                                 dn_onorm_g[i], hyb_w_out[i], zero_state, zero_state)
            else:
                s_f, s_b = dn_context_states(hc, hyb_w_in[i], dn_conv_w[i], dn_a_log[i], dn_dt_bias[i])
            mix, _, _ = hybrid_mixer(h, hyb_w_in[i], dn_conv_w[i], dn_a_log[i], dn_dt_bias[i],
                                     dn_onorm_g[i], hyb_w_out[i], s_f, s_b)
        else:
            mix = conformer_conv(h, conf_w1[i], conf_b1[i], conf_dw_w[i], conf_dw_b[i], conf_ln_g[i],
                                 conf_ln_b[i], conf_w2[i], conf_b2[i])
            if ctx_continues:
                ctx_mix = conformer_conv(hc, conf_w1[i], conf_b1[i], conf_dw_w[i], conf_dw_b[i], conf_ln_g[i],
                                         conf_ln_b[i], conf_w2[i], conf_b2[i])
        x = x + g1 * mix
        x = x + g2 * moe_ffn(rmsnorm(x, norm2_g[l]) * (1 + sc2) + sh2, router_w, router_bias,
                             moe_w_gate[l], moe_w_up[l], moe_w_down[l])
        if ctx_continues:
            ctx = ctx + cm[2] * ctx_mix
            ctx = ctx + cm[5] * moe_ffn(rmsnorm(ctx, norm2_g[l]) * (1 + cm[4]) + cm[3], router_w, router_bias,
                                        moe_w_gate[l], moe_w_up[l], moe_w_down[l])
    return rmsnorm(x, final_g)
```

```python
import numpy as np
import ml_dtypes
from contextlib import ExitStack
import concourse.bass as bass
import concourse.mybir as mybir
from concourse.bass_utils import run_bass_kernel_spmd

F32 = mybir.dt.float32
BF16 = mybir.dt.bfloat16
AF = mybir.ActivationFunctionType
ALU = mybir.AluOpType
AX = mybir.AxisListType

D = 1024
SEQ = 4096
CTX = 256
NEXP = 32
FF = 512
T = 512
NT = SEQ // T
EPS = 1e-6
SAME_ENG_SYNC = True


class Buf:
    def __init__(self, t, name):
        self.t = t
        self.name = name
        self.wr = None
        self.rd = {}
        self.ds = None

    def __getitem__(self, idx):
        return self.t[idx]


class SubBuf:
    def __init__(self, parent, rows, c0, width):
        self.p = parent
        self.rows = rows
        self.c0 = c0
        self.width = width
        self.name = parent.name

    def __getitem__(self, idx):
        rs, cs = idx
        if rs == slice(None):
            rs = slice(0, self.rows)
        a = 0 if cs.start is None else cs.start
        b = self.width if cs.stop is None else cs.stop
        return self.p.t[rs, self.c0 + a:self.c0 + b]

    wr = property(lambda self: self.p.wr, lambda self, v: setattr(self.p, "wr", v))
    rd = property(lambda self: self.p.rd, lambda self, v: setattr(self.p, "rd", v))
    ds = property(lambda self: self.p.ds, lambda self, v: setattr(self.p, "ds", v))


class KB:
    ENG = ("pe", "act", "dve", "pool", "sp")

    def __init__(self, nc):
        self.nc = nc
        self.e = {"pe": nc.tensor, "act": nc.scalar, "dve": nc.vector, "pool": nc.gpsimd, "sp": nc.sync}
        self.sem = {k: nc.alloc_semaphore("s_" + k) for k in self.ENG}
        self.cnt = {k: 0 for k in self.ENG}
        self.seen = {k: {} for k in self.ENG}
        self.dpool = {"sp": [], "pool": [], "act": []}
        self.dall = []
        self.gstack = ExitStack()
        self.stack = None
        self.stage_bufs = []
        self.nbank = 0
        self.ps = [Buf(nc.alloc_psum_tensor("ps%d" % i, [128, 512], F32), "ps%d" % i) for i in range(8)]
        self.rr = 0
        self.nstage = 0

    def sb(self, name, shape, dtype, persistent=False):
        st = self.gstack if persistent else self.stack
        t = st.enter_context(self.nc.sbuf_tensor("sb%d_%s" % (self.nstage, name), list(shape), dtype))
        b = Buf(t, name)
        if not persistent:
            self.stage_bufs.append(b)
        return b

    def begin(self):
        self.nstage += 1
        self.stack = ExitStack()
        self.stage_bufs = []

    def end(self):
        self.barrier()
        for b in self.stage_bufs:
            if b.ds is not None:
                for q_, ds_ in b.ds.items():
                    self.dpool[q_].append(ds_)
                b.ds = None
        self.stack.close()
        self.stack = None

    def bank(self):
        b = self.ps[self.nbank % 8]
        self.nbank += 1
        return b

    def _wait(self, eng, ev):
        key, sem, val = ev
        if self.seen[eng].get(key, 0) >= val:
            return
        self.e[eng].wait_ge(sem, val)
        self.seen[eng][key] = val

    def _deps(self, eng, reads, writes):
        own = "e:" + eng
        evs = []
        for b in reads:
            if b.wr is not None:
                evs.append(b.wr)
        for b in writes:
            if b.wr is not None:
                evs.append(b.wr)
            evs.extend(b.rd.values())
        for ev in evs:
            if ev[0] == own and (eng == "pe" or not SAME_ENG_SYNC):
                continue
            self._wait(eng, ev)

    def _commit(self, eng, ins, reads, writes):
        own = "e:" + eng
        self.cnt[eng] += 1
        ins.then_inc(self.sem[eng], 1)
        ev = (own, self.sem[eng], self.cnt[eng])
        for b in reads:
            b.rd[own] = ev
        for b in writes:
            b.wr = ev
            b.rd = {}

    def op(self, eng, fn, reads=(), writes=()):
        self._deps(eng, reads, writes)
        ins = fn(self.e[eng])
        self._commit(eng, ins, reads, writes)
        return ins

    def mm(self, outb, steps, reads):
        self._deps("pe", reads, [outb])
        ins = None
        for (o, l, r, st, sp) in steps:
            ins = self.nc.tensor.matmul(o, lhsT=l, rhs=r, start=st, stop=sp)
        self._commit("pe", ins, reads, [outb])

    def tr(self, outb, steps, reads, ident):
        self._deps("pe", list(reads) + [ident], [outb])
        ins = None
        for (o, i) in steps:
            ins = self.nc.tensor.transpose(out=o, in_=i, identity=ident[0:i.shape[0], 0:i.shape[0]])
        self._commit("pe", ins, list(reads) + [ident], [outb])

    def _dsem(self, b, q):
        if b.ds is None:
            b.ds = {}
        if q not in b.ds:
            if self.dpool[q]:
                b.ds[q] = self.dpool[q].pop()
            else:
                name = "d%d" % len(self.dall)
                b.ds[q] = [self.nc.alloc_semaphore(name), 0, "d:" + name]
                self.dall.append(b.ds[q])
        return b.ds[q]

    def dma(self, q, out, in_, buf, load):
        if load:
            self._deps(q, [], [buf])
        else:
            self._deps(q, [buf], [])
        ds = self._dsem(buf, q)
        ds[1] += 16
        self.e[q].dma_start(out=out, in_=in_).then_inc(ds[0], 16)
        ev = (ds[2], ds[0], ds[1])
        if load:
            buf.wr = ev
            buf.rd = {}
        else:
            buf.rd[ds[2]] = ev

    def idma(self, out, in_, idxb, idx_ap, buf, gather, nrows):
        q = "pool"
        if gather:
            self._deps(q, [idxb], [buf])
        else:
            self._deps(q, [buf, idxb], [])
        ds = self._dsem(buf, q)
        ds[1] += 16
        off = bass.IndirectOffsetOnAxis(ap=idx_ap, axis=0)
        if gather:
            self.nc.gpsimd.indirect_dma_start(out=out, out_offset=None, in_=in_, in_offset=off).then_inc(ds[0], 16)
        else:
            r_ = self.nc.gpsimd.indirect_dma_start(out=out, out_offset=off, in_=in_, in_offset=None)
            r_.then_inc(ds[0], 16)
        ev = (ds[2], ds[0], ds[1])
        if gather:
            buf.wr = ev
            buf.rd = {}
        else:
            buf.rd[ds[2]] = ev
        idxb.rd[ds[2]] = ev

    def load(self, buf, out, in_, q="sp"):
        self.dma(q, out, in_, buf, True)

    def store(self, buf, out, in_, q="sp"):
        self.dma(q, out, in_, buf, False)

    def barrier(self):
        evs = [("e:" + k, self.sem[k], self.cnt[k]) for k in ("pe", "act", "dve", "pool") if self.cnt[k] > 0]
        evs += [(d[2], d[0], d[1]) for d in self.dall if d[1] > 0]
        for eng in self.ENG:
            for ev in evs:
                if ev[0] == "e:" + eng:
                    continue
                self._wait(eng, ev)

    def cp(self, eng, outb, out, inb, in_):
        if eng == "act":
            self.op("act", lambda e: e.copy(out=out, in_=in_), [inb], [outb])
        else:
            self.op(eng, lambda e: e.tensor_copy(out=out, in_=in_), [inb], [outb])


def _dram(nc, name, shape, dt, dbg):
    return nc.dram_tensor(name, list(shape), dt, kind=("ExternalOutput" if dbg else "Internal")).ap()


class Prog:
    def __init__(self, stages=None, dbg=(), ext_in=()):
        self.stages = stages
        self.dbgset = set(dbg)
        nc = bass.Bass("TRN2", target_bir_lowering=False)
        self.nc = nc
        self.k = KB(nc)
        self.inp = {}
        self.ext_in = set(ext_in)
        self.scr = {}

    def I(self, name, shape, dt=F32):
        if name not in self.inp:
            self.inp[name] = self.nc.dram_tensor(name, list(shape), dt, kind="ExternalInput").ap()
        return self.inp[name]

    def S(self, name, shape, dt=F32):
        if name not in self.scr:
            if name in self.ext_in:
                self.scr[name] = self.nc.dram_tensor(name, list(shape), dt, kind="ExternalInput").ap()
            else:
                self.scr[name] = _dram(self.nc, name, shape, dt, name in self.dbgset)
        return self.scr[name]

    def consts(self):
        k = self.k
        nc = self.nc
        self.ident = k.sb("ident", [128, 128], F32, True)
        self.ones = k.sb("ones", [128, 128], F32, True)
        k.op("pool", lambda e: e.memset(self.ident[:, :], 0.0), [], [self.ident])
        k.op("pool", lambda e: e.affine_select(out=self.ident[:, :], in_=self.ident[:, :], pattern=[[-1, 128]],
                                               compare_op=ALU.not_equal, fill=1.0, base=0, channel_multiplier=1),
             [self.ident], [self.ident])
        k.op("pool", lambda e: e.memset(self.ones[:, :], 1.0), [], [self.ones])
        self.NG = 11
        self.gains = k.sb("gains", [128, self.NG, 8], F32, True)
        k.load(self.gains, self.gains[:, :, :], self.I("gains", [128, self.NG, 8]))
        self.coef = k.sb("coef", [128, 2, 6, 8], F32, True)
        self.coefc = k.sb("coefc", [128, 2, 8], F32, True)
        self.eps_c = k.sb("eps_c", [128, 1], F32, True)
        self.gbtok = k.sb("gbtok", [64, 68, 16], F32, True)
        self.SLOTI_p = k.sb("SLOTI", [128, 32], mybir.dt.int32, True)
        self.IDXI_p = k.sb("IDXI", [128, 64], mybir.dt.int32, True)
        k.op("pool", lambda e: e.memset(self.eps_c[:, :], EPS), [], [self.eps_c])

    def start_wcast(self):
        nc = self.nc
        NR = 2 * NEXP * 128
        srcs = (self.I("moe_wgl", [NR, 8 * FF]), self.I("moe_wul", [NR, 8 * FF]), self.I("moe_wdl", [NR, 4 * D]))
        self.w16 = (self.S("W16g", [NR, 8 * FF], BF16), self.S("W16u", [NR, 8 * FF], BF16), self.S("W16d", [NR, 4 * D], BF16))
        self.wc_sem = nc.alloc_semaphore("wcast")
        self.wc_todo = []
        self.wc_issued = 0
        for l in range(2):
            for e_ in range(NEXP):
                r0 = (l * NEXP + e_) * 128
                for src, dst in zip(srcs, self.w16):
                    self.wc_todo.append((dst[r0:r0 + 128, :], src[r0:r0 + 128, :]))

    def wcast_tick(self, n=1, window=6):
        nc = self.nc
        for _ in range(n):
            if not self.wc_todo:
                return
            if self.wc_issued >= window:
                nc.gpsimd.wait_ge(self.wc_sem, 16 * (self.wc_issued - window + 1))
            dst, src = self.wc_todo.pop(0)
            nc.gpsimd.dma_start(out=dst, in_=src).then_inc(self.wc_sem, 16)
            self.wc_issued += 1

    def wcast_finish(self):
        self.wcast_tick(n=10 ** 6)
        ev = ("d:wcast", self.wc_sem, 16 * self.wc_issued)
        for eng_ in KB.ENG:
            self.k._wait(eng_, ev)

    def stage_mods(self):
        k = self.k
        k.begin()
        ada_w = self.I("ada_w", [2, D, 6 * D])
        ada_b = self.I("ada_b", [128, 2, 48])
        cc_in = self.I("cc", [128, 8, 2])
        cc = k.sb("cc", [128, 8, 2], F32)
        scc = k.sb("scc", [128, 8, 2], F32)
        adab = k.sb("adab", [128, 2, 48], F32)
        modT = k.sb("modT", [128, 2, 48, 2], F32)
        wp = [k.sb("adaw%d" % i, [128, 8, 768], F32) for i in range(2)]
        k.load(cc, cc[:, :, :], cc_in)
        k.load(adab, adab[:, :, :], ada_b)
        k.op("act", lambda e: e.activation(out=scc[:, :, :], in_=cc[:, :, :], func=AF.Silu), [cc], [scc])
        n = 0
        for l in range(2):
            bank = k.bank()
            for pnl in range(8):
                w = wp[n % 2]
                n += 1
                k.load(w, w[:, :, :], ada_w[l, :, pnl * 768:(pnl + 1) * 768].rearrange("(k p) n -> p k n", p=128),
                       q=("sp" if n % 2 else "pool"))
                steps = []
                for jj in range(6):
                    jc = pnl * 6 + jj
                    for kc in range(8):
                        steps.append((bank[:, jc * 2:jc * 2 + 2], w[:, kc, jj * 128:(jj + 1) * 128], scc[:, kc, :],
                                      kc == 0, kc == 7))
                k.mm(bank, steps, [w, scc])
            for col in range(2):
                src = bank[:, 0:96].rearrange("p (j c) -> p j c", c=2)[:, :, col]
                k.op("dve", lambda e, src=src, col=col, l=l: e.tensor_tensor(out=modT[:, l, :, col], in0=src,
                                                                             in1=adab[:, l, :], op=ALU.add),
                     [bank, adab], [modT])
        g = self.gains
        cf = self.coef
        for l in range(2):
            k.op("dve", lambda e, l=l: e.scalar_tensor_tensor(out=cf[:, l, 0, :], in0=modT[:, l, 8:16, 0], scalar=1.0,
                                                             in1=g[:, 0 + l, :], op0=ALU.add, op1=ALU.mult),
                 [modT, g], [cf])
            k.op("dve", lambda e, l=l: e.tensor_copy(out=cf[:, l, 1, :], in_=modT[:, l, 0:8, 0]), [modT], [cf])
            k.op("dve", lambda e, l=l: e.tensor_copy(out=cf[:, l, 2, :], in_=modT[:, l, 16:24, 0]), [modT], [cf])
            k.op("dve", lambda e, l=l: e.scalar_tensor_tensor(out=cf[:, l, 3, :], in0=modT[:, l, 32:40, 0], scalar=1.0,
                                                             in1=g[:, 2 + l, :], op0=ALU.add, op1=ALU.mult),
                 [modT, g], [cf])
            k.op("dve", lambda e, l=l: e.tensor_copy(out=cf[:, l, 4, :], in_=modT[:, l, 24:32, 0]), [modT], [cf])
            k.op("dve", lambda e, l=l: e.tensor_copy(out=cf[:, l, 5, :], in_=modT[:, l, 40:48, 0]), [modT], [cf])
        cfc = self.coefc
        k.op("dve", lambda e: e.scalar_tensor_tensor(out=cfc[:, 0, :], in0=modT[:, 0, 8:16, 1], scalar=1.0,
                                                     in1=g[:, 0, :], op0=ALU.add, op1=ALU.mult), [modT, g], [cfc])
        k.op("dve", lambda e: e.tensor_copy(out=cfc[:, 1, :], in_=modT[:, 0, 0:8, 1]), [modT], [cfc])
        if "coef" in self.dbgset:
            k.store(cf, self.S("coef", [128, 2, 6, 8]), cf[:, :, :, :])
            k.store(cfc, self.S("coefc", [128, 2, 8]), cfc[:, :, :])
        k.end()

    def load_w16(self, dst, src, kch, ncols, stg, n0=0):
        k = self.k
        engs = ("dve", "act")
        n = n0
        for c0 in range(0, ncols, 256):
            w = min(256, ncols - c0)
            s = stg[n % 2]
            k.load(s, s[:, :, 0:w], src[:, c0:c0 + w].rearrange("(k p) n -> p k n", p=128), q=("sp" if n % 2 else "pool"))
            k.cp(engs[n % 2], dst, dst[:, :, c0:c0 + w], s, s[:, :, 0:w])
            n += 1
        return n

    def rstd_fm(self, xT, nch, Tn, sq, rstd, mean_scale):
        k = self.k
        k.op("act", lambda e: e.activation(out=sq[:, 0:nch, 0:Tn], in_=xT[:, 0:nch, 0:Tn], func=AF.Square), [xT], [sq])
        bank = k.bank()
        k.mm(bank, [(bank[:, 0:Tn], self.ones[:, :], sq[:, c, 0:Tn], c == 0, c == nch - 1) for c in range(nch)],
             [self.ones, sq])
        k.op("act", lambda e: e.activation(out=rstd[:, 0:Tn], in_=bank[:, 0:Tn], func=AF.Sqrt, bias=self.eps_c[:, 0:1],
                                           scale=mean_scale), [bank, self.eps_c], [rstd])
        k.op("dve", lambda e: e.reciprocal(out=rstd[:, 0:Tn], in_=rstd[:, 0:Tn]), [rstd], [rstd])

    def modulate(self, xT, rstd, A, Ab, Bc, Bb, tmp, hT, Tn):
        k = self.k
        for c in range(8):
            k.op("dve", lambda e, c=c: e.scalar_tensor_tensor(out=tmp[:, c, 0:Tn], in0=xT[:, c, 0:Tn], scalar=A[:, c:c + 1],
                                                             in1=rstd[:, 0:Tn], op0=ALU.mult, op1=ALU.mult),
                 [xT, rstd, Ab], [tmp])
        for c in range(8):
            k.op("act", lambda e, c=c: e.activation(out=hT[:, c, 0:Tn], in_=tmp[:, c, 0:Tn], func=AF.Identity,
                                                   bias=Bc[:, c:c + 1], scale=1.0), [tmp, Bb], [hT])

    def stage_inproj(self):
        k = self.k
        k.begin()
        x = self.I("x", [SEQ, D])
        pos = self.I("pos", [SEQ, D])
        ctx = self.I("ctx", [CTX, D])
        XT = self.S("XT", [D, SEQ])
        QKV = self.S("QKV_T", [1536, SEQ])
        QKVc = self.S("QKVc_T", [1536, CTX])
        AT = self.S("A_T", [8, SEQ])
        BT = self.S("B_T", [8, SEQ])
        ATc = self.S("Ac_T", [8, CTX])
        BTc = self.S("Bc_T", [8, CTX])
        Z = self.S("Z", [SEQ, 512])
        GC = self.S("GC", [SEQ, 512], BF16)
        GS = self.S("GS", [SEQ, 512], BF16)
        stg = [k.sb("stg%d" % i, [128, 8, 256], F32) for i in range(2)]
        wqkv = k.sb("wqkv", [128, 8, 1536], BF16)
        wab = k.sb("wab", [128, 8, 16], BF16)
        wz = k.sb("wz", [128, 8, 512], BF16)
        wf = k.sb("wf", [128, 8, 512], BF16)
        csch = k.sb("csch", [128, 256], BF16)
        n = self.load_w16(wqkv, self.I("w_qkv", [D, 1536]), 8, 1536, stg)
        n = self.load_w16(wab, self.I("w_ab", [D, 16]), 8, 16, stg, n)
        n = self.load_w16(wz, self.I("w_z", [D, 512]), 8, 512, stg, n)
        n = self.load_w16(wf, self.I("w_f", [D, 512]), 8, 512, stg, n)
        k.load(csch, csch[:, :], self.I("cs_ch", [128, 256], BF16))
        xin = [k.sb("xin%d" % i, [128, 4, D], F32) for i in range(2)]
        pin = k.sb("pin", [128, 4, D], F32)
        xpT = k.sb("xpT", [128, 8, T], F32)
        tmp = k.sb("tmp", [128, 8, T], F32)
        rstd = k.sb("rstd", [128, T], F32)
        hT = k.sb("hT", [128, 8, T], BF16)
        ev = [k.sb("ev%d" % i, [128, T], F32) for i in range(4)]
        fT = k.sb("fT", [128, 4, T], BF16)
        gcs = [k.sb("gcs%d" % i, [128, 4, 256], BF16) for i in range(2)]
        cf = self.coef
        cfc = self.coefc
        nev = 0
        tiles = [("ctx", 0, CTX)] + [("x", i, T) for i in range(NT)]
        for ti, (kind, i, Tn) in enumerate(tiles):
            nj = Tn // 128
            xi = xin[ti % 2]
            if kind == "x":
                k.load(xi, xi[:, :, :], x[i * T:(i + 1) * T, :].rearrange("(j p) d -> p j d", p=128))
                k.load(pin, pin[:, :, :], pos[i * T:(i + 1) * T, :].rearrange("(j p) d -> p j d", p=128), q="pool")
                k.op("dve", lambda e, xi=xi: e.tensor_tensor(out=xi[:, :, :], in0=xi[:, :, :], in1=pin[:, :, :], op=ALU.add),
                     [xi, pin], [xi])
            else:
                k.load(xi, xi[:, 0:nj, :], ctx.rearrange("(j p) d -> p j d", p=128))
            for c in range(8):
                bank = k.bank()
                k.tr(bank, [(bank[:, j * 128:(j + 1) * 128], xi[:, j, c * 128:(c + 1) * 128]) for j in range(nj)], [xi],
                     self.ident)
                k.cp("act" if c % 2 else "dve", xpT, xpT[:, c, 0:Tn], bank, bank[:, 0:Tn])
            if kind == "x":
                k.store(xpT, XT[:, i * T:(i + 1) * T].rearrange("(c p) t -> p c t", p=128), xpT[:, :, :])
            self.rstd_fm(xpT, 8, Tn, tmp, rstd, 1.0 / D)
            if kind == "x":
                self.modulate(xpT, rstd, cf[:, 0, 0, :], cf, cf[:, 0, 1, :], cf, tmp, hT, Tn)
            else:
                self.modulate(xpT, rstd, cfc[:, 0, :], cfc, cfc[:, 1, :], cfc, tmp, hT, Tn)
            sl = slice(i * T, (i + 1) * T) if kind == "x" else slice(0, CTX)
            dq = QKV if kind == "x" else QKVc
            for oc in range(12):
                bank = k.bank()
                k.mm(bank, [(bank[:, 0:Tn], wqkv[:, kc, oc * 128:(oc + 1) * 128], hT[:, kc, 0:Tn], kc == 0, kc == 7)
                            for kc in range(8)], [wqkv, hT])
                e = ev[nev % 4]
                k.cp("act" if nev % 2 else "dve", e, e[:, 0:Tn], bank, bank[:, 0:Tn])
                k.store(e, dq[oc * 128:(oc + 1) * 128, sl], e[:, 0:Tn], q=("sp" if nev % 2 else "pool"))
                nev += 1
            for gi, dst in enumerate(((AT, BT) if kind == "x" else (ATc, BTc))):
                bank = k.bank()
                k.mm(bank, [(bank[0:8, 0:Tn], wab[:, kc, gi * 8:(gi + 1) * 8], hT[:, kc, 0:Tn], kc == 0, kc == 7)
                            for kc in range(8)], [wab, hT])
                e = ev[nev % 4]
                k.cp("dve", e, e[0:8, 0:Tn], bank, bank[0:8, 0:Tn])
                k.store(e, dst[:, sl], e[0:8, 0:Tn])
                nev += 1
            if kind != "x":
                continue
            for j in range(4):
                bank = k.bank()
                k.mm(bank, [(bank[:, :], hT[:, kc, j * 128:(j + 1) * 128], wz[:, kc, :], kc == 0, kc == 7)
                            for kc in range(8)], [wz, hT])
                e = ev[nev % 4]
                k.op("act", lambda e_, e=e, bank=bank: e_.activation(out=e[:, :], in_=bank[:, :], func=AF.Silu), [bank], [e])
                k.store(e, Z[i * T + j * 128:i * T + (j + 1) * 128, :], e[:, :], q=("sp" if nev % 2 else "pool"))
                nev += 1
            for oc in range(4):
                bank = k.bank()
                k.mm(bank, [(bank[:, :], wf[:, kc, oc * 128:(oc + 1) * 128], hT[:, kc, :], kc == 0, kc == 7)
                            for kc in range(8)], [wf, hT])
                k.cp("act" if oc % 2 else "dve", fT, fT[:, oc, :], bank, bank[:, :])
            for j in range(4):
                gb = gcs[j % 2]
                for half in range(2):
                    bank = k.bank()
                    k.mm(bank, [(bank[:, (gg - 2 * half) * 256:(gg - 2 * half + 1) * 256], fT[:, gg, j * 128:(j + 1) * 128],
                                 csch[:, :], True, True) for gg in range(2 * half, 2 * half + 2)], [fT, csch])
                    k.cp("act" if half else "dve", gb, gb[:, 2 * half:2 * half + 2, :],
                         bank, bank[:, :].rearrange("p (g c) -> p g c", c=256))
                r0 = i * T + j * 128
                k.store(gb, GC[r0:r0 + 128, :].rearrange("t (g c) -> t g c", c=128), gb[:, :, 0:128])
                k.store(gb, GS[r0:r0 + 128, :].rearrange("t (g c) -> t g c", c=128), gb[:, :, 128:256], q="pool")
        k.end()

    def stage_conv(self):
        k = self.k
        k.begin()
        convw = k.sb("convw", [128, 12, 5], F32)
        k.load(convw, convw[:, :, :], self.I("conv_w", [128, 12, 5]))
        adt = k.sb("adt", [8, 2], F32)
        k.load(adt, adt[:, :], self.I("adt", [8, 2]))
        one_c = k.sb("one_c", [128, 1], F32)
        k.op("pool", lambda e: e.memset(one_c[:, :], 1.0), [], [one_c])
        nexpa = k.sb("nexpa", [8, 1], F32)
        k.op("act", lambda e: e.activation(out=nexpa[:, :], in_=adt[:, 0:1], func=AF.Exp), [adt], [nexpa])
        k.op("dve", lambda e: e.tensor_scalar(out=nexpa[:, :], in0=nexpa[:, :], scalar1=-1.0, scalar2=None, op0=ALU.mult),
             [nexpa], [nexpa])
        gb = self.gbtok
        for (L, qname, aname, bname, oname, ch0) in ((CTX, "QKVc_T", "Ac_T", "Bc_T", "QNc_T", 0), (SEQ, "QKV_T", "A_T", "B_T", "QN_T", 4)):
            src = self.S(qname, [1536, L])
            dst = self.S(oname, [1536, L])
            xin = [k.sb("cx%d_%d" % (L, i), [128, L + 4], F32) for i in range(2)]
            acc = [k.sb("ca%d_%d" % (L, i), [128, L], F32) for i in range(2)]
            sq = k.sb("csq%d" % L, [128, L], F32)
            rin = k.sb("crin%d" % L, [128, L], F32)
            for b_ in xin:
                k.op("pool", lambda e, b_=b_: e.memset(b_[:, :], 0.0), [], [b_])
            for cc in range(12):
                xi = xin[cc % 2]
                a = acc[cc % 2]
                eng = "dve"
                k.load(xi, xi[:, 2:L + 2], src[cc * 128:(cc + 1) * 128, :], q=("sp" if cc % 2 else "pool"))
                k.op(eng, lambda e, xi=xi, a=a, cc=cc: e.tensor_scalar(out=a[:, :], in0=xi[:, 0:L], scalar1=convw[:, cc, 0:1],
                                                                      scalar2=None, op0=ALU.mult), [xi, convw], [a])
                for j in range(1, 5):
                    k.op(eng, lambda e, xi=xi, a=a, cc=cc, j=j: e.scalar_tensor_tensor(
                        out=a[:, :], in0=xi[:, j:j + L], scalar=convw[:, cc, j:j + 1], in1=a[:, :], op0=ALU.mult, op1=ALU.add),
                        [xi, convw, a], [a])
                k.op("act", lambda e, a=a: e.activation(out=a[:, :], in_=a[:, :], func=AF.Silu), [a], [a])
                if cc < 8:
                    k.op("act", lambda e, a=a: e.activation(out=sq[:, :], in_=a[:, :], func=AF.Square), [a], [sq])
                    for t0 in range(0, L, 512):
                        w = min(512, L - t0)
                        bank = k.bank()
                        k.mm(bank, [(bank[:, 0:w], self.ones[:, :], sq[:, t0:t0 + w], True, True)], [self.ones, sq])
                        k.op("act", lambda e, bank=bank, t0=t0, w=w: e.activation(out=rin[:, t0:t0 + w], in_=bank[:, 0:w], func=AF.Sqrt,
                                                                                bias=self.eps_c[:, 0:1], scale=1.0), [bank, self.eps_c], [rin])
                    k.op("dve", lambda e: e.reciprocal(out=rin[:, :], in_=rin[:, :]), [rin], [rin])
                    k.op("dve", lambda e, a=a: e.tensor_tensor(out=a[:, :], in0=a[:, :], in1=rin[:, :], op=ALU.mult), [a, rin], [a])
                k.store(a, dst[cc * 128:(cc + 1) * 128, :], a[:, :], q=("sp" if cc % 2 == 0 else "pool"))
            ga = k.sb("ga%d" % L, [8, L], F32)
            gbb = k.sb("gb%d" % L, [8, L], F32)
            gy = k.sb("gy%d" % L, [8, L], F32)
            gl = k.sb("gl%d" % L, [8, L], F32)
            k.load(ga, ga[:, :], self.S(aname, [8, L]))
            k.load(gbb, gbb[:, :], self.S(bname, [8, L]))
            k.op("dve", lambda e: e.tensor_scalar(out=gy[:, :], in0=ga[:, :], scalar1=adt[:, 1:2], scalar2=None, op0=ALU.add), [ga, adt], [gy])
            k.op("act", lambda e: e.activation(out=gl[:, :], in_=gy[:, :], func=AF.Abs), [gy], [gl])
            k.op("act", lambda e: e.activation(out=gl[:, :], in_=gl[:, :], func=AF.Exp, scale=-1.0), [gl], [gl])
            k.op("act", lambda e: e.activation(out=gl[:, :], in_=gl[:, :], func=AF.Ln, bias=one_c[0:8, 0:1], scale=1.0), [gl, one_c], [gl])
            k.op("dve", lambda e: e.tensor_scalar(out=gy[:, :], in0=gy[:, :], scalar1=0.0, scalar2=None, op0=ALU.max), [gy], [gy])
            k.op("dve", lambda e: e.tensor_tensor(out=gy[:, :], in0=gy[:, :], in1=gl[:, :], op=ALU.add), [gy, gl], [gy])
            k.op("dve", lambda e: e.tensor_scalar(out=gy[:, :], in0=gy[:, :], scalar1=nexpa[:, 0:1], scalar2=None, op0=ALU.mult), [gy, nexpa], [gy])
            k.op("act", lambda e: e.activation(out=gbb[:, :], in_=gbb[:, :], func=AF.Sigmoid), [gbb], [gbb])
            nch = L // 64
            for gi, srcb in enumerate((gy, gbb)):
                bank = k.bank()
                k.tr(bank, [(bank[0:64, n * 8:(n + 1) * 8], srcb[:, n * 64:(n + 1) * 64]) for n in range(nch)], [srcb], self.ident)
                k.op("dve", lambda e, bank=bank, gi=gi: e.tensor_copy(out=gb[:, ch0:ch0 + nch, gi * 8:(gi + 1) * 8],
                                                                      in_=bank[0:64, 0:nch * 8].rearrange("p (n c) -> p n c", c=8)), [bank], [gb])
        if "gbtok" in self.dbgset:
            k.store(gb, self.S("gbtok", [64, 68, 16]), gb[:, :, :])
        k.end()

    def stage_delta(self):
        k = self.k
        k.begin()
        QN = self.S("QN_T", [1536, SEQ])
        QNc = self.S("QNc_T", [1536, CTX])
        Z = self.S("Z", [SEQ, 512])
        OFB = [self.S("OF", [SEQ, 512]), self.S("OB", [SEQ, 512])]
        DN = self.S("DN_T", [512, SEQ], BF16)
        gb = self.gbtok
        ident = self.ident
        ones = self.ones
        dmask = k.sb("dmask", [64, 4, 64], F32)
        k.load(dmask, dmask[:, :, :], self.I("dmask", [64, 4, 64]))
        qt = [k.sb("qt%d" % i, [128, 12, 512], F32) for i in range(2)]
        qt16 = [k.sb("qt16_%d" % i, [128, 12, 512], BF16) for i in range(2)]
        S = [[k.sb("S%d_%d" % (d, h), [128, 128], F32) for h in range(4)] for d in range(2)]
        ob = [[k.sb("ob%d_%d" % (d, i), [64, 4, 128], F32) for i in range(2)] for d in range(2)]

        def mk(tag):
            d_ = {}
            big = k.sb("wk_%s" % tag, [128, 1936], F32)
            c = 0
            for nm, shp in (("gB", [64, 128]), ("bB", [64, 64]), ("E", [64, 64]), ("DTm", [64, 64]), ("DTs", [64, 64]),
                            ("Bm", [64, 64]), ("Am", [64, 64]), ("QKm", [64, 64]), ("PQ0", [64, 128]), ("PQ1", [64, 128]),
                            ("TTA0", [64, 128]), ("TTA1", [64, 128]), ("ru", [64, 128]), ("rw", [64, 128]), ("kt", [64, 128]),
                            ("U", [64, 128]), ("Vn", [64, 128]), ("O2", [64, 128]), ("WT", [128, 64]), ("sc", [128, 8])):
                d_[nm] = SubBuf(big, shp[0], c, shp[1])
                c += shp[1]
            big16 = k.sb("wk16_%s" % tag, [128, 512], BF16)
            c = 0
            for nm, shp in (("WT16", [128, 64]), ("S16", [128, 128]), ("kt16", [64, 128]), ("QKm16", [64, 64]), ("Vn16", [64, 128])):
                d_[nm] = SubBuf(big16, shp[0], c, shp[1])
                c += shp[1]
            return d_
        W = [[mk("%d%d" % (d, h)) for h in range(4)] for d in range(2)]
        sc_dk = 128.0 ** -0.5

        def sched_of(d):
            sched = []
            cchunks = list(range(4))
            mchunks = list(range(64))
            if d == 1:
                cchunks.reverse()
                mchunks.reverse()
            for n in cchunks:
                sched.append((False, 0, n, n, n * 64))
            for n in mchunks:
                sched.append((True, n // 8, n % 8, 4 + n, n * 64))
            return sched

        def chain(d, h):
            w = W[d][h]
            Sh = S[d][h]
            MI = dmask[:, 2 * d, :]
            MS = dmask[:, 2 * d + 1, :]
            tb = qt[d]
            tb16 = qt16[d]
            pb = k.ps[d * 4 + h]
            k.op("pool", lambda e: e.memset(Sh[:, :], 0.0), [], [Sh])
            k.op("dve", lambda e: e.memset(w["S16"][:, :], 0.0), [], [w["S16"]])
            cur_tile = None
            for si, (is_main, ti, cin, gi, tok0) in enumerate(sched_of(d)):
                key = (is_main, ti)
                if key != cur_tile:
                    cur_tile = key
                    if h == 0:
                        if is_main:
                            k.load(tb, tb[:, :, :], QN[:, ti * 512:(ti + 1) * 512].rearrange("(c p) t -> p c t", p=128), q="sp")
                            wdt = 512
                        else:
                            k.load(tb, tb[:, :, 0:CTX], QNc.rearrange("(c p) t -> p c t", p=128), q="sp")
                            wdt = CTX
                        k.op("act", lambda e: e.copy(out=tb16[:, 0:4, 0:wdt], in_=tb[:, 0:4, 0:wdt]), [tb], [tb16])
                        k.op("dve", lambda e: e.tensor_copy(out=tb16[:, 4:8, 0:wdt], in_=tb[:, 4:8, 0:wdt]), [tb], [tb16])
                c0 = cin * 64
                oc = ob[d][si % 2]
                QT = tb[:, h, c0:c0 + 64]
                KT = tb[:, 4 + h, c0:c0 + 64]
                VT = tb[:, 8 + h, c0:c0 + 64]
                QT16 = tb16[:, h, c0:c0 + 64]
                KT16 = tb16[:, 4 + h, c0:c0 + 64]
                gcol = gb[:, gi, d * 4 + h:d * 4 + h + 1]
                bcol = gb[:, gi, 8 + d * 4 + h:8 + d * 4 + h + 1]
                sc = w["sc"]
                steps = [(pb[0:64, 256:320], KT16, KT16, True, True)]
                if is_main:
                    steps.append((pb[0:64, 320:384], KT16, QT16, True, True))
                k.mm(pb, steps, [tb16])
                k.tr(pb, [(pb[0:64, 0:128], KT), (pb[0:64, 128:256], VT)], [tb], ident)
                k.op("dve", lambda e: e.tensor_scalar(out=w["gB"][:, :], in0=ones[0:64, 0:128], scalar1=gcol, scalar2=None, op0=ALU.mult), [ones, gb], [w["gB"]])
                k.op("pool", lambda e: e.tensor_scalar(out=w["bB"][:, :], in0=ones[0:64, 0:64], scalar1=bcol, scalar2=None, op0=ALU.mult), [ones, gb], [w["bB"]])
                yield
                k.op("act", lambda e: e.activation(out=w["kt"][:, :], in_=pb[0:64, 0:128], func=AF.Copy), [pb], [w["kt"]])
                k.op("dve", lambda e: e.tensor_copy(out=w["U"][:, :], in_=pb[0:64, 128:256]), [pb], [w["U"]])
                yield
                k.mm(pb, [(pb[0:128, 384:448], w["gB"][:, :], MI, True, True),
                          (pb[0:64, 448:512], w["bB"][:, :], ident[0:64, 0:64], True, True),
                          (pb[0:64, 0:1], MI, gcol, True, True)], [w["gB"], w["bB"], dmask, ident, ones, gb])
                lastc = 384 + (63 if d == 0 else 0)
                yield
                k.op("dve", lambda e: e.tensor_copy(out=sc[:, 1:2], in_=pb[:, lastc:lastc + 1]), [pb], [sc])
                k.op("dve", lambda e: e.tensor_copy(out=sc[0:64, 0:1], in_=pb[0:64, 0:1]), [pb], [sc])
                yield
                k.op("dve", lambda e: e.tensor_scalar(out=w["E"][:, :], in0=pb[0:64, 384:448], scalar1=sc[0:64, 0:1], scalar2=0.0, op0=ALU.subtract, op1=ALU.min), [pb, sc], [w["E"]])
                k.op("act", lambda e: e.activation(out=sc[0:64, 2:3], in_=sc[0:64, 0:1], func=AF.Exp), [sc], [sc])
                k.op("act", lambda e: e.activation(out=sc[0:64, 3:4], in_=sc[0:64, 0:1], func=AF.Exp, bias=sc[0:64, 1:2], scale=-1.0), [sc], [sc])
                k.op("act", lambda e: e.activation(out=sc[:, 4:5], in_=sc[:, 1:2], func=AF.Exp), [sc], [sc])
                yield
                k.op("act", lambda e: e.activation(out=w["E"][:, :], in_=w["E"][:, :], func=AF.Exp), [w["E"]], [w["E"]])
                k.op("dve", lambda e: e.tensor_tensor(out=sc[0:64, 5:6], in0=sc[0:64, 2:3], in1=bcol, op=ALU.mult), [sc, gb], [sc])
                k.op("dve", lambda e: e.tensor_scalar(out=sc[0:64, 6:7], in0=sc[0:64, 2:3], scalar1=sc_dk, scalar2=None, op0=ALU.mult), [sc], [sc])
                k.op("dve", lambda e: e.tensor_scalar(out=w["ru"][:, :], in0=w["U"][:, :], scalar1=bcol, scalar2=None, op0=ALU.mult), [w["U"], gb], [w["ru"]])
                yield
                k.op("pool", lambda e: e.tensor_tensor(out=w["DTs"][:, :], in0=w["E"][:, :], in1=MS, op=ALU.mult), [w["E"], dmask], [w["DTs"]])
                if is_main:
                    k.op("pool", lambda e: e.tensor_tensor(out=w["DTm"][:, :], in0=w["E"][:, :], in1=MI, op=ALU.mult), [w["E"], dmask], [w["DTm"]])
                k.op("dve", lambda e: e.tensor_scalar(out=w["rw"][:, :], in0=w["kt"][:, :], scalar1=sc[0:64, 5:6], scalar2=None, op0=ALU.mult), [w["kt"], sc], [w["rw"]])
                yield
                k.op("act", lambda e: e.activation(out=w["kt16"][:, :], in_=w["kt"][:, :], func=AF.Copy, scale=sc[0:64, 3:4]), [w["kt"], sc], [w["kt16"]])
                k.op("dve", lambda e: e.tensor_tensor(out=w["Bm"][:, :], in0=pb[0:64, 448:512], in1=w["DTs"][:, :], op=ALU.mult), [pb, w["DTs"]], [w["Bm"]])
                if is_main:
                    k.op("dve", lambda e: e.scalar_tensor_tensor(out=w["QKm16"][:, :], in0=pb[0:64, 320:384], scalar=sc_dk, in1=w["DTm"][:, :], op0=ALU.mult, op1=ALU.mult), [pb, w["DTm"]], [w["QKm16"]])
                yield
                k.op("dve", lambda e: e.tensor_tensor(out=w["Bm"][:, :], in0=pb[0:64, 256:320], in1=w["Bm"][:, :], op=ALU.mult), [pb, w["Bm"]], [w["Bm"]])
                yield
                k.tr(pb, [(pb[0:64, 0:64], w["Bm"][:, :])], [w["Bm"]], ident)
                tta = w["TTA0"]
                k.op("dve", lambda e: e.tensor_tensor(out=tta[:, 0:64], in0=ident[0:64, 0:64], in1=w["Bm"][:, :], op=ALU.subtract), [ident, w["Bm"]], [tta])
                yield
                k.op("act", lambda e: e.copy(out=w["Am"][:, :], in_=pb[0:64, 0:64]), [pb], [w["Am"]])
                yield
                k.op("pool", lambda e: e.tensor_tensor(out=tta[:, 64:128], in0=ident[0:64, 0:64], in1=w["Am"][:, :], op=ALU.subtract), [ident, w["Am"]], [tta])
                pq_p, pq_q = w["Am"][:, :], w["Bm"][:, :]
                pqb = [w["Am"], w["Bm"]]
                for lev in range(1, 6):
                    last = lev == 5
                    pqn = w["PQ%d" % (lev % 2)]
                    steps = [(pb[0:64, 64:128], pq_p, pq_q, True, True)]
                    if not last:
                        steps.append((pb[0:64, 0:64], pq_q, pq_p, True, True))
                    k.mm(pb, steps, pqb)
                    yield
                    if last:
                        k.op("act", lambda e: e.copy(out=pqn[:, 64:128], in_=pb[0:64, 64:128]), [pb], [pqn])
                    else:
                        k.op("act", lambda e: e.copy(out=pqn[:, :], in_=pb[0:64, 0:128]), [pb], [pqn])
                    yield
                    ttn = w["TTA%d" % (lev % 2)]
                    steps = [(pb[0:64, 128:192], tta[:, 64:128], pqn[:, 64:128], True, True)]
                    if not last:
                        steps.append((pb[0:64, 192:256], tta[:, 0:64], pqn[:, 0:64], True, True))
                    k.mm(pb, steps, [tta, pqn])
                    yield
                    if last:
                        k.op("dve", lambda e: e.tensor_tensor(out=ttn[:, 0:64], in0=pb[0:64, 128:192], in1=tta[:, 0:64], op=ALU.add), [pb, tta], [ttn])
                    else:
                        k.op("dve", lambda e: e.tensor_tensor(out=ttn[:, :], in0=pb[0:64, 128:256], in1=tta[:, :], op=ALU.add), [pb, tta], [ttn])
                    yield
                    tta = ttn
                    pq_p, pq_q = pqn[:, 0:64], pqn[:, 64:128]
                    pqb = [pqn]
                TT = tta[:, 0:64]
                k.mm(pb, [(pb[0:64, 256:384], TT, w["ru"][:, :], True, True), (pb[0:128, 384:448], w["rw"][:, :], TT, True, True)], [tta, w["ru"], w["rw"]])
                yield
                k.op("act", lambda e: e.copy(out=w["WT16"][:, :], in_=pb[0:128, 384:448]), [pb], [w["WT16"]])
                k.op("dve", lambda e: e.tensor_copy(out=w["U"][:, :], in_=pb[0:64, 256:384]), [pb], [w["U"]])
                yield
                steps = [(pb[0:64, 0:128], w["WT16"][:, :], w["S16"][:, :], True, True)]
                if is_main:
                    steps.append((pb[0:64, 128:256], QT16, w["S16"][:, :], True, True))
                k.mm(pb, steps, [w["WT16"], w["S16"], tb16])
                yield
                k.op("dve", lambda e: e.tensor_tensor(out=w["Vn16"][:, :], in0=w["U"][:, :], in1=pb[0:64, 0:128], op=ALU.subtract), [w["U"], pb], [w["Vn16"]])
                yield
                steps = [(pb[0:128, 256:384], w["kt16"][:, :], w["Vn16"][:, :], True, True)]
                if is_main:
                    steps.append((pb[0:64, 384:512], w["QKm16"][:, :], w["Vn16"][:, :], True, True))
                k.mm(pb, steps, [w["kt16"], w["Vn16"], w["QKm16"]])
                yield
                k.op("dve", lambda e: e.scalar_tensor_tensor(out=Sh[:, :], in0=Sh[:, :], scalar=sc[:, 4:5], in1=pb[0:128, 256:384], op0=ALU.mult, op1=ALU.add), [Sh, sc, pb], [Sh])
                k.op("act", lambda e: e.copy(out=w["S16"][:, :], in_=Sh[:, :]), [Sh], [w["S16"]])
                if is_main:
                    k.op("act", lambda e: e.copy(out=w["O2"][:, :], in_=pb[0:64, 384:512]), [pb], [w["O2"]])
                    yield
                    k.op("dve", lambda e: e.scalar_tensor_tensor(out=oc[:, h, :], in0=pb[0:64, 128:256], scalar=sc[0:64, 6:7], in1=w["O2"][:, :], op0=ALU.mult, op1=ALU.add), [pb, sc, w["O2"]], [oc])
                    if h == 3:
                        k.store(oc, OFB[d][tok0:tok0 + 64, :].rearrange("t (h v) -> t h v", h=4), oc[:, :, :], q="sp")
                yield

        import os
        IL = int(os.environ.get("DELTA_IL", "8"))
        allg = [chain(d, h) for h in range(4) for d in range(2)]
        if IL >= 8:
            groups = [allg]
        elif IL >= 4:
            groups = [[g_ for i_, g_ in enumerate(allg) if i_ % 2 == 0], [g_ for i_, g_ in enumerate(allg) if i_ % 2 == 1]]
        else:
            groups = None
        if groups is None:
            for g_ in allg:
                for _ in g_:
                    pass
        else:
            nround = 0
            for gens in groups:
                gens = list(gens)
                while gens:
                    nround += 1
                    if nround % 8 == 0 and hasattr(self, "wc_todo"):
                        self.wcast_tick()
                    for g_ in list(gens):
                        try:
                            next(g_)
                        except StopIteration:
                            gens.remove(g_)
        k.end()
        if os.environ.get("DELTA_NOCOMBINE"):
            return
        k.begin()
        og = k.sb("og", [128, 128], F32)
        k.load(og, og[:, :], self.I("onorm_g", [128, 128]))
        fo = [k.sb("fo%d" % i, [128, 4, 512], F32) for i in range(2)]
        bo = [k.sb("bo%d" % i, [128, 4, 512], F32) for i in range(2)]
        zz = [k.sb("zz%d" % i, [128, 4, 512], F32) for i in range(2)]
        dnT = [k.sb("dnT%d" % i, [128, 4, 512], BF16) for i in range(2)]
        ss = k.sb("ss", [128, 32], F32)
        junk = k.sb("junk", [128, 128], F32)
        for i in range(NT):
            f_, b_, z_, dn_ = fo[i % 2], bo[i % 2], zz[i % 2], dnT[i % 2]
            rows = slice(i * T, (i + 1) * T)
            k.load(f_, f_[:, :, :], OFB[0][rows, :].rearrange("(j p) d -> p j d", p=128), q="sp")
            k.load(b_, b_[:, :, :], OFB[1][rows, :].rearrange("(j p) d -> p j d", p=128), q="pool")
            k.load(z_, z_[:, :, :], Z[rows, :].rearrange("(j p) d -> p j d", p=128), q="sp")
            k.op("pool", lambda e, f_=f_, b_=b_: e.tensor_tensor(out=f_[:, :, :], in0=f_[:, :, :], in1=b_[:, :, :], op=ALU.add), [f_, b_], [f_])
            for j in range(4):
                for h in range(4):
                    k.op("act", lambda e, f_=f_, j=j, h=h: e.activation(out=junk[:, :], in_=f_[:, j, h * 128:(h + 1) * 128], func=AF.Square,
                                                                       accum_out=ss[:, j * 4 + h:j * 4 + h + 1]), [f_], [junk, ss])
            k.op("act", lambda e: e.activation(out=ss[:, 16:32], in_=ss[:, 0:16], func=AF.Sqrt, bias=self.eps_c[:, 0:1], scale=1.0 / 128), [ss, self.eps_c], [ss])
            k.op("dve", lambda e: e.reciprocal(out=ss[:, 16:32], in_=ss[:, 16:32]), [ss], [ss])
            for j in range(4):
                for h in range(4):
                    k.op("dve", lambda e, f_=f_, j=j, h=h: e.scalar_tensor_tensor(out=f_[:, j, h * 128:(h + 1) * 128], in0=f_[:, j, h * 128:(h + 1) * 128],
                                                                                 scalar=ss[:, 16 + j * 4 + h:17 + j * 4 + h], in1=og[:, :], op0=ALU.mult, op1=ALU.mult), [f_, ss, og], [f_])
            k.op("pool", lambda e, f_=f_, z_=z_: e.tensor_tensor(out=f_[:, :, :], in0=f_[:, :, :], in1=z_[:, :, :], op=ALU.mult), [f_, z_], [f_])
            for j in range(4):
                bt = k.bank()
                k.tr(bt, [(bt[:, h * 128:(h + 1) * 128], f_[:, j, h * 128:(h + 1) * 128]) for h in range(4)], [f_], ident)
                k.cp("act" if j % 2 else "dve", dn_, dn_[:, :, j * 128:(j + 1) * 128], bt, bt[:, :].rearrange("p (h t) -> p h t", h=4))
            k.store(dn_, DN[:, rows].rearrange("(h p) t -> p h t", p=128), dn_[:, :, :])
        k.end()

    def stage_fnet(self):
        k = self.k
        k.begin()
        GC = self.S("GC", [SEQ, 512], BF16)
        GS = self.S("GS", [SEQ, 512], BF16)
        FN = self.S("FN_T", [512, SEQ], BF16)
        dc = self.I("dft_c", [SEQ, SEQ], BF16)
        dsn = self.I("dft_s", [SEQ, SEQ], BF16)
        gc = k.sb("gc", [128, 32, 512], BF16)
        gs = k.sb("gs", [128, 32, 512], BF16)
        for q4 in range(4):
            k.load(gc, gc[:, q4 * 8:(q4 + 1) * 8, :], GC[q4 * 1024:(q4 + 1) * 1024, :].rearrange("(c p) n -> p c n", p=128), q="sp")
            k.load(gs, gs[:, q4 * 8:(q4 + 1) * 8, :], GS[q4 * 1024:(q4 + 1) * 1024, :].rearrange("(c p) n -> p c n", p=128), q="pool")
        pan = [k.sb("pan%d" % i, [128, 32, 512], BF16) for i in range(3)]
        fo = [k.sb("fo%d" % i, [128, 4, 512], BF16) for i in range(2)]
        scale = float(1.0 / np.sqrt(SEQ * 128.0))
        npan = 0
        for ft in range(8):
            ps_ = []
            for (tab, q_) in ((dc, "sp"), (dsn, "pool")):
                p_ = pan[npan % 3]
                npan += 1
                for hlf in range(2):
                    k.load(p_, p_[:, hlf * 16:(hlf + 1) * 16, :], tab[hlf * 2048:(hlf + 1) * 2048, ft * 512:(ft + 1) * 512].rearrange("(c p) n -> p c n", p=128), q=q_)
                ps_.append(p_)
            c_, s_ = ps_
            f = fo[ft % 2]
            for g in range(4):
                bank = k.bank()
                steps = []
                for tc in range(32):
                    steps.append((bank[:, :], gc[:, tc, g * 128:(g + 1) * 128], c_[:, tc, :], tc == 0, False))
                for tc in range(32):
                    steps.append((bank[:, :], gs[:, tc, g * 128:(g + 1) * 128], s_[:, tc, :], False, tc == 31))
                k.mm(bank, steps, [gc, gs, c_, s_])
                k.op("act", lambda e, f=f, g=g, bank=bank: e.activation(out=f[:, g, :], in_=bank[:, :], func=AF.Copy, scale=scale), [bank], [f])
            k.store(f, FN[:, ft * 512:(ft + 1) * 512].rearrange("(g p) n -> p g n", p=128), f[:, :, :])
        k.end()

    def stage_mix(self):
        k = self.k
        k.begin()
        XT = self.S("XT", [D, SEQ])
        DN = self.S("DN_T", [512, SEQ], BF16)
        FN = self.S("FN_T", [512, SEQ], BF16)
        stg = [k.sb("stg%d" % i, [128, 8, 256], F32) for i in range(2)]
        wo = k.sb("wo", [128, 8, D], BF16)
        self.load_w16(wo, self.I("w_out", [D, D]), 8, D, stg)
        mx = [k.sb("mx%d" % i, [128, 8, T], BF16) for i in range(2)]
        xt = [k.sb("xt%d" % i, [128, 8, T], F32) for i in range(2)]
        cf = self.coef
        for i in range(NT):
            m, x_ = mx[i % 2], xt[i % 2]
            k.load(m, m[:, 0:4, :], DN[:, i * T:(i + 1) * T].rearrange("(c p) t -> p c t", p=128), q="sp")
            k.load(m, m[:, 4:8, :], FN[:, i * T:(i + 1) * T].rearrange("(c p) t -> p c t", p=128), q="sp")
            k.load(x_, x_[:, :, :], XT[:, i * T:(i + 1) * T].rearrange("(c p) t -> p c t", p=128), q="pool")
            for oc in range(8):
                bank = k.bank()
                k.mm(bank, [(bank[:, :], wo[:, kc, oc * 128:(oc + 1) * 128], m[:, kc, :], kc == 0, kc == 7) for kc in range(8)], [wo, m])
                k.op("dve", lambda e, x_=x_, oc=oc, bank=bank: e.scalar_tensor_tensor(out=x_[:, oc, :], in0=bank[:, :], scalar=cf[:, 0, 2, oc:oc + 1],
                                                                                     in1=x_[:, oc, :], op0=ALU.mult, op1=ALU.add), [bank, cf, x_], [x_])
            k.store(x_, XT[:, i * T:(i + 1) * T].rearrange("(c p) t -> p c t", p=128), x_[:, :, :])
        k.end()

    def stage_moe(self, l):
        k = self.k
        k.begin()
        XT = self.S("XT", [D, SEQ])
        wg_d = self.I("moe_wg", [2, NEXP, D, FF])
        wu_d = self.I("moe_wu", [2, NEXP, D, FF])
        wd_d = self.I("moe_wd", [2, NEXP, FF, D])
        cf = self.coef
        ST = 2048
        wr = k.sb("wr", [128, 8, 32], F32)
        k.load(wr, wr[:, :, :], self.I("router_w", [D, 32]).rearrange("(k p) n -> p k n", p=128))
        rb = k.sb("rb", [128, 128], F32)
        k.load(rb, rb[:, :], self.I("rbias", [128, 128]))
        selb = [k.sb("selm%d" % i, [32, 128], F32) for i in range(2)]
        tT = k.sb("tT", [128, 8, ST], BF16)
        yacc = k.sb("yacc", [128, 8, ST], F32)
        combT = k.sb("combT", [32, ST], F32)
        wgb = [k.sb("wg%d" % i, [128, 8, FF], BF16) for i in range(2)]
        wub = [k.sb("wu%d" % i, [128, 8, FF], BF16) for i in range(2)]
        wdb = [k.sb("wd%d" % i, [128, 4, D], BF16) for i in range(2)]
        cb = [k.sb("cb%d" % i, [128, T], F32) for i in range(2)]
        sgb = [k.sb("sg%d" % i, [128, T], F32) for i in range(2)]
        t1b = [k.sb("t1%d" % i, [128, T], F32) for i in range(2)]
        hid = [k.sb("hid%d" % i, [128, 4, T], BF16) for i in range(2)]
        rstd = k.sb("rstd", [128, T], F32)
        R = {}
        for nm, shp in (("sc", [128, 128]), ("sel", [128, 128]), ("sel2", [128, 128]), ("eq", [128, 32]), ("m1", [128, 32]), ("m2", [128, 32]),
                        ("gs", [128, 32]), ("gmax", [128, 4]), ("ghot", [128, 32]), ("mask", [128, 128]), ("den", [128, 4]), ("comb", [128, 128])):
            R[nm] = k.sb("r_" + nm, shp, F32)
        xt = Buf(yacc.t, "yv")
        for st in range(SEQ // ST):
            for tt in range(4):
                tok = slice(st * ST + tt * T, st * ST + (tt + 1) * T)
                xv = yacc[:, :, 0:T]
                t32 = yacc[:, :, T:2 * T]
                sq = yacc[:, :, 2 * T:3 * T]
                k.load(yacc, xv, XT[:, tok].rearrange("(c p) t -> p c t", p=128))
                k.op("act", lambda e, sq=sq, xv=xv: e.activation(out=sq, in_=xv, func=AF.Square), [yacc], [yacc])
                bank = k.bank()
                k.mm(bank, [(bank[:, :], self.ones[:, :], yacc[:, c, 2 * T:3 * T], c == 0, c == 7) for c in range(8)], [self.ones, yacc])
                k.op("act", lambda e, bank=bank: e.activation(out=rstd[:, :], in_=bank[:, :], func=AF.Sqrt, bias=self.eps_c[:, 0:1], scale=1.0 / D), [bank, self.eps_c], [rstd])
                k.op("dve", lambda e: e.reciprocal(out=rstd[:, :], in_=rstd[:, :]), [rstd], [rstd])
                for c in range(8):
                    k.op("dve", lambda e, c=c: e.scalar_tensor_tensor(out=yacc[:, c, 2 * T:3 * T], in0=yacc[:, c, 0:T], scalar=cf[:, l, 3, c:c + 1],
                                                                     in1=rstd[:, :], op0=ALU.mult, op1=ALU.mult), [yacc, rstd, cf], [yacc])
                for c in range(8):
                    k.op("act", lambda e, c=c: e.activation(out=yacc[:, c, T:2 * T], in_=yacc[:, c, 2 * T:3 * T], func=AF.Identity,
                                                           bias=cf[:, l, 4, c:c + 1], scale=1.0), [yacc, cf], [yacc])
                k.op("pool", lambda e, tt=tt, t32=t32: e.tensor_copy(out=tT[:, :, tt * T:(tt + 1) * T], in_=t32), [yacc], [tT])
                bank = k.bank()
                steps = []
                for j in range(4):
                    for kc in range(8):
                        steps.append((bank[:, j * 32:(j + 1) * 32], yacc[:, kc, T + j * 128:T + (j + 1) * 128], wr[:, kc, :], kc == 0, kc == 7))
                k.mm(bank, steps, [yacc, wr])
                sc, sel, sel2, eq, m1, m2, gsm, gmax, ghot, mask, den, comb = (R[n] for n in ("sc", "sel", "sel2", "eq", "m1", "m2", "gs", "gmax", "ghot", "mask", "den", "comb"))
                k.op("act", lambda e, bank=bank: e.activation(out=sc[:, :], in_=bank[:, 0:128], func=AF.Sigmoid), [bank], [sc])
                k.op("dve", lambda e: e.tensor_tensor(out=sel[:, :], in0=sc[:, :], in1=rb[:, :], op=ALU.add), [sc, rb], [sel])
                v4 = lambda b_: b_[:, :].rearrange("p (a i) -> p a i", i=4)
                k.op("dve", lambda e: e.tensor_reduce(out=m1[:, :], in_=v4(sel), axis=AX.X, op=ALU.max), [sel], [m1])
                for i4 in range(4):
                    k.op("dve", lambda e, i4=i4: e.tensor_tensor(out=eq[:, :], in0=v4(sel)[:, :, i4], in1=m1[:, :], op=ALU.is_equal), [sel, m1], [eq])
                    k.op("dve", lambda e, i4=i4: e.scalar_tensor_tensor(out=v4(sel2)[:, :, i4], in0=eq[:, :], scalar=-1.0e9, in1=v4(sel)[:, :, i4],
                                                                       op0=ALU.mult, op1=ALU.add), [eq, sel], [sel2])
                k.op("dve", lambda e: e.tensor_reduce(out=m2[:, :], in_=v4(sel2), axis=AX.X, op=ALU.max), [sel2], [m2])
                k.op("dve", lambda e: e.tensor_tensor(out=gsm[:, :], in0=m1[:, :], in1=m2[:, :], op=ALU.add), [m1, m2], [gsm])
                v8 = lambda b_: b_[:, :].rearrange("p (j g) -> p j g", g=8)
                k.op("dve", lambda e: e.tensor_reduce(out=gmax[:, :], in_=v8(gsm), axis=AX.X, op=ALU.max), [gsm], [gmax])
                for g8 in range(8):
                    k.op("dve", lambda e, g8=g8: e.tensor_tensor(out=v8(ghot)[:, :, g8], in0=v8(gsm)[:, :, g8], in1=gmax[:, :], op=ALU.is_equal), [gsm, gmax], [ghot])
                for i4 in range(4):
                    k.op("dve", lambda e, i4=i4: e.tensor_tensor(out=eq[:, :], in0=v4(sel)[:, :, i4], in1=m2[:, :], op=ALU.is_ge), [sel, m2], [eq])
                    k.op("dve", lambda e, i4=i4: e.tensor_tensor(out=v4(mask)[:, :, i4], in0=eq[:, :], in1=ghot[:, :], op=ALU.mult), [eq, ghot], [mask])
                k.op("dve", lambda e: e.tensor_tensor(out=mask[:, :], in0=mask[:, :], in1=sc[:, :], op=ALU.mult), [mask, sc], [mask])
                v32 = lambda b_: b_[:, :].rearrange("p (j e) -> p j e", e=32)
                k.op("dve", lambda e: e.tensor_reduce(out=den[:, :], in_=v32(mask), axis=AX.X, op=ALU.add), [mask], [den])
                k.op("dve", lambda e: e.reciprocal(out=den[:, :], in_=den[:, :]), [den], [den])
                for j in range(4):
                    k.op("dve", lambda e, j=j: e.tensor_scalar(out=comb[:, j * 32:(j + 1) * 32], in0=mask[:, j * 32:(j + 1) * 32], scalar1=den[:, j:j + 1],
                                                              scalar2=None, op0=ALU.mult), [mask, den], [comb])
                bank = k.bank()
                k.tr(bank, [(bank[0:32, j * 128:(j + 1) * 128], comb[:, j * 32:(j + 1) * 32]) for j in range(4)], [comb], self.ident)
                k.op("act", lambda e, bank=bank, tt=tt: e.copy(out=combT[:, tt * T:(tt + 1) * T], in_=bank[0:32, :]), [bank], [combT])
            if "combT" in self.dbgset:
                k.store(combT, self.S("combT", [2, 32, ST])[st], combT[:, :])
            for e_ in range(NEXP):
                wg, wu, wd = wgb[e_ % 2], wub[e_ % 2], wdb[e_ % 2]
                k.load(wg, wg[:, :, :], wg_d[l, e_].rearrange("(k p) n -> p k n", p=128), q="pool")
                k.load(wu, wu[:, :, :], wu_d[l, e_].rearrange("(k p) n -> p k n", p=128), q="pool")
                k.load(wd, wd[:, :, :], wd_d[l, e_].rearrange("(k p) n -> p k n", p=128), q="pool")
                selm = selb[e_ % 2]
                k.op("pool", lambda e, selm=selm, e_=e_: e.tensor_scalar(out=selm[:, :], in0=self.ones[0:32, :], scalar1=self.ident[0:32, e_:e_ + 1],
                                                                        scalar2=None, op0=ALU.mult), [self.ones, self.ident], [selm])
                for tt in range(4):
                    n = e_ * 4 + tt
                    c_ = cb[n % 2]
                    bank = k.bank()
                    k.mm(bank, [(bank[:, :], selm[:, :], combT[:, tt * T:(tt + 1) * T], True, True)], [selm, combT])
                    k.op("act", lambda e, c_=c_, bank=bank: e.copy(out=c_[:, :], in_=bank[:, :]), [bank], [c_])
                    h_ = hid[n % 2]
                    for fc in range(4):
                        m = n * 4 + fc
                        bg = k.bank()
                        k.mm(bg, [(bg[:, :], wg[:, kc, fc * 128:(fc + 1) * 128], tT[:, kc, tt * T:(tt + 1) * T], kc == 0, kc == 7) for kc in range(8)], [wg, tT])
                        bu = k.bank()
                        k.mm(bu, [(bu[:, :], wu[:, kc, fc * 128:(fc + 1) * 128], tT[:, kc, tt * T:(tt + 1) * T], kc == 0, kc == 7) for kc in range(8)], [wu, tT])
                        sg, t1 = sgb[m % 2], t1b[m % 2]
                        k.op("act", lambda e, sg=sg, bg=bg: e.activation(out=sg[:, :], in_=bg[:, :], func=AF.Silu), [bg], [sg])
                        k.op("dve", lambda e, t1=t1, bu=bu, c_=c_: e.tensor_tensor(out=t1[:, :], in0=bu[:, :], in1=c_[:, :], op=ALU.mult), [bu, c_], [t1])
                        k.op("pool", lambda e, h_=h_, fc=fc, sg=sg, t1=t1: e.tensor_tensor(out=h_[:, fc, :], in0=sg[:, :], in1=t1[:, :], op=ALU.mult), [sg, t1], [h_])
                    for oc in range(8):
                        bd = k.bank()
                        k.mm(bd, [(bd[:, :], wd[:, fc, oc * 128:(oc + 1) * 128], h_[:, fc, :], fc == 0, fc == 3) for fc in range(4)], [wd, h_])
                        if e_ == 0:
                            k.op("dve", lambda e, oc=oc, tt=tt, bd=bd: e.tensor_copy(out=yacc[:, oc, tt * T:(tt + 1) * T], in_=bd[:, :]), [bd], [yacc])
                        else:
                            k.op("dve", lambda e, oc=oc, tt=tt, bd=bd: e.tensor_tensor(out=yacc[:, oc, tt * T:(tt + 1) * T], in0=bd[:, :],
                                                                                      in1=yacc[:, oc, tt * T:(tt + 1) * T], op=ALU.add), [bd, yacc], [yacc])
            xr = [k.sb("xr%d_%d" % (st, i), [128, 8, T], F32) for i in range(1)] if st == 0 else xr
            for tt in range(4):
                tok = slice(st * ST + tt * T, st * ST + (tt + 1) * T)
                x_ = xr[0]
                k.load(x_, x_[:, :, :], XT[:, tok].rearrange("(c p) t -> p c t", p=128))
                for oc in range(8):
                    k.op("dve", lambda e, x_=x_, oc=oc, tt=tt: e.scalar_tensor_tensor(out=x_[:, oc, :], in0=yacc[:, oc, tt * T:(tt + 1) * T], scalar=cf[:, l, 5, oc:oc + 1],
                                                                                     in1=x_[:, oc, :], op0=ALU.mult, op1=ALU.add), [yacc, cf, x_], [x_])
                k.store(x_, XT[:, tok].rearrange("(c p) t -> p c t", p=128), x_[:, :, :])
            k.barrier()
        k.end()

    def stage_moe_r(self, l):
        k = self.k
        I32 = mybir.dt.int32
        NS = 8192
        XT = self.S("XT", [D, SEQ])
        TTOK = self.S("T_tok", [SEQ, D])
        TS = self.S("TS", [NS, D])
        YS = self.S("YS", [NS, D])
        W4S = self.S("W4S", [NS, 128])
        wgl, wul, wdl = self.w16
        cf = self.coef
        k.begin()
        wr = k.sb("wr", [128, 8, 32], F32)
        k.load(wr, wr[:, :, :], self.I("router_w", [D, 32]).rearrange("(k p) n -> p k n", p=128))
        rb = k.sb("rb", [128, 128], F32)
        k.load(rb, rb[:, :], self.I("rbias", [128, 128]))
        mc = k.sb("mc", [128, 129], F32)
        k.load(mc, mc[:, :], self.I("mconst", [128, 129]))
        H = k.sb("H", [128, 32, 8], F32)
        W4 = k.sb("W4", [128, 32, 4], F32)
        RK = k.sb("RK", [128, 32, 8], F32)
        TOT = k.sb("TOT", [128, 32, 8], F32)
        PRE = k.sb("PRE", [128, 32, 8], F32)
        sm = k.sb("sm", [128, 64], F32)
        SLOTF = k.sb("SLOTF", [128, 32], F32)
        SLOTI = self.SLOTI_p
        GID = k.sb("GID", [128, 16], F32)
        IDXF = k.sb("IDXF", [128, 16, 4], F32)
        IDXI = self.IDXI_p
        xv = k.sb("xv", [128, 8, T], F32)
        t32 = k.sb("t32", [128, 8, T], F32)
        sq = k.sb("sq", [128, 8, T], F32)
        rstd = k.sb("rstd", [128, T], F32)
        ttok = [k.sb("ttok%d" % i, [128, 4, D], F32) for i in range(2)]
        R = {}
        for nm, shp in (("sc", [128, 128]), ("sel", [128, 128]), ("sel2", [128, 128]), ("eq", [128, 32]), ("m1", [128, 32]), ("m2", [128, 32]),
                        ("gs", [128, 32]), ("gmax", [128, 4]), ("ghot", [128, 32]), ("mask", [128, 128]), ("den", [128, 4]), ("comb", [128, 128])):
            R[nm] = k.sb("r_" + nm, shp, F32)
        sc, sel, sel2, eq, m1, m2, gsm, gmax, ghot, mask, den, comb = (R[n] for n in ("sc", "sel", "sel2", "eq", "m1", "m2", "gs", "gmax", "ghot", "mask", "den", "comb"))
        v4 = lambda b_: b_[:, :].rearrange("p (a i) -> p a i", i=4)
        v8 = lambda b_: b_[:, :].rearrange("p (j g) -> p j g", g=8)
        v32 = lambda b_: b_[:, :].rearrange("p (j e) -> p j e", e=32)
        for tt in range(NT):
            tok = slice(tt * T, (tt + 1) * T)
            k.load(xv, xv[:, :, :], XT[:, tok].rearrange("(c p) t -> p c t", p=128))
            self.rstd_fm(xv, 8, T, sq, rstd, 1.0 / D)
            self.modulate(xv, rstd, cf[:, l, 3, :], cf, cf[:, l, 4, :], cf, sq, t32, T)
            bank = k.bank()
            steps = []
            for j in range(4):
                for kc in range(8):
                    steps.append((bank[:, j * 32:(j + 1) * 32], t32[:, kc, j * 128:(j + 1) * 128], wr[:, kc, :], kc == 0, kc == 7))
            k.mm(bank, steps, [t32, wr])
            k.op("act", lambda e, bank=bank: e.activation(out=sc[:, :], in_=bank[:, 0:128], func=AF.Sigmoid), [bank], [sc])
            k.op("dve", lambda e: e.tensor_tensor(out=sel[:, :], in0=sc[:, :], in1=rb[:, :], op=ALU.add), [sc, rb], [sel])
            k.op("dve", lambda e: e.tensor_reduce(out=m1[:, :], in_=v4(sel), axis=AX.X, op=ALU.max), [sel], [m1])
            for i4 in range(4):
                k.op("dve", lambda e, i4=i4: e.tensor_tensor(out=eq[:, :], in0=v4(sel)[:, :, i4], in1=m1[:, :], op=ALU.is_equal), [sel, m1], [eq])
                k.op("dve", lambda e, i4=i4: e.scalar_tensor_tensor(out=v4(sel2)[:, :, i4], in0=eq[:, :], scalar=-1.0e9, in1=v4(sel)[:, :, i4],
                                                                   op0=ALU.mult, op1=ALU.add), [eq, sel], [sel2])
            k.op("dve", lambda e: e.tensor_reduce(out=m2[:, :], in_=v4(sel2), axis=AX.X, op=ALU.max), [sel2], [m2])
            k.op("dve", lambda e: e.tensor_tensor(out=gsm[:, :], in0=m1[:, :], in1=m2[:, :], op=ALU.add), [m1, m2], [gsm])
            k.op("dve", lambda e: e.tensor_reduce(out=gmax[:, :], in_=v8(gsm), axis=AX.X, op=ALU.max), [gsm], [gmax])
            for g8 in range(8):
                k.op("dve", lambda e, g8=g8: e.tensor_tensor(out=v8(ghot)[:, :, g8], in0=v8(gsm)[:, :, g8], in1=gmax[:, :], op=ALU.is_equal), [gsm, gmax], [ghot])
            for i4 in range(4):
                k.op("dve", lambda e, i4=i4: e.tensor_tensor(out=eq[:, :], in0=v4(sel)[:, :, i4], in1=m2[:, :], op=ALU.is_ge), [sel, m2], [eq])
                k.op("dve", lambda e, i4=i4: e.tensor_tensor(out=v4(mask)[:, :, i4], in0=eq[:, :], in1=ghot[:, :], op=ALU.mult), [eq, ghot], [mask])
            k.op("dve", lambda e: e.tensor_tensor(out=mask[:, :], in0=mask[:, :], in1=sc[:, :], op=ALU.mult), [mask, sc], [mask])
            k.op("dve", lambda e: e.tensor_reduce(out=den[:, :], in_=v32(mask), axis=AX.X, op=ALU.add), [mask], [den])
            k.op("dve", lambda e: e.reciprocal(out=den[:, :], in_=den[:, :]), [den], [den])
            for j in range(4):
                k.op("dve", lambda e, j=j: e.tensor_scalar(out=comb[:, j * 32:(j + 1) * 32], in0=mask[:, j * 32:(j + 1) * 32], scalar1=den[:, j:j + 1],
                                                          scalar2=None, op0=ALU.mult), [mask, den], [comb])
            k.op("dve", lambda e, tt=tt: e.tensor_copy(out=H[:, tt * 4:(tt + 1) * 4, :], in_=v8(ghot)), [ghot], [H])
            for j in range(4):
                k.op("dve", lambda e, tt=tt, j=j: e.tensor_reduce(out=W4[:, tt * 4 + j, :], in_=comb[:, j * 32:(j + 1) * 32].rearrange("p (g i) -> p i g", i=4),
                                                                 axis=AX.X, op=ALU.add), [comb], [W4])
            tk = ttok[tt % 2]
            for j in range(4):
                for hf in range(2):
                    bank = k.bank()
                    k.tr(bank, [(bank[:, cc * 128:(cc + 1) * 128], t32[:, hf * 4 + cc, j * 128:(j + 1) * 128]) for cc in range(4)], [t32], self.ident)
                    k.cp("act" if hf else "dve", tk, tk[:, j, hf * 512:(hf + 1) * 512], bank, bank[:, :])
            k.store(tk, TTOK[tok, :].rearrange("(j p) d -> p j d", p=128), tk[:, :, :])
        Hf = H[:, :, :].rearrange("p b g -> p (b g)")
        bank = k.bank()
        k.mm(bank, [(bank[:, 0:256], mc[:, 0:128], Hf, True, True)], [mc, H])
        k.op("dve", lambda e, bank=bank: e.tensor_copy(out=RK[:, :, :].rearrange("p b g -> p (b g)"), in_=bank[:, 0:256]), [bank], [RK])
        bank = k.bank()
        k.mm(bank, [(bank[:, 0:256], self.ones[:, :], Hf, True, True)], [self.ones, H])
        k.op("dve", lambda e, bank=bank: e.tensor_copy(out=TOT[:, :, :].rearrange("p b g -> p (b g)"), in_=bank[:, 0:256]), [bank], [TOT])
        k.op("dve", lambda e: e.memset(PRE[:, 0, :], 0.0), [], [PRE])
        for b in range(1, 32):
            k.op("dve", lambda e, b=b: e.tensor_tensor(out=PRE[:, b, :], in0=PRE[:, b - 1, :], in1=TOT[:, b - 1, :], op=ALU.add), [PRE, TOT], [PRE])
        k.op("dve", lambda e: e.tensor_tensor(out=sm[:, 0:8], in0=PRE[:, 31, :], in1=TOT[:, 31, :], op=ALU.add), [PRE, TOT], [sm])
        k.op("dve", lambda e: e.memset(sm[:, 8:16], 0.0), [], [sm])
        for m in range(8):
            k.op("dve", lambda e, m=m: e.tensor_scalar(out=sm[:, 32:40], in0=sm[:, 0:8], scalar1=float(512 * m), scalar2=None, op0=ALU.is_gt), [sm], [sm])
            k.op("dve", lambda e: e.tensor_tensor(out=sm[:, 8:16], in0=sm[:, 8:16], in1=sm[:, 32:40], op=ALU.add), [sm], [sm])
        k.op("dve", lambda e: e.memset(sm[:, 16:17], 0.0), [], [sm])
        k.op("dve", lambda e: e.tensor_copy(out=sm[:, 24:25], in_=sm[:, 8:9]), [sm], [sm])
        for g8 in range(1, 8):
            k.op("dve", lambda e, g8=g8: e.scalar_tensor_tensor(out=sm[:, 16 + g8:17 + g8], in0=sm[:, 8 + g8 - 1:9 + g8 - 1], scalar=512.0, in1=sm[:, 16 + g8 - 1:17 + g8 - 1],
                                                               op0=ALU.mult, op1=ALU.add), [sm], [sm])
            k.op("dve", lambda e, g8=g8: e.tensor_tensor(out=sm[:, 24 + g8:25 + g8], in0=sm[:, 24 + g8 - 1:25 + g8 - 1], in1=sm[:, 8 + g8:9 + g8], op=ALU.add), [sm], [sm])
        k.op("dve", lambda e: e.tensor_tensor(out=RK[:, :, :], in0=RK[:, :, :], in1=PRE[:, :, :], op=ALU.add), [RK, PRE], [RK])
        for b in range(32):
            k.op("dve", lambda e, b=b: e.tensor_tensor(out=RK[:, b, :], in0=RK[:, b, :], in1=sm[:, 16:24], op=ALU.add), [RK, sm], [RK])
        k.op("dve", lambda e: e.tensor_tensor(out=RK[:, :, :], in0=RK[:, :, :], in1=H[:, :, :], op=ALU.mult), [RK, H], [RK])
        k.op("dve", lambda e: e.tensor_reduce(out=SLOTF[:, :], in_=RK[:, :, :], axis=AX.X, op=ALU.add), [RK], [SLOTF])
        k.op("dve", lambda e: e.tensor_copy(out=SLOTI[:, :], in_=SLOTF[:, :]), [SLOTF], [SLOTI])
        for i in range(16):
            k.op("dve", lambda e, i=i: e.tensor_scalar(out=sm[:, 32:40], in0=sm[:, 24:32], scalar1=float(i), scalar2=None, op0=ALU.is_le), [sm], [sm])
            k.op("dve", lambda e, i=i: e.tensor_reduce(out=GID[:, i:i + 1], in_=sm[:, 32:40], axis=AX.X, op=ALU.add), [sm], [GID])
        k.op("dve", lambda e: e.tensor_scalar(out=GID[:, :], in0=GID[:, :], scalar1=7.0, scalar2=None, op0=ALU.min), [GID], [GID])
        for j in range(4):
            k.op("dve", lambda e, j=j: e.tensor_scalar(out=IDXF[:, :, j], in0=GID[:, :], scalar1=512.0, scalar2=mc[:, 128:129], op0=ALU.mult, op1=ALU.add), [GID, mc], [IDXF])
            k.op("dve", lambda e, j=j: e.tensor_scalar(out=IDXF[:, :, j], in0=IDXF[:, :, j], scalar1=float((l * NEXP + j) * 128), scalar2=None, op0=ALU.add), [IDXF], [IDXF])
        k.op("dve", lambda e: e.tensor_copy(out=IDXI[:, :], in_=IDXF[:, :, :].rearrange("p i j -> p (i j)")), [IDXF], [IDXI])
        if "slots" in self.dbgset:
            k.store(SLOTF, self.S("slots", [128, 32]), SLOTF[:, :])
            k.store(GID, self.S("gid", [128, 16]), GID[:, :])
        k.barrier()
        tb = [k.sb("tb%d" % i, [128, D], F32) for i in range(3)]
        w4b = [k.sb("w4b%d" % i, [128, 128], F32) for i in range(2)]
        for b_ in w4b:
            k.op("pool", lambda e, b_=b_: e.memset(b_[:, :], 0.0), [], [b_])
        for b in range(32):
            t_ = tb[b % 3]
            k.load(t_, t_[:, :], TTOK[b * 128:(b + 1) * 128, :])
            k.idma(TS[:, :], t_[:, :], SLOTI, SLOTI[:, b:b + 1], t_, False, NS)
            w_ = w4b[b % 2]
            k.op("dve", lambda e, w_=w_, b=b: e.tensor_copy(out=w_[:, 0:4], in_=W4[:, b, :]), [W4], [w_])
            k.idma(W4S[:, :], w_[:, :], SLOTI, SLOTI[:, b:b + 1], w_, False, NS)
        k.end()
        k.begin()
        wg16 = [k.sb("wg%d" % i, [128, 8, FF], BF16) for i in range(2)]
        wu16 = [k.sb("wu%d" % i, [128, 8, FF], BF16) for i in range(2)]
        wd16 = [k.sb("wd%d" % i, [128, 4, D], BF16) for i in range(2)]
        self.wcast_finish()
        tsr = [k.sb("tsr%d" % i, [128, 4, D], F32) for i in range(2)]
        tsT = [k.sb("tsT%d" % i, [128, 8, T], BF16) for i in range(2)]
        w4s = [k.sb("w4s%d" % i, [128, 4, 128], F32) for i in range(2)]
        yac = [k.sb("yac%d" % i, [128, 4, D], F32) for i in range(2)]
        sgb = [k.sb("sg%d" % i, [128, T], F32) for i in range(2)]
        hid = [k.sb("hid%d" % i, [128, 4, T], BF16) for i in range(2)]
        nst = 0
        ncast = 0
        for i in range(15):
            tr_, tT_, w4_, ya = tsr[i % 2], tsT[i % 2], w4s[i % 2], yac[i % 2]
            k.load(tr_, tr_[:, :, :], TS[i * T:(i + 1) * T, :].rearrange("(j p) d -> p j d", p=128))
            k.load(w4_, w4_[:, :, :], W4S[i * T:(i + 1) * T, :].rearrange("(j p) d -> p j d", p=128))
            for c in range(8):
                bank = k.bank()
                k.tr(bank, [(bank[:, j * 128:(j + 1) * 128], tr_[:, j, c * 128:(c + 1) * 128]) for j in range(4)], [tr_], self.ident)
                k.cp("act" if c % 2 else "dve", tT_, tT_[:, c, :], bank, bank[:, :])
            for j in range(4):
                n = i * 4 + j
                wg, wu, wd = wg16[n % 2], wu16[n % 2], wd16[n % 2]
                for (dst, srcw) in ((wg, wgl), (wu, wul), (wd, wdl)):
                    k.idma(dst[:, :, :].rearrange("p a b -> p (a b)"), srcw[:, :], self.IDXI_p, self.IDXI_p[:, n:n + 1], dst, True, 2 * NEXP * 128)
                h_ = hid[n % 2]
                for fc in range(4):
                    m = n * 4 + fc
                    bg = k.bank()
                    k.mm(bg, [(bg[:, :], wg[:, kc, fc * 128:(fc + 1) * 128], tT_[:, kc, :], kc == 0, kc == 7) for kc in range(8)], [wg, tT_])
                    bu = k.bank()
                    k.mm(bu, [(bu[:, :], wu[:, kc, fc * 128:(fc + 1) * 128], tT_[:, kc, :], kc == 0, kc == 7) for kc in range(8)], [wu, tT_])
                    sg = sgb[m % 2]
                    k.op("act", lambda e, sg=sg, bg=bg: e.activation(out=sg[:, :], in_=bg[:, :], func=AF.Silu), [bg], [sg])
                    k.op("dve", lambda e, h_=h_, fc=fc, sg=sg, bu=bu: e.tensor_tensor(out=h_[:, fc, :], in0=bu[:, :], in1=sg[:, :], op=ALU.mult), [bu, sg], [h_])
                for blk in range(4):
                    for hf in range(2):
                        bd = k.bank()
                        k.mm(bd, [(bd[:, :], h_[:, fc, blk * 128:(blk + 1) * 128], wd[:, fc, hf * 512:(hf + 1) * 512], fc == 0, fc == 3) for fc in range(4)], [wd, h_])
                        if j == 0:
                            k.op("dve", lambda e, ya=ya, blk=blk, hf=hf, bd=bd, w4_=w4_: e.tensor_scalar(out=ya[:, blk, hf * 512:(hf + 1) * 512], in0=bd[:, :],
                                                                                                     scalar1=w4_[:, blk, 0:1], scalar2=None, op0=ALU.mult), [bd, w4_], [ya])
                        else:
                            k.op("dve", lambda e, ya=ya, blk=blk, hf=hf, bd=bd, w4_=w4_, j=j: e.scalar_tensor_tensor(
                                out=ya[:, blk, hf * 512:(hf + 1) * 512], in0=bd[:, :], scalar=w4_[:, blk, j:j + 1], in1=ya[:, blk, hf * 512:(hf + 1) * 512],
                                op0=ALU.mult, op1=ALU.add), [bd, w4_, ya], [ya])
            k.store(ya, YS[i * T:(i + 1) * T, :].rearrange("(j p) d -> p j d", p=128), ya[:, :, :])
        k.end()
        k.begin()
        yg = [k.sb("yg%d" % i, [128, D], F32) for i in range(4)]
        xr = [k.sb("xr%d" % i, [128, 8, T], F32) for i in range(2)]
        for tt in range(NT):
            tok = slice(tt * T, (tt + 1) * T)
            x_ = xr[tt % 2]
            k.load(x_, x_[:, :, :], XT[:, tok].rearrange("(c p) t -> p c t", p=128))
            for j in range(4):
                b = tt * 4 + j
                k.idma(yg[j][:, :], YS[:, :], self.SLOTI_p, self.SLOTI_p[:, b:b + 1], yg[j], True, NS)
            for c in range(8):
                bank = k.bank()
                k.tr(bank, [(bank[:, j * 128:(j + 1) * 128], yg[j][:, c * 128:(c + 1) * 128]) for j in range(4)], yg, self.ident)
                k.op("dve", lambda e, x_=x_, c=c, bank=bank: e.scalar_tensor_tensor(out=x_[:, c, :], in0=bank[:, :], scalar=cf[:, l, 5, c:c + 1], in1=x_[:, c, :],
                                                                                   op0=ALU.mult, op1=ALU.add), [bank, cf, x_], [x_])
            k.store(x_, XT[:, tok].rearrange("(c p) t -> p c t", p=128), x_[:, :, :])
        k.end()

    def stage_conf(self):
        k = self.k
        cf = self.coef
        g = self.gains
        XT = self.S("XT", [D, SEQ])
        GLU = self.S("GLU_T", [D, SEQ])
        k.begin()
        stg = [k.sb("stg%d" % i, [128, 8, 256], F32) for i in range(2)]
        w1 = k.sb("w1", [128, 8, 2 * D], BF16)
        self.load_w16(w1, self.I("conf_w1", [D, 2 * D]), 8, 2 * D, stg)
        xt = [k.sb("xt%d" % i, [128, 8, T], F32) for i in range(2)]
        tmp = k.sb("tmp", [128, 8, T], F32)
        rstd = k.sb("rstd", [128, T], F32)
        hT = k.sb("hT", [128, 8, T], BF16)
        sig = [k.sb("sig%d" % i, [128, T], F32) for i in range(2)]
        glu = [k.sb("glu%d" % i, [128, T], F32) for i in range(2)]
        for i in range(NT):
            x_ = xt[i % 2]
            k.load(x_, x_[:, :, :], XT[:, i * T:(i + 1) * T].rearrange("(c p) t -> p c t", p=128))
            self.rstd_fm(x_, 8, T, tmp, rstd, 1.0 / D)
            self.modulate(x_, rstd, cf[:, 1, 0, :], cf, cf[:, 1, 1, :], cf, tmp, hT, T)
            for oc in range(8):
                n = i * 8 + oc
                bgt = k.bank()
                k.mm(bgt, [(bgt[:, :], w1[:, kc, D + oc * 128:D + (oc + 1) * 128], hT[:, kc, :], kc == 0, kc == 7) for kc in range(8)], [w1, hT])
                bv = k.bank()
                k.mm(bv, [(bv[:, :], w1[:, kc, oc * 128:(oc + 1) * 128], hT[:, kc, :], kc == 0, kc == 7) for kc in range(8)], [w1, hT])
                s_, gl = sig[n % 2], glu[n % 2]
                k.op("act", lambda e, s_=s_, bgt=bgt, oc=oc: e.activation(out=s_[:, :], in_=bgt[:, :], func=AF.Sigmoid, bias=g[:, 10, oc:oc + 1], scale=1.0), [bgt, g], [s_])
                k.op("dve", lambda e, gl=gl, bv=bv, s_=s_, oc=oc: e.scalar_tensor_tensor(out=gl[:, :], in0=bv[:, :], scalar=g[:, 9, oc:oc + 1], in1=s_[:, :],
                                                                                        op0=ALU.add, op1=ALU.mult), [bv, g, s_], [gl])
                k.store(gl, GLU[oc * 128:(oc + 1) * 128, i * T:(i + 1) * T], gl[:, :], q=("sp" if n % 2 else "pool"))
        k.end()
        k.begin()
        stg = [k.sb("stg%d" % i, [128, 8, 256], F32) for i in range(2)]
        w2 = k.sb("w2", [128, 8, D], BF16)
        self.load_w16(w2, self.I("conf_w2", [D, D]), 8, D, stg)
        dww = k.sb("dww", [128, 8, 31], F32)
        k.load(dww, dww[:, :, :], self.I("conf_dw", [128, 8, 31]))
        dg = k.sb("dg", [128, 8, 31, 128], BF16)
        for c in range(8):
            for j in range(31):
                if (c * 31 + j) % 2:
                    k.op("dve", lambda e, c=c, j=j: e.tensor_scalar(out=dg[:, c, j, :], in0=self.ident[:, :], scalar1=dww[:, c, j:j + 1],
                                                                    scalar2=None, op0=ALU.mult), [self.ident, dww], [dg])
                else:
                    k.op("act", lambda e, c=c, j=j: e.activation(out=dg[:, c, j, :], in_=self.ident[:, :], func=AF.Copy, scale=dww[:, c, j:j + 1]),
                         [self.ident, dww], [dg])
        HL = T + 30
        gin = [k.sb("gin%d" % i, [128, HL], F32) for i in range(2)]
        g16 = [k.sb("g16%d" % i, [128, HL], BF16) for i in range(2)]
        cv = k.sb("cv", [128, 8, T], F32)
        sq = k.sb("sq", [128, 8, T], F32)
        mean = k.sb("mean", [128, T], F32)
        rstd = k.sb("rstd", [128, T], F32)
        uT = k.sb("uT", [128, 8, T], BF16)
        xt = [k.sb("xt%d" % i, [128, 8, T], F32) for i in range(2)]
        mt = [k.sb("mt%d" % i, [128, T], F32) for i in range(2)]
        nld = 0
        for i in range(NT):
            x_ = xt[i % 2]
            k.load(x_, x_[:, :, :], XT[:, i * T:(i + 1) * T].rearrange("(c p) t -> p c t", p=128), q="pool")
            lo = max(0, i * T - 15)
            hi = min(SEQ, (i + 1) * T + 15)
            off = lo - (i * T - 15)
            for c in range(8):
                gi_, gb_ = gin[nld % 2], g16[nld % 2]
                nld += 1
                if i == 0 or i == NT - 1:
                    k.op("dve", lambda e, gi_=gi_: e.memset(gi_[:, :], 0.0), [], [gi_])
                k.load(gi_, gi_[:, off:off + (hi - lo)], GLU[c * 128:(c + 1) * 128, lo:hi])
                k.cp("dve" if c % 2 else "act", gb_, gb_[:, :], gi_, gi_[:, :])
                bank = k.bank()
                k.mm(bank, [(bank[:, :], dg[:, c, j, :], gb_[:, j:j + T], j == 0, j == 30) for j in range(31)], [dg, gb_])
                k.op("act", lambda e, c=c, bank=bank: e.activation(out=cv[:, c, :], in_=bank[:, :], func=AF.Identity, bias=g[:, 5, c:c + 1], scale=1.0), [bank, g], [cv])
            k.op("act", lambda e: e.activation(out=sq[:, :, :], in_=cv[:, :, :], func=AF.Square), [cv], [sq])
            bm = k.bank()
            k.mm(bm, [(bm[:, :], self.ones[:, :], cv[:, c, :], c == 0, c == 7) for c in range(8)], [self.ones, cv])
            bq = k.bank()
            k.mm(bq, [(bq[:, :], self.ones[:, :], sq[:, c, :], c == 0, c == 7) for c in range(8)], [self.ones, sq])
            k.op("act", lambda e, bm=bm: e.activation(out=mean[:, :], in_=bm[:, :], func=AF.Copy, scale=1.0 / D), [bm], [mean])
            k.op("dve", lambda e: e.tensor_tensor(out=rstd[:, :], in0=mean[:, :], in1=mean[:, :], op=ALU.mult), [mean], [rstd])
            k.op("dve", lambda e, bq=bq: e.scalar_tensor_tensor(out=rstd[:, :], in0=bq[:, :], scalar=1.0 / D, in1=rstd[:, :], op0=ALU.mult, op1=ALU.subtract), [bq, rstd], [rstd])
            k.op("act", lambda e: e.activation(out=rstd[:, :], in_=rstd[:, :], func=AF.Sqrt, bias=self.eps_c[:, 0:1], scale=1.0), [rstd, self.eps_c], [rstd])
            k.op("dve", lambda e: e.reciprocal(out=rstd[:, :], in_=rstd[:, :]), [rstd], [rstd])
            for c in range(8):
                k.op("pool", lambda e, c=c: e.tensor_tensor(out=sq[:, c, :], in0=cv[:, c, :], in1=mean[:, :], op=ALU.subtract), [cv, mean], [sq])
                k.op("dve", lambda e, c=c: e.scalar_tensor_tensor(out=sq[:, c, :], in0=sq[:, c, :], scalar=g[:, 6, c:c + 1], in1=rstd[:, :], op0=ALU.mult, op1=ALU.mult), [sq, g, rstd], [sq])
                k.op("act", lambda e, c=c: e.activation(out=uT[:, c, :], in_=sq[:, c, :], func=AF.Silu, bias=g[:, 7, c:c + 1], scale=1.0), [sq, g], [uT])
            for oc in range(8):
                m_ = mt[oc % 2]
                bank = k.bank()
                k.mm(bank, [(bank[:, :], w2[:, kc, oc * 128:(oc + 1) * 128], uT[:, kc, :], kc == 0, kc == 7) for kc in range(8)], [w2, uT])
                k.op("act", lambda e, m_=m_, bank=bank, oc=oc: e.activation(out=m_[:, :], in_=bank[:, :], func=AF.Identity, bias=g[:, 8, oc:oc + 1], scale=1.0), [bank, g], [m_])
                k.op("dve", lambda e, m_=m_, x_=x_, oc=oc: e.scalar_tensor_tensor(out=x_[:, oc, :], in0=m_[:, :], scalar=cf[:, 1, 2, oc:oc + 1], in1=x_[:, oc, :],
                                                                                 op0=ALU.mult, op1=ALU.add), [m_, cf, x_], [x_])
            k.store(x_, XT[:, i * T:(i + 1) * T].rearrange("(c p) t -> p c t", p=128), x_[:, :, :])
        k.end()

    def stage_final(self):
        k = self.k
        k.begin()
        XT = self.S("XT", [D, SEQ])
        out = self.nc.dram_tensor("out", [SEQ, D], F32, kind="ExternalOutput").ap()
        g = self.gains
        xt = [k.sb("xt%d" % i, [128, 8, T], F32) for i in range(2)]
        sq = k.sb("sq", [128, 8, T], F32)
        rstd = k.sb("rstd", [128, T], F32)
        ot = [k.sb("ot%d" % i, [128, 4, D], F32) for i in range(2)]
        for i in range(NT):
            x_ = xt[i % 2]
            o_ = ot[i % 2]
            k.load(x_, x_[:, :, :], XT[:, i * T:(i + 1) * T].rearrange("(c p) t -> p c t", p=128))
            self.rstd_fm(x_, 8, T, sq, rstd, 1.0 / D)
            for c in range(8):
                k.op("dve", lambda e, c=c, x_=x_: e.scalar_tensor_tensor(out=sq[:, c, :], in0=x_[:, c, :], scalar=g[:, 4, c:c + 1], in1=rstd[:, :],
                                                                        op0=ALU.mult, op1=ALU.mult), [x_, g, rstd], [sq])
            for j in range(4):
                for hf in range(2):
                    bank = k.bank()
                    k.tr(bank, [(bank[:, cc * 128:(cc + 1) * 128], sq[:, hf * 4 + cc, j * 128:(j + 1) * 128]) for cc in range(4)], [sq], self.ident)
                    k.cp("act" if hf else "dve", o_, o_[:, j, hf * 512:(hf + 1) * 512], bank, bank[:, :])
            k.store(o_, out[i * T:(i + 1) * T, :].rearrange("(j p) d -> p j d", p=128), o_[:, :, :])
        k.end()

    def build(self):
        self.consts()
        st = self.stages
        if st is None or "moe0r" in st or "moe1r" in st:
            self.start_wcast()
        if st is None or "mods" in st:
            self.stage_mods()
        if st is None or "inproj" in st:
            self.stage_inproj()
        if st is None or "conv" in st:
            self.stage_conv()
        if st is None or "delta" in st:
            self.stage_delta()
        if st is None or "fnet" in st:
            self.stage_fnet()
        if st is None or "mix" in st:
            self.stage_mix()
        if st is not None and "moe0" in st:
            self.stage_moe(0)
        if st is None or "moe0r" in st:
            self.stage_moe_r(0)
        if st is None or "conf" in st:
            self.stage_conf()
        if st is not None and "moe1" in st:
            self.stage_moe(1)
        if st is None or "moe1r" in st:
            self.stage_moe_r(1)
        if st is None or "final" in st:
            self.stage_final()
        self.k.barrier()
        return self.nc


def _fm(v):
    return np.ascontiguousarray(np.asarray(v, np.float32).reshape(8, 128).T)


def _const_tables():
    t = {}
    quarter = D // 4
    omega = (1.0 / np.power(np.float32(10000.0), np.arange(quarter, dtype=np.float32) / np.float32(quarter))).astype(np.float32)

    def axis_emb(n):
        ang = (np.arange(n, dtype=np.float32)[:, None] * omega[None, :]).astype(np.float32)
        return np.concatenate([np.sin(ang), np.cos(ang)], axis=-1).astype(np.float32)

    rows, cols = SEQ // 64, 64
    er = np.broadcast_to(axis_emb(rows)[:, None, :], (rows, cols, D // 2))
    ec = np.broadcast_to(axis_emb(cols)[None, :, :], (rows, cols, D // 2))
    t["pos"] = np.ascontiguousarray(np.concatenate([er, ec], axis=-1).reshape(SEQ, D).astype(np.float32))
    n = np.arange(SEQ, dtype=np.int64)
    ang = 2.0 * np.pi * ((n[:, None] * n[None, :]) % SEQ).astype(np.float64) / SEQ
    t["dft_c"] = np.cos(ang).astype(ml_dtypes.bfloat16)
    t["dft_s"] = (-np.sin(ang)).astype(ml_dtypes.bfloat16)
    m = np.arange(128, dtype=np.int64)
    a2 = 2.0 * np.pi * ((m[:, None] * m[None, :]) % 128).astype(np.float64) / 128
    t["cs_ch"] = np.concatenate([np.cos(a2), np.sin(a2)], axis=1).astype(ml_dtypes.bfloat16)
    return t


_TABLES = None


def tables():
    global _TABLES
    if _TABLES is None:
        _TABLES = _const_tables()
    return _TABLES


def shared_inputs(inp):
    f = lambda a: np.ascontiguousarray(np.asarray(a, np.float32))
    s = dict(tables())
    s["ada_w"] = f(inp["ada_w"])
    s["ada_b"] = np.ascontiguousarray(f(inp["ada_b"]).reshape(2, 48, 128).transpose(2, 0, 1))
    vecs = [inp["norm1_g"][0], inp["norm1_g"][1], inp["norm2_g"][0], inp["norm2_g"][1], inp["final_g"],
            inp["conf_dw_b"][0], inp["conf_ln_g"][0], inp["conf_ln_b"][0], inp["conf_b2"][0],
            inp["conf_b1"][0][:D], inp["conf_b1"][0][D:]]
    s["gains"] = np.ascontiguousarray(np.stack([_fm(v) for v in vecs], axis=1))
    w_in = f(inp["hyb_w_in"][0])
    s["w_qkv"] = np.ascontiguousarray(w_in[:, 0:1536])
    s["w_ab"] = np.ascontiguousarray(w_in[:, 1536:1552])
    s["w_z"] = np.ascontiguousarray(w_in[:, 1552:2064])
    s["w_f"] = np.ascontiguousarray(w_in[:, 2064:2576])
    s["conv_w"] = np.ascontiguousarray(f(inp["dn_conv_w"][0]).T.reshape(12, 128, 5).transpose(1, 0, 2))
    s["adt"] = np.ascontiguousarray(np.stack([f(inp["dn_a_log"][0]).reshape(8), f(inp["dn_dt_bias"][0]).reshape(8)], axis=1))
    s["onorm_g"] = np.ascontiguousarray(np.tile(f(inp["dn_onorm_g"][0])[None, :], (128, 1)))
    s["w_out"] = f(inp["hyb_w_out"][0])
    s["router_w"] = f(inp["router_w"])
    s["rbias"] = np.ascontiguousarray(np.tile(f(inp["router_bias"])[None, :], (128, 4)))
    s["moe_wg"] = f(inp["moe_w_gate"]); s["moe_wu"] = f(inp["moe_w_up"]); s["moe_wd"] = f(inp["moe_w_down"])
    s["moe_wgl"] = np.ascontiguousarray(s["moe_wg"].reshape(2, NEXP, 8, 128, FF).transpose(0, 1, 3, 2, 4)).reshape(2 * NEXP * 128, 8 * FF)
    s["moe_wul"] = np.ascontiguousarray(s["moe_wu"].reshape(2, NEXP, 8, 128, FF).transpose(0, 1, 3, 2, 4)).reshape(2 * NEXP * 128, 8 * FF)
    s["moe_wdl"] = np.ascontiguousarray(s["moe_wd"].reshape(2, NEXP, 4, 128, D).transpose(0, 1, 3, 2, 4)).reshape(2 * NEXP * 128, 4 * D)
    tt_ = np.arange(128)
    s["mconst"] = np.ascontiguousarray(np.concatenate([(tt_[:, None] < tt_[None, :]).astype(np.float32), tt_[:, None].astype(np.float32)], axis=1))
    s["conf_w1"] = f(inp["conf_w1"][0]); s["conf_w2"] = f(inp["conf_w2"][0])
    s["conf_dw"] = np.ascontiguousarray(f(inp["conf_dw_w"][0]).T.reshape(8, 128, 31).transpose(1, 0, 2))
    j = np.arange(64)
    s["dmask"] = np.ascontiguousarray(np.stack([(j[:, None] <= j[None, :]), (j[:, None] < j[None, :]),
                                                (j[:, None] >= j[None, :]), (j[:, None] > j[None, :])], axis=1).astype(np.float32))
    return s


def core_inputs(inp, b):
    f = lambda a: np.ascontiguousarray(np.asarray(a, np.float32))
    c = {}
    c["x"] = f(inp["x"][b])
    c["ctx"] = f(inp["ctx"][b])
    cc = np.stack([f(inp["c"][b]), f(inp["c_ctx"])], axis=-1)
    c["cc"] = np.ascontiguousarray(cc.reshape(8, 128, 2).transpose(1, 0, 2))
    return c


_PROG = None


def kernel(**inputs):
    global _PROG
    if _PROG is None:
        import os
        st = os.environ.get("KSTAGES")
        P = Prog(stages=(st.split(",") if st else None))
        P.build()
        _PROG = P
    P = _PROG
    shared = shared_inputs(inputs)
    in_maps = []
    for b in range(8):
        allin = dict(shared)
        allin.update(core_inputs(inputs, b))
        in_maps.append({n: allin[n] for n in P.inp})
    res = run_bass_kernel_spmd(P.nc, in_maps, core_ids=list(range(8)))
    return np.stack([np.asarray(r["out"], np.float32) for r in res.results], axis=0)
```

```python
import numpy as np
import ml_dtypes
from contextlib import ExitStack
import concourse.bass as bass
import concourse.mybir as mybir
from concourse.bass_utils import run_bass_kernel_spmd

F32 = mybir.dt.float32
BF16 = mybir.dt.bfloat16
AF = mybir.ActivationFunctionType
ALU = mybir.AluOpType
AX = mybir.AxisListType

D = 1024
SEQ = 4096
CTX = 256
NEXP = 32
FF = 512
T = 512
NT = SEQ // T
EPS = 1e-6
SAME_ENG_SYNC = True


class Buf:
    def __init__(self, t, name):
        self.t = t
        self.name = name
        self.wr = None
        self.rd = {}
        self.ds = None

    def __getitem__(self, idx):
        return self.t[idx]


class SubBuf:
    def __init__(self, parent, rows, c0, width):
        self.p = parent
        self.rows = rows
        self.c0 = c0
        self.width = width
        self.name = parent.name

    def __getitem__(self, idx):
        rs, cs = idx
        if rs == slice(None):
            rs = slice(0, self.rows)
        a = 0 if cs.start is None else cs.start
        b = self.width if cs.stop is None else cs.stop
        return self.p.t[rs, self.c0 + a:self.c0 + b]

    wr = property(lambda self: self.p.wr, lambda self, v: setattr(self.p, "wr", v))
    rd = property(lambda self: self.p.rd, lambda self, v: setattr(self.p, "rd", v))
    ds = property(lambda self: self.p.ds, lambda self, v: setattr(self.p, "ds", v))


class KB:
    ENG = ("pe", "act", "dve", "pool", "sp")

    def __init__(self, nc):
        self.nc = nc
        self.e = {"pe": nc.tensor, "act": nc.scalar, "dve": nc.vector, "pool": nc.gpsimd, "sp": nc.sync}
        self.sem = {k: nc.alloc_semaphore("s_" + k) for k in self.ENG}
        self.cnt = {k: 0 for k in self.ENG}
        self.seen = {k: {} for k in self.ENG}
        self.dpool = {"sp": [], "pool": [], "act": []}
        self.dall = []
        self.gstack = ExitStack()
        self.stack = None
        self.stage_bufs = []
        self.nbank = 0
        self.ps = [Buf(nc.alloc_psum_tensor("ps%d" % i, [128, 512], F32), "ps%d" % i) for i in range(8)]
        self.rr = 0
        self.nstage = 0

    def sb(self, name, shape, dtype, persistent=False):
        st = self.gstack if persistent else self.stack
        t = st.enter_context(self.nc.sbuf_tensor("sb%d_%s" % (self.nstage, name), list(shape), dtype))
        b = Buf(t, name)
        if not persistent:
            self.stage_bufs.append(b)
        return b

    def begin(self):
        self.nstage += 1
        self.stack = ExitStack()
        self.stage_bufs = []

    def end(self):
        self.barrier()
        for b in self.stage_bufs:
            if b.ds is not None:
                for q_, ds_ in b.ds.items():
                    self.dpool[q_].append(ds_)
                b.ds = None
        self.stack.close()
        self.stack = None

    def bank(self):
        b = self.ps[self.nbank % 8]
        self.nbank += 1
        return b

    def _wait(self, eng, ev):
        key, sem, val = ev
        if self.seen[eng].get(key, 0) >= val:
            return
        self.e[eng].wait_ge(sem, val)
        self.seen[eng][key] = val

    def _deps(self, eng, reads, writes):
        own = "e:" + eng
        evs = []
        for b in reads:
            if b.wr is not None:
                evs.append(b.wr)
        for b in writes:
            if b.wr is not None:
                evs.append(b.wr)
            evs.extend(b.rd.values())
        for ev in evs:
            if ev[0] == own and (eng == "pe" or not SAME_ENG_SYNC):
                continue
            self._wait(eng, ev)

    def _commit(self, eng, ins, reads, writes):
        own = "e:" + eng
        self.cnt[eng] += 1
        ins.then_inc(self.sem[eng], 1)
        ev = (own, self.sem[eng], self.cnt[eng])
        for b in reads:
            b.rd[own] = ev
        for b in writes:
            b.wr = ev
            b.rd = {}

    def op(self, eng, fn, reads=(), writes=()):
        self._deps(eng, reads, writes)
        ins = fn(self.e[eng])
        self._commit(eng, ins, reads, writes)
        return ins

    def mm(self, outb, steps, reads):
        self._deps("pe", reads, [outb])
        ins = None
        for (o, l, r, st, sp) in steps:
            ins = self.nc.tensor.matmul(o, lhsT=l, rhs=r, start=st, stop=sp)
        self._commit("pe", ins, reads, [outb])

    def tr(self, outb, steps, reads, ident):
        self._deps("pe", list(reads) + [ident], [outb])
        ins = None
        for (o, i) in steps:
            ins = self.nc.tensor.transpose(out=o, in_=i, identity=ident[0:i.shape[0], 0:i.shape[0]])
        self._commit("pe", ins, list(reads) + [ident], [outb])

    def _dsem(self, b, q):
        if b.ds is None:
            b.ds = {}
        if q not in b.ds:
            if self.dpool[q]:
                b.ds[q] = self.dpool[q].pop()
            else:
                name = "d%d" % len(self.dall)
                b.ds[q] = [self.nc.alloc_semaphore(name), 0, "d:" + name]
                self.dall.append(b.ds[q])
        return b.ds[q]

    def dma(self, q, out, in_, buf, load):
        if load:
            self._deps(q, [], [buf])
        else:
            self._deps(q, [buf], [])
        ds = self._dsem(buf, q)
        ds[1] += 16
        self.e[q].dma_start(out=out, in_=in_).then_inc(ds[0], 16)
        ev = (ds[2], ds[0], ds[1])
        if load:
            buf.wr = ev
            buf.rd = {}
        else:
            buf.rd[ds[2]] = ev

    def idma(self, out, in_, idxb, idx_ap, buf, gather, nrows):
        q = "pool"
        if gather:
            self._deps(q, [idxb], [buf])
        else:
            self._deps(q, [buf, idxb], [])
        ds = self._dsem(buf, q)
        ds[1] += 16
        off = bass.IndirectOffsetOnAxis(ap=idx_ap, axis=0)
        if gather:
            self.nc.gpsimd.indirect_dma_start(out=out, out_offset=None, in_=in_, in_offset=off).then_inc(ds[0], 16)
        else:
            r_ = self.nc.gpsimd.indirect_dma_start(out=out, out_offset=off, in_=in_, in_offset=None)
            r_.then_inc(ds[0], 16)
        ev = (ds[2], ds[0], ds[1])
        if gather:
            buf.wr = ev
            buf.rd = {}
        else:
            buf.rd[ds[2]] = ev
        idxb.rd[ds[2]] = ev

    def load(self, buf, out, in_, q="sp"):
        self.dma(q, out, in_, buf, True)

    def store(self, buf, out, in_, q="sp"):
        self.dma(q, out, in_, buf, False)

    def barrier(self):
        evs = [("e:" + k, self.sem[k], self.cnt[k]) for k in ("pe", "act", "dve", "pool") if self.cnt[k] > 0]
        evs += [(d[2], d[0], d[1]) for d in self.dall if d[1] > 0]
        for eng in self.ENG:
            for ev in evs:
                if ev[0] == "e:" + eng:
                    continue
                self._wait(eng, ev)

    def cp(self, eng, outb, out, inb, in_):
        if eng == "act":
            self.op("act", lambda e: e.copy(out=out, in_=in_), [inb], [outb])
        else:
            self.op(eng, lambda e: e.tensor_copy(out=out, in_=in_), [inb], [outb])


def _dram(nc, name, shape, dt, dbg):
    return nc.dram_tensor(name, list(shape), dt, kind=("ExternalOutput" if dbg else "Internal")).ap()


class Prog:
    def __init__(self, stages=None, dbg=(), ext_in=()):
        self.stages = stages
        self.dbgset = set(dbg)
        nc = bass.Bass("TRN2", target_bir_lowering=False)
        self.nc = nc
        self.k = KB(nc)
        self.inp = {}
        self.ext_in = set(ext_in)
        self.scr = {}

    def I(self, name, shape, dt=F32):
        if name not in self.inp:
            self.inp[name] = self.nc.dram_tensor(name, list(shape), dt, kind="ExternalInput").ap()
        return self.inp[name]

    def S(self, name, shape, dt=F32):
        if name not in self.scr:
            if name in self.ext_in:
                self.scr[name] = self.nc.dram_tensor(name, list(shape), dt, kind="ExternalInput").ap()
            else:
                self.scr[name] = _dram(self.nc, name, shape, dt, name in self.dbgset)
        return self.scr[name]

    def consts(self):
        k = self.k
        nc = self.nc
        self.ident = k.sb("ident", [128, 128], F32, True)
        self.ones = k.sb("ones", [128, 128], F32, True)
        k.op("pool", lambda e: e.memset(self.ident[:, :], 0.0), [], [self.ident])
        k.op("pool", lambda e: e.affine_select(out=self.ident[:, :], in_=self.ident[:, :], pattern=[[-1, 128]],
                                               compare_op=ALU.not_equal, fill=1.0, base=0, channel_multiplier=1),
             [self.ident], [self.ident])
        k.op("pool", lambda e: e.memset(self.ones[:, :], 1.0), [], [self.ones])
        self.NG = 11
        self.gains = k.sb("gains", [128, self.NG, 8], F32, True)
        k.load(self.gains, self.gains[:, :, :], self.I("gains", [128, self.NG, 8]))
        self.coef = k.sb("coef", [128, 2, 6, 8], F32, True)
        self.coefc = k.sb("coefc", [128, 2, 8], F32, True)
        self.eps_c = k.sb("eps_c", [128, 1], F32, True)
        self.gbtok = k.sb("gbtok", [64, 68, 16], F32, True)
        self.SLOTI_p = k.sb("SLOTI", [128, 32], mybir.dt.int32, True)
        self.IDXI_p = k.sb("IDXI", [128, 64], mybir.dt.int32, True)
        k.op("pool", lambda e: e.memset(self.eps_c[:, :], EPS), [], [self.eps_c])

    def start_wcast(self):
        nc = self.nc
        NR = 2 * NEXP * 128
        srcs = (self.I("moe_wgl", [NR, 8 * FF]), self.I("moe_wul", [NR, 8 * FF]), self.I("moe_wdl", [NR, 4 * D]))
        self.w16 = (self.S("W16g", [NR, 8 * FF], BF16), self.S("W16u", [NR, 8 * FF], BF16), self.S("W16d", [NR, 4 * D], BF16))
        self.WCW = 6
        self.wc_sems = [nc.alloc_semaphore("wcast%d" % i) for i in range(self.WCW)]
        self.wc_todo = []
        self.wc_issued = 0
        for l in range(2):
            for e_ in range(NEXP):
                r0 = (l * NEXP + e_) * 128
                for src, dst in zip(srcs, self.w16):
                    self.wc_todo.append((dst[r0:r0 + 128, :], src[r0:r0 + 128, :]))

    def wcast_tick(self, n=1):
        nc = self.nc
        for _ in range(n):
            if not self.wc_todo:
                return
            i = self.wc_issued
            sem = self.wc_sems[i % self.WCW]
            if i >= self.WCW:
                nc.gpsimd.wait_ge(sem, 16 * (i // self.WCW))
            dst, src = self.wc_todo.pop(0)
            nc.gpsimd.dma_start(out=dst, in_=src).then_inc(sem, 16)
            self.wc_issued += 1

    def wcast_finish(self):
        self.wcast_tick(n=10 ** 6)
        n = self.wc_issued
        for j, sem in enumerate(self.wc_sems):
            cnt = (n - j + self.WCW - 1) // self.WCW
            if cnt > 0:
                ev = ("d:wcast%d" % j, sem, 16 * cnt)
                for eng_ in KB.ENG:
                    self.k._wait(eng_, ev)

    def stage_mods(self):
        k = self.k
        k.begin()
        ada_w = self.I("ada_w", [2, D, 6 * D])
        ada_b = self.I("ada_b", [128, 2, 48])
        cc_in = self.I("cc", [128, 8, 2])
        cc = k.sb("cc", [128, 8, 2], F32)
        scc = k.sb("scc", [128, 8, 2], F32)
        adab = k.sb("adab", [128, 2, 48], F32)
        modT = k.sb("modT", [128, 2, 48, 2], F32)
        wp = [k.sb("adaw%d" % i, [128, 8, 768], F32) for i in range(2)]
        k.load(cc, cc[:, :, :], cc_in)
        k.load(adab, adab[:, :, :], ada_b)
        k.op("act", lambda e: e.activation(out=scc[:, :, :], in_=cc[:, :, :], func=AF.Silu), [cc], [scc])
        n = 0
        for l in range(2):
            bank = k.bank()
            for pnl in range(8):
                w = wp[n % 2]
                n += 1
                k.load(w, w[:, :, :], ada_w[l, :, pnl * 768:(pnl + 1) * 768].rearrange("(k p) n -> p k n", p=128),
                       q=("sp" if n % 2 else "pool"))
                steps = []
                for jj in range(6):
                    jc = pnl * 6 + jj
                    for kc in range(8):
                        steps.append((bank[:, jc * 2:jc * 2 + 2], w[:, kc, jj * 128:(jj + 1) * 128], scc[:, kc, :],
                                      kc == 0, kc == 7))
                k.mm(bank, steps, [w, scc])
            for col in range(2):
                src = bank[:, 0:96].rearrange("p (j c) -> p j c", c=2)[:, :, col]
                k.op("dve", lambda e, src=src, col=col, l=l: e.tensor_tensor(out=modT[:, l, :, col], in0=src,
                                                                             in1=adab[:, l, :], op=ALU.add),
                     [bank, adab], [modT])
        g = self.gains
        cf = self.coef
        for l in range(2):
            k.op("dve", lambda e, l=l: e.scalar_tensor_tensor(out=cf[:, l, 0, :], in0=modT[:, l, 8:16, 0], scalar=1.0,
                                                             in1=g[:, 0 + l, :], op0=ALU.add, op1=ALU.mult),
                 [modT, g], [cf])
            k.op("dve", lambda e, l=l: e.tensor_copy(out=cf[:, l, 1, :], in_=modT[:, l, 0:8, 0]), [modT], [cf])
            k.op("dve", lambda e, l=l: e.tensor_copy(out=cf[:, l, 2, :], in_=modT[:, l, 16:24, 0]), [modT], [cf])
            k.op("dve", lambda e, l=l: e.scalar_tensor_tensor(out=cf[:, l, 3, :], in0=modT[:, l, 32:40, 0], scalar=1.0,
                                                             in1=g[:, 2 + l, :], op0=ALU.add, op1=ALU.mult),
                 [modT, g], [cf])
            k.op("dve", lambda e, l=l: e.tensor_copy(out=cf[:, l, 4, :], in_=modT[:, l, 24:32, 0]), [modT], [cf])
            k.op("dve", lambda e, l=l: e.tensor_copy(out=cf[:, l, 5, :], in_=modT[:, l, 40:48, 0]), [modT], [cf])
        cfc = self.coefc
        k.op("dve", lambda e: e.scalar_tensor_tensor(out=cfc[:, 0, :], in0=modT[:, 0, 8:16, 1], scalar=1.0,
                                                     in1=g[:, 0, :], op0=ALU.add, op1=ALU.mult), [modT, g], [cfc])
        k.op("dve", lambda e: e.tensor_copy(out=cfc[:, 1, :], in_=modT[:, 0, 0:8, 1]), [modT], [cfc])
        if "coef" in self.dbgset:
            k.store(cf, self.S("coef", [128, 2, 6, 8]), cf[:, :, :, :])
            k.store(cfc, self.S("coefc", [128, 2, 8]), cfc[:, :, :])
        k.end()

    def load_w16(self, dst, src, kch, ncols, stg, n0=0):
        k = self.k
        engs = ("dve", "act")
        n = n0
        for c0 in range(0, ncols, 256):
            w = min(256, ncols - c0)
            s = stg[n % 2]
            k.load(s, s[:, :, 0:w], src[:, c0:c0 + w].rearrange("(k p) n -> p k n", p=128), q=("sp" if n % 2 else "pool"))
            k.cp(engs[n % 2], dst, dst[:, :, c0:c0 + w], s, s[:, :, 0:w])
            n += 1
        return n

    def rstd_fm(self, xT, nch, Tn, sq, rstd, mean_scale):
        k = self.k
        k.op("act", lambda e: e.activation(out=sq[:, 0:nch, 0:Tn], in_=xT[:, 0:nch, 0:Tn], func=AF.Square), [xT], [sq])
        bank = k.bank()
        k.mm(bank, [(bank[:, 0:Tn], self.ones[:, :], sq[:, c, 0:Tn], c == 0, c == nch - 1) for c in range(nch)],
             [self.ones, sq])
        k.op("act", lambda e: e.activation(out=rstd[:, 0:Tn], in_=bank[:, 0:Tn], func=AF.Sqrt, bias=self.eps_c[:, 0:1],
                                           scale=mean_scale), [bank, self.eps_c], [rstd])
        k.op("dve", lambda e: e.reciprocal(out=rstd[:, 0:Tn], in_=rstd[:, 0:Tn]), [rstd], [rstd])

    def modulate(self, xT, rstd, A, Ab, Bc, Bb, tmp, hT, Tn):
        k = self.k
        for c in range(8):
            k.op("dve", lambda e, c=c: e.scalar_tensor_tensor(out=tmp[:, c, 0:Tn], in0=xT[:, c, 0:Tn], scalar=A[:, c:c + 1],
                                                             in1=rstd[:, 0:Tn], op0=ALU.mult, op1=ALU.mult),
                 [xT, rstd, Ab], [tmp])
        for c in range(8):
            k.op("act", lambda e, c=c: e.activation(out=hT[:, c, 0:Tn], in_=tmp[:, c, 0:Tn], func=AF.Identity,
                                                   bias=Bc[:, c:c + 1], scale=1.0), [tmp, Bb], [hT])

    def stage_inproj(self):
        k = self.k
        k.begin()
        x = self.I("x", [SEQ, D])
        pos = self.I("pos", [SEQ, D])
        ctx = self.I("ctx", [CTX, D])
        XT = self.S("XT", [D, SEQ])
        QKV = self.S("QKV_T", [1536, SEQ])
        QKVc = self.S("QKVc_T", [1536, CTX])
        AT = self.S("A_T", [8, SEQ])
        BT = self.S("B_T", [8, SEQ])
        ATc = self.S("Ac_T", [8, CTX])
        BTc = self.S("Bc_T", [8, CTX])
        Z = self.S("Z", [SEQ, 512])
        GC = self.S("GC", [SEQ, 512], BF16)
        GS = self.S("GS", [SEQ, 512], BF16)
        stg = [k.sb("stg%d" % i, [128, 8, 256], F32) for i in range(2)]
        wqkv = k.sb("wqkv", [128, 8, 1536], BF16)
        wab = k.sb("wab", [128, 8, 16], BF16)
        wz = k.sb("wz", [128, 8, 512], BF16)
        wf = k.sb("wf", [128, 8, 512], BF16)
        csch = k.sb("csch", [128, 256], BF16)
        n = self.load_w16(wqkv, self.I("w_qkv", [D, 1536]), 8, 1536, stg)
        n = self.load_w16(wab, self.I("w_ab", [D, 16]), 8, 16, stg, n)
        n = self.load_w16(wz, self.I("w_z", [D, 512]), 8, 512, stg, n)
        n = self.load_w16(wf, self.I("w_f", [D, 512]), 8, 512, stg, n)
        k.load(csch, csch[:, :], self.I("cs_ch", [128, 256], BF16))
        xin = [k.sb("xin%d" % i, [128, 4, D], F32) for i in range(2)]
        pin = k.sb("pin", [128, 4, D], F32)
        xpT = k.sb("xpT", [128, 8, T], F32)
        tmp = k.sb("tmp", [128, 8, T], F32)
        rstd = k.sb("rstd", [128, T], F32)
        hT = k.sb("hT", [128, 8, T], BF16)
        ev = [k.sb("ev%d" % i, [128, T], F32) for i in range(4)]
        fT = k.sb("fT", [128, 4, T], BF16)
        gcs = [k.sb("gcs%d" % i, [128, 4, 256], BF16) for i in range(2)]
        cf = self.coef
        cfc = self.coefc
        nev = 0
        tiles = [("ctx", 0, CTX)] + [("x", i, T) for i in range(NT)]
        for ti, (kind, i, Tn) in enumerate(tiles):
            nj = Tn // 128
            xi = xin[ti % 2]
            if kind == "x":
                k.load(xi, xi[:, :, :], x[i * T:(i + 1) * T, :].rearrange("(j p) d -> p j d", p=128))
                k.load(pin, pin[:, :, :], pos[i * T:(i + 1) * T, :].rearrange("(j p) d -> p j d", p=128), q="pool")
                k.op("dve", lambda e, xi=xi: e.tensor_tensor(out=xi[:, :, :], in0=xi[:, :, :], in1=pin[:, :, :], op=ALU.add),
                     [xi, pin], [xi])
            else:
                k.load(xi, xi[:, 0:nj, :], ctx.rearrange("(j p) d -> p j d", p=128))
            for c in range(8):
                bank = k.bank()
                k.tr(bank, [(bank[:, j * 128:(j + 1) * 128], xi[:, j, c * 128:(c + 1) * 128]) for j in range(nj)], [xi],
                     self.ident)
                k.cp("act" if c % 2 else "dve", xpT, xpT[:, c, 0:Tn], bank, bank[:, 0:Tn])
            if kind == "x":
                k.store(xpT, XT[:, i * T:(i + 1) * T].rearrange("(c p) t -> p c t", p=128), xpT[:, :, :])
            self.rstd_fm(xpT, 8, Tn, tmp, rstd, 1.0 / D)
            if kind == "x":
                self.modulate(xpT, rstd, cf[:, 0, 0, :], cf, cf[:, 0, 1, :], cf, tmp, hT, Tn)
            else:
                self.modulate(xpT, rstd, cfc[:, 0, :], cfc, cfc[:, 1, :], cfc, tmp, hT, Tn)
            sl = slice(i * T, (i + 1) * T) if kind == "x" else slice(0, CTX)
            dq = QKV if kind == "x" else QKVc
            for oc in range(12):
                bank = k.bank()
                k.mm(bank, [(bank[:, 0:Tn], wqkv[:, kc, oc * 128:(oc + 1) * 128], hT[:, kc, 0:Tn], kc == 0, kc == 7)
                            for kc in range(8)], [wqkv, hT])
                e = ev[nev % 4]
                k.cp("act" if nev % 2 else "dve", e, e[:, 0:Tn], bank, bank[:, 0:Tn])
                k.store(e, dq[oc * 128:(oc + 1) * 128, sl], e[:, 0:Tn], q=("sp" if nev % 2 else "pool"))
                nev += 1
            for gi, dst in enumerate(((AT, BT) if kind == "x" else (ATc, BTc))):
                bank = k.bank()
                k.mm(bank, [(bank[0:8, 0:Tn], wab[:, kc, gi * 8:(gi + 1) * 8], hT[:, kc, 0:Tn], kc == 0, kc == 7)
                            for kc in range(8)], [wab, hT])
                e = ev[nev % 4]
                k.cp("dve", e, e[0:8, 0:Tn], bank, bank[0:8, 0:Tn])
                k.store(e, dst[:, sl], e[0:8, 0:Tn])
                nev += 1
            if kind != "x":
                continue
            for j in range(4):
                bank = k.bank()
                k.mm(bank, [(bank[:, :], hT[:, kc, j * 128:(j + 1) * 128], wz[:, kc, :], kc == 0, kc == 7)
                            for kc in range(8)], [wz, hT])
                e = ev[nev % 4]
                k.op("act", lambda e_, e=e, bank=bank: e_.activation(out=e[:, :], in_=bank[:, :], func=AF.Silu), [bank], [e])
                k.store(e, Z[i * T + j * 128:i * T + (j + 1) * 128, :], e[:, :], q=("sp" if nev % 2 else "pool"))
                nev += 1
            for oc in range(4):
                bank = k.bank()
                k.mm(bank, [(bank[:, :], wf[:, kc, oc * 128:(oc + 1) * 128], hT[:, kc, :], kc == 0, kc == 7)
                            for kc in range(8)], [wf, hT])
                k.cp("act" if oc % 2 else "dve", fT, fT[:, oc, :], bank, bank[:, :])
            for j in range(4):
                gb = gcs[j % 2]
                for half in range(2):
                    bank = k.bank()
                    k.mm(bank, [(bank[:, (gg - 2 * half) * 256:(gg - 2 * half + 1) * 256], fT[:, gg, j * 128:(j + 1) * 128],
                                 csch[:, :], True, True) for gg in range(2 * half, 2 * half + 2)], [fT, csch])
                    k.cp("act" if half else "dve", gb, gb[:, 2 * half:2 * half + 2, :],
                         bank, bank[:, :].rearrange("p (g c) -> p g c", c=256))
                r0 = i * T + j * 128
                k.store(gb, GC[r0:r0 + 128, :].rearrange("t (g c) -> t g c", c=128), gb[:, :, 0:128])
                k.store(gb, GS[r0:r0 + 128, :].rearrange("t (g c) -> t g c", c=128), gb[:, :, 128:256], q="pool")
        k.end()

    def stage_conv(self):
        k = self.k
        k.begin()
        convw = k.sb("convw", [128, 12, 5], F32)
        k.load(convw, convw[:, :, :], self.I("conv_w", [128, 12, 5]))
        adt = k.sb("adt", [8, 2], F32)
        k.load(adt, adt[:, :], self.I("adt", [8, 2]))
        one_c = k.sb("one_c", [128, 1], F32)
        k.op("pool", lambda e: e.memset(one_c[:, :], 1.0), [], [one_c])
        nexpa = k.sb("nexpa", [8, 1], F32)
        k.op("act", lambda e: e.activation(out=nexpa[:, :], in_=adt[:, 0:1], func=AF.Exp), [adt], [nexpa])
        k.op("dve", lambda e: e.tensor_scalar(out=nexpa[:, :], in0=nexpa[:, :], scalar1=-1.0, scalar2=None, op0=ALU.mult),
             [nexpa], [nexpa])
        gb = self.gbtok
        for (L, qname, aname, bname, oname, ch0) in ((CTX, "QKVc_T", "Ac_T", "Bc_T", "QNc_T", 0), (SEQ, "QKV_T", "A_T", "B_T", "QN_T", 4)):
            src = self.S(qname, [1536, L])
            dst = self.S(oname, [1536, L])
            xin = [k.sb("cx%d_%d" % (L, i), [128, L + 4], F32) for i in range(2)]
            acc = [k.sb("ca%d_%d" % (L, i), [128, L], F32) for i in range(2)]
            sq = k.sb("csq%d" % L, [128, L], F32)
            rin = k.sb("crin%d" % L, [128, L], F32)
            for b_ in xin:
                k.op("pool", lambda e, b_=b_: e.memset(b_[:, :], 0.0), [], [b_])
            for cc in range(12):
                xi = xin[cc % 2]
                a = acc[cc % 2]
                eng = "dve"
                k.load(xi, xi[:, 2:L + 2], src[cc * 128:(cc + 1) * 128, :], q=("sp" if cc % 2 else "pool"))
                k.op(eng, lambda e, xi=xi, a=a, cc=cc: e.tensor_scalar(out=a[:, :], in0=xi[:, 0:L], scalar1=convw[:, cc, 0:1],
                                                                      scalar2=None, op0=ALU.mult), [xi, convw], [a])
                for j in range(1, 5):
                    k.op(eng, lambda e, xi=xi, a=a, cc=cc, j=j: e.scalar_tensor_tensor(
                        out=a[:, :], in0=xi[:, j:j + L], scalar=convw[:, cc, j:j + 1], in1=a[:, :], op0=ALU.mult, op1=ALU.add),
                        [xi, convw, a], [a])
                k.op("act", lambda e, a=a: e.activation(out=a[:, :], in_=a[:, :], func=AF.Silu), [a], [a])
                if cc < 8:
                    k.op("act", lambda e, a=a: e.activation(out=sq[:, :], in_=a[:, :], func=AF.Square), [a], [sq])
                    for t0 in range(0, L, 512):
                        w = min(512, L - t0)
                        bank = k.bank()
                        k.mm(bank, [(bank[:, 0:w], self.ones[:, :], sq[:, t0:t0 + w], True, True)], [self.ones, sq])
                        k.op("act", lambda e, bank=bank, t0=t0, w=w: e.activation(out=rin[:, t0:t0 + w], in_=bank[:, 0:w], func=AF.Sqrt,
                                                                                bias=self.eps_c[:, 0:1], scale=1.0), [bank, self.eps_c], [rin])
                    k.op("dve", lambda e: e.reciprocal(out=rin[:, :], in_=rin[:, :]), [rin], [rin])
                    k.op("dve", lambda e, a=a: e.tensor_tensor(out=a[:, :], in0=a[:, :], in1=rin[:, :], op=ALU.mult), [a, rin], [a])
                k.store(a, dst[cc * 128:(cc + 1) * 128, :], a[:, :], q=("sp" if cc % 2 == 0 else "pool"))
            ga = k.sb("ga%d" % L, [8, L], F32)
            gbb = k.sb("gb%d" % L, [8, L], F32)
            gy = k.sb("gy%d" % L, [8, L], F32)
            gl = k.sb("gl%d" % L, [8, L], F32)
            k.load(ga, ga[:, :], self.S(aname, [8, L]))
            k.load(gbb, gbb[:, :], self.S(bname, [8, L]))
            k.op("dve", lambda e: e.tensor_scalar(out=gy[:, :], in0=ga[:, :], scalar1=adt[:, 1:2], scalar2=None, op0=ALU.add), [ga, adt], [gy])
            k.op("act", lambda e: e.activation(out=gl[:, :], in_=gy[:, :], func=AF.Abs), [gy], [gl])
            k.op("act", lambda e: e.activation(out=gl[:, :], in_=gl[:, :], func=AF.Exp, scale=-1.0), [gl], [gl])
            k.op("act", lambda e: e.activation(out=gl[:, :], in_=gl[:, :], func=AF.Ln, bias=one_c[0:8, 0:1], scale=1.0), [gl, one_c], [gl])
            k.op("dve", lambda e: e.tensor_scalar(out=gy[:, :], in0=gy[:, :], scalar1=0.0, scalar2=None, op0=ALU.max), [gy], [gy])
            k.op("dve", lambda e: e.tensor_tensor(out=gy[:, :], in0=gy[:, :], in1=gl[:, :], op=ALU.add), [gy, gl], [gy])
            k.op("dve", lambda e: e.tensor_scalar(out=gy[:, :], in0=gy[:, :], scalar1=nexpa[:, 0:1], scalar2=None, op0=ALU.mult), [gy, nexpa], [gy])
            k.op("act", lambda e: e.activation(out=gbb[:, :], in_=gbb[:, :], func=AF.Sigmoid), [gbb], [gbb])
            nch = L // 64
            for gi, srcb in enumerate((gy, gbb)):
                bank = k.bank()
                k.tr(bank, [(bank[0:64, n * 8:(n + 1) * 8], srcb[:, n * 64:(n + 1) * 64]) for n in range(nch)], [srcb], self.ident)
                k.op("dve", lambda e, bank=bank, gi=gi: e.tensor_copy(out=gb[:, ch0:ch0 + nch, gi * 8:(gi + 1) * 8],
                                                                      in_=bank[0:64, 0:nch * 8].rearrange("p (n c) -> p n c", c=8)), [bank], [gb])
        if "gbtok" in self.dbgset:
            k.store(gb, self.S("gbtok", [64, 68, 16]), gb[:, :, :])
        k.end()

    def stage_delta(self):
        k = self.k
        k.begin()
        QN = self.S("QN_T", [1536, SEQ])
        QNc = self.S("QNc_T", [1536, CTX])
        Z = self.S("Z", [SEQ, 512])
        OFB = [self.S("OF", [SEQ, 512]), self.S("OB", [SEQ, 512])]
        DN = self.S("DN_T", [512, SEQ], BF16)
        gb = self.gbtok
        ident = self.ident
        ones = self.ones
        dmask = k.sb("dmask", [64, 4, 64], F32)
        k.load(dmask, dmask[:, :, :], self.I("dmask", [64, 4, 64]))
        qt = [k.sb("qt%d" % i, [128, 12, 512], F32) for i in range(2)]
        qt16 = [k.sb("qt16_%d" % i, [128, 12, 512], BF16) for i in range(2)]
        S = [[k.sb("S%d_%d" % (d, h), [128, 128], F32) for h in range(4)] for d in range(2)]
        ob = [[k.sb("ob%d_%d" % (d, i), [64, 4, 128], F32) for i in range(2)] for d in range(2)]

        def mk(tag):
            d_ = {}
            big = k.sb("wk_%s" % tag, [128, 1936], F32)
            c = 0
            for nm, shp in (("gB", [64, 128]), ("bB", [64, 64]), ("E", [64, 64]), ("DTm", [64, 64]), ("DTs", [64, 64]),
                            ("Bm", [64, 64]), ("Am", [64, 64]), ("QKm", [64, 64]), ("PQ0", [64, 128]), ("PQ1", [64, 128]),
                            ("TTA0", [64, 128]), ("TTA1", [64, 128]), ("ru", [64, 128]), ("rw", [64, 128]), ("kt", [64, 128]),
                            ("U", [64, 128]), ("Vn", [64, 128]), ("O2", [64, 128]), ("WT", [128, 64]), ("sc", [128, 8])):
                d_[nm] = SubBuf(big, shp[0], c, shp[1])
                c += shp[1]
            big16 = k.sb("wk16_%s" % tag, [128, 512], BF16)
            c = 0
            for nm, shp in (("WT16", [128, 64]), ("S16", [128, 128]), ("kt16", [64, 128]), ("QKm16", [64, 64]), ("Vn16", [64, 128])):
                d_[nm] = SubBuf(big16, shp[0], c, shp[1])
                c += shp[1]
            return d_
        W = [[mk("%d%d" % (d, h)) for h in range(4)] for d in range(2)]
        sc_dk = 128.0 ** -0.5

        def sched_of(d):
            sched = []
            cchunks = list(range(4))
            mchunks = list(range(64))
            if d == 1:
                cchunks.reverse()
                mchunks.reverse()
            for n in cchunks:
                sched.append((False, 0, n, n, n * 64))
            for n in mchunks:
                sched.append((True, n // 8, n % 8, 4 + n, n * 64))
            return sched

        def chain(d, h):
            w = W[d][h]
            Sh = S[d][h]
            MI = dmask[:, 2 * d, :]
            MS = dmask[:, 2 * d + 1, :]
            tb = qt[d]
            tb16 = qt16[d]
            pb = k.ps[d * 4 + h]
            k.op("pool", lambda e: e.memset(Sh[:, :], 0.0), [], [Sh])
            k.op("dve", lambda e: e.memset(w["S16"][:, :], 0.0), [], [w["S16"]])
            cur_tile = None
            for si, (is_main, ti, cin, gi, tok0) in enumerate(sched_of(d)):
                key = (is_main, ti)
                if key != cur_tile:
                    cur_tile = key
                    if h == 0:
                        if is_main:
                            k.load(tb, tb[:, :, :], QN[:, ti * 512:(ti + 1) * 512].rearrange("(c p) t -> p c t", p=128), q="sp")
                            wdt = 512
                        else:
                            k.load(tb, tb[:, :, 0:CTX], QNc.rearrange("(c p) t -> p c t", p=128), q="sp")
                            wdt = CTX
                        k.op("act", lambda e: e.copy(out=tb16[:, 0:4, 0:wdt], in_=tb[:, 0:4, 0:wdt]), [tb], [tb16])
                        k.op("dve", lambda e: e.tensor_copy(out=tb16[:, 4:8, 0:wdt], in_=tb[:, 4:8, 0:wdt]), [tb], [tb16])
                c0 = cin * 64
                oc = ob[d][si % 2]
                QT = tb[:, h, c0:c0 + 64]
                KT = tb[:, 4 + h, c0:c0 + 64]
                VT = tb[:, 8 + h, c0:c0 + 64]
                QT16 = tb16[:, h, c0:c0 + 64]
                KT16 = tb16[:, 4 + h, c0:c0 + 64]
                gcol = gb[:, gi, d * 4 + h:d * 4 + h + 1]
                bcol = gb[:, gi, 8 + d * 4 + h:8 + d * 4 + h + 1]
                sc = w["sc"]
                steps = [(pb[0:64, 256:320], KT16, KT16, True, True)]
                if is_main:
                    steps.append((pb[0:64, 320:384], KT16, QT16, True, True))
                k.mm(pb, steps, [tb16])
                k.tr(pb, [(pb[0:64, 0:128], KT), (pb[0:64, 128:256], VT)], [tb], ident)
                k.op("dve", lambda e: e.tensor_scalar(out=w["gB"][:, :], in0=ones[0:64, 0:128], scalar1=gcol, scalar2=None, op0=ALU.mult), [ones, gb], [w["gB"]])
                k.op("pool", lambda e: e.tensor_scalar(out=w["bB"][:, :], in0=ones[0:64, 0:64], scalar1=bcol, scalar2=None, op0=ALU.mult), [ones, gb], [w["bB"]])
                yield
                k.op("act", lambda e: e.activation(out=w["kt"][:, :], in_=pb[0:64, 0:128], func=AF.Copy), [pb], [w["kt"]])
                k.op("dve", lambda e: e.tensor_copy(out=w["U"][:, :], in_=pb[0:64, 128:256]), [pb], [w["U"]])
                yield
                k.mm(pb, [(pb[0:128, 384:448], w["gB"][:, :], MI, True, True),
                          (pb[0:64, 448:512], w["bB"][:, :], ident[0:64, 0:64], True, True),
                          (pb[0:64, 0:1], MI, gcol, True, True)], [w["gB"], w["bB"], dmask, ident, ones, gb])
                lastc = 384 + (63 if d == 0 else 0)
                yield
                k.op("dve", lambda e: e.tensor_copy(out=sc[:, 1:2], in_=pb[:, lastc:lastc + 1]), [pb], [sc])
                k.op("dve", lambda e: e.tensor_copy(out=sc[0:64, 0:1], in_=pb[0:64, 0:1]), [pb], [sc])
                yield
                k.op("dve", lambda e: e.tensor_scalar(out=w["E"][:, :], in0=pb[0:64, 384:448], scalar1=sc[0:64, 0:1], scalar2=0.0, op0=ALU.subtract, op1=ALU.min), [pb, sc], [w["E"]])
                k.op("act", lambda e: e.activation(out=sc[0:64, 2:3], in_=sc[0:64, 0:1], func=AF.Exp), [sc], [sc])
                k.op("act", lambda e: e.activation(out=sc[0:64, 3:4], in_=sc[0:64, 0:1], func=AF.Exp, bias=sc[0:64, 1:2], scale=-1.0), [sc], [sc])
                k.op("act", lambda e: e.activation(out=sc[:, 4:5], in_=sc[:, 1:2], func=AF.Exp), [sc], [sc])
                yield
                k.op("act", lambda e: e.activation(out=w["E"][:, :], in_=w["E"][:, :], func=AF.Exp), [w["E"]], [w["E"]])
                k.op("dve", lambda e: e.tensor_tensor(out=sc[0:64, 5:6], in0=sc[0:64, 2:3], in1=bcol, op=ALU.mult), [sc, gb], [sc])
                k.op("dve", lambda e: e.tensor_scalar(out=sc[0:64, 6:7], in0=sc[0:64, 2:3], scalar1=sc_dk, scalar2=None, op0=ALU.mult), [sc], [sc])
                k.op("dve", lambda e: e.tensor_scalar(out=w["ru"][:, :], in0=w["U"][:, :], scalar1=bcol, scalar2=None, op0=ALU.mult), [w["U"], gb], [w["ru"]])
                yield
                k.op("pool", lambda e: e.tensor_tensor(out=w["DTs"][:, :], in0=w["E"][:, :], in1=MS, op=ALU.mult), [w["E"], dmask], [w["DTs"]])
                if is_main:
                    k.op("pool", lambda e: e.tensor_tensor(out=w["DTm"][:, :], in0=w["E"][:, :], in1=MI, op=ALU.mult), [w["E"], dmask], [w["DTm"]])
                k.op("dve", lambda e: e.tensor_scalar(out=w["rw"][:, :], in0=w["kt"][:, :], scalar1=sc[0:64, 5:6], scalar2=None, op0=ALU.mult), [w["kt"], sc], [w["rw"]])
                yield
                k.op("act", lambda e: e.activation(out=w["kt16"][:, :], in_=w["kt"][:, :], func=AF.Copy, scale=sc[0:64, 3:4]), [w["kt"], sc], [w["kt16"]])
                k.op("dve", lambda e: e.tensor_tensor(out=w["Bm"][:, :], in0=pb[0:64, 448:512], in1=w["DTs"][:, :], op=ALU.mult), [pb, w["DTs"]], [w["Bm"]])
                if is_main:
                    k.op("dve", lambda e: e.scalar_tensor_tensor(out=w["QKm16"][:, :], in0=pb[0:64, 320:384], scalar=sc_dk, in1=w["DTm"][:, :], op0=ALU.mult, op1=ALU.mult), [pb, w["DTm"]], [w["QKm16"]])
                yield
                k.op("dve", lambda e: e.tensor_tensor(out=w["Bm"][:, :], in0=pb[0:64, 256:320], in1=w["Bm"][:, :], op=ALU.mult), [pb, w["Bm"]], [w["Bm"]])
                yield
                k.tr(pb, [(pb[0:64, 0:64], w["Bm"][:, :])], [w["Bm"]], ident)
                tta = w["TTA0"]
                k.op("dve", lambda e: e.tensor_tensor(out=tta[:, 0:64], in0=ident[0:64, 0:64], in1=w["Bm"][:, :], op=ALU.subtract), [ident, w["Bm"]], [tta])
                yield
                k.op("act", lambda e: e.copy(out=w["Am"][:, :], in_=pb[0:64, 0:64]), [pb], [w["Am"]])
                yield
                k.op("pool", lambda e: e.tensor_tensor(out=tta[:, 64:128], in0=ident[0:64, 0:64], in1=w["Am"][:, :], op=ALU.subtract), [ident, w["Am"]], [tta])
                pq_p, pq_q = w["Am"][:, :], w["Bm"][:, :]
                pqb = [w["Am"], w["Bm"]]
                for lev in range(1, 6):
                    last = lev == 5
                    pqn = w["PQ%d" % (lev % 2)]
                    steps = [(pb[0:64, 64:128], pq_p, pq_q, True, True)]
                    if not last:
                        steps.append((pb[0:64, 0:64], pq_q, pq_p, True, True))
                    k.mm(pb, steps, pqb)
                    yield
                    if last:
                        k.op("act", lambda e: e.copy(out=pqn[:, 64:128], in_=pb[0:64, 64:128]), [pb], [pqn])
                    else:
                        k.op("act", lambda e: e.copy(out=pqn[:, :], in_=pb[0:64, 0:128]), [pb], [pqn])
                    yield
                    ttn = w["TTA%d" % (lev % 2)]
                    steps = [(pb[0:64, 128:192], tta[:, 64:128], pqn[:, 64:128], True, True)]
                    if not last:
                        steps.append((pb[0:64, 192:256], tta[:, 0:64], pqn[:, 0:64], True, True))
                    k.mm(pb, steps, [tta, pqn])
                    yield
                    if last:
                        k.op("dve", lambda e: e.tensor_tensor(out=ttn[:, 0:64], in0=pb[0:64, 128:192], in1=tta[:, 0:64], op=ALU.add), [pb, tta], [ttn])
                    else:
                        k.op("dve", lambda e: e.tensor_tensor(out=ttn[:, :], in0=pb[0:64, 128:256], in1=tta[:, :], op=ALU.add), [pb, tta], [ttn])
                    yield
                    tta = ttn
                    pq_p, pq_q = pqn[:, 0:64], pqn[:, 64:128]
                    pqb = [pqn]
                TT = tta[:, 0:64]
                k.mm(pb, [(pb[0:64, 256:384], TT, w["ru"][:, :], True, True), (pb[0:128, 384:448], w["rw"][:, :], TT, True, True)], [tta, w["ru"], w["rw"]])
                yield
                k.op("act", lambda e: e.copy(out=w["WT16"][:, :], in_=pb[0:128, 384:448]), [pb], [w["WT16"]])
                k.op("dve", lambda e: e.tensor_copy(out=w["U"][:, :], in_=pb[0:64, 256:384]), [pb], [w["U"]])
                yield
                steps = [(pb[0:64, 0:128], w["WT16"][:, :], w["S16"][:, :], True, True)]
                if is_main:
                    steps.append((pb[0:64, 128:256], QT16, w["S16"][:, :], True, True))
                k.mm(pb, steps, [w["WT16"], w["S16"], tb16])
                yield
                k.op("dve", lambda e: e.tensor_tensor(out=w["Vn16"][:, :], in0=w["U"][:, :], in1=pb[0:64, 0:128], op=ALU.subtract), [w["U"], pb], [w["Vn16"]])
                yield
                steps = [(pb[0:128, 256:384], w["kt16"][:, :], w["Vn16"][:, :], True, True)]
                if is_main:
                    steps.append((pb[0:64, 384:512], w["QKm16"][:, :], w["Vn16"][:, :], True, True))
                k.mm(pb, steps, [w["kt16"], w["Vn16"], w["QKm16"]])
                yield
                k.op("dve", lambda e: e.scalar_tensor_tensor(out=Sh[:, :], in0=Sh[:, :], scalar=sc[:, 4:5], in1=pb[0:128, 256:384], op0=ALU.mult, op1=ALU.add), [Sh, sc, pb], [Sh])
                k.op("act", lambda e: e.copy(out=w["S16"][:, :], in_=Sh[:, :]), [Sh], [w["S16"]])
                if is_main:
                    k.op("act", lambda e: e.copy(out=w["O2"][:, :], in_=pb[0:64, 384:512]), [pb], [w["O2"]])
                    yield
                    k.op("dve", lambda e: e.scalar_tensor_tensor(out=oc[:, h, :], in0=pb[0:64, 128:256], scalar=sc[0:64, 6:7], in1=w["O2"][:, :], op0=ALU.mult, op1=ALU.add), [pb, sc, w["O2"]], [oc])
                    if h == 3:
                        k.store(oc, OFB[d][tok0:tok0 + 64, :].rearrange("t (h v) -> t h v", h=4), oc[:, :, :], q="sp")
                yield

        import os
        IL = int(os.environ.get("DELTA_IL", "8"))
        allg = [chain(d, h) for h in range(4) for d in range(2)]
        if IL >= 8:
            groups = [allg]
        elif IL >= 4:
            groups = [[g_ for i_, g_ in enumerate(allg) if i_ % 2 == 0], [g_ for i_, g_ in enumerate(allg) if i_ % 2 == 1]]
        else:
            groups = None
        if groups is None:
            for g_ in allg:
                for _ in g_:
                    pass
        else:
            nround = 0
            for gens in groups:
                gens = list(gens)
                while gens:
                    nround += 1
                    if nround % 8 == 0 and hasattr(self, "wc_todo"):
                        self.wcast_tick()
                    for g_ in list(gens):
                        try:
                            next(g_)
                        except StopIteration:
                            gens.remove(g_)
        k.end()
        if os.environ.get("DELTA_NOCOMBINE"):
            return
        k.begin()
        og = k.sb("og", [128, 128], F32)
        k.load(og, og[:, :], self.I("onorm_g", [128, 128]))
        fo = [k.sb("fo%d" % i, [128, 4, 512], F32) for i in range(2)]
        bo = [k.sb("bo%d" % i, [128, 4, 512], F32) for i in range(2)]
        zz = [k.sb("zz%d" % i, [128, 4, 512], F32) for i in range(2)]
        dnT = [k.sb("dnT%d" % i, [128, 4, 512], BF16) for i in range(2)]
        ss = k.sb("ss", [128, 32], F32)
        junk = k.sb("junk", [128, 128], F32)
        for i in range(NT):
            f_, b_, z_, dn_ = fo[i % 2], bo[i % 2], zz[i % 2], dnT[i % 2]
            rows = slice(i * T, (i + 1) * T)
            k.load(f_, f_[:, :, :], OFB[0][rows, :].rearrange("(j p) d -> p j d", p=128), q="sp")
            k.load(b_, b_[:, :, :], OFB[1][rows, :].rearrange("(j p) d -> p j d", p=128), q="pool")
            k.load(z_, z_[:, :, :], Z[rows, :].rearrange("(j p) d -> p j d", p=128), q="sp")
            k.op("pool", lambda e, f_=f_, b_=b_: e.tensor_tensor(out=f_[:, :, :], in0=f_[:, :, :], in1=b_[:, :, :], op=ALU.add), [f_, b_], [f_])
            for j in range(4):
                for h in range(4):
                    k.op("act", lambda e, f_=f_, j=j, h=h: e.activation(out=junk[:, :], in_=f_[:, j, h * 128:(h + 1) * 128], func=AF.Square,
                                                                       accum_out=ss[:, j * 4 + h:j * 4 + h + 1]), [f_], [junk, ss])
            k.op("act", lambda e: e.activation(out=ss[:, 16:32], in_=ss[:, 0:16], func=AF.Sqrt, bias=self.eps_c[:, 0:1], scale=1.0 / 128), [ss, self.eps_c], [ss])
            k.op("dve", lambda e: e.reciprocal(out=ss[:, 16:32], in_=ss[:, 16:32]), [ss], [ss])
            for j in range(4):
                for h in range(4):
                    k.op("dve", lambda e, f_=f_, j=j, h=h: e.scalar_tensor_tensor(out=f_[:, j, h * 128:(h + 1) * 128], in0=f_[:, j, h * 128:(h + 1) * 128],
                                                                                 scalar=ss[:, 16 + j * 4 + h:17 + j * 4 + h], in1=og[:, :], op0=ALU.mult, op1=ALU.mult), [f_, ss, og], [f_])
            k.op("pool", lambda e, f_=f_, z_=z_: e.tensor_tensor(out=f_[:, :, :], in0=f_[:, :, :], in1=z_[:, :, :], op=ALU.mult), [f_, z_], [f_])
            for j in range(4):
                bt = k.bank()
                k.tr(bt, [(bt[:, h * 128:(h + 1) * 128], f_[:, j, h * 128:(h + 1) * 128]) for h in range(4)], [f_], ident)
                k.cp("act" if j % 2 else "dve", dn_, dn_[:, :, j * 128:(j + 1) * 128], bt, bt[:, :].rearrange("p (h t) -> p h t", h=4))
            k.store(dn_, DN[:, rows].rearrange("(h p) t -> p h t", p=128), dn_[:, :, :])
        k.end()

    def stage_fnet(self):
        k = self.k
        k.begin()
        GC = self.S("GC", [SEQ, 512], BF16)
        GS = self.S("GS", [SEQ, 512], BF16)
        FN = self.S("FN_T", [512, SEQ], BF16)
        dc = self.I("dft_c", [SEQ, SEQ], BF16)
        dsn = self.I("dft_s", [SEQ, SEQ], BF16)
        gc = k.sb("gc", [128, 32, 512], BF16)
        gs = k.sb("gs", [128, 32, 512], BF16)
        for q4 in range(4):
            k.load(gc, gc[:, q4 * 8:(q4 + 1) * 8, :], GC[q4 * 1024:(q4 + 1) * 1024, :].rearrange("(c p) n -> p c n", p=128), q="sp")
            k.load(gs, gs[:, q4 * 8:(q4 + 1) * 8, :], GS[q4 * 1024:(q4 + 1) * 1024, :].rearrange("(c p) n -> p c n", p=128), q="pool")
        pan = [k.sb("pan%d" % i, [128, 32, 512], BF16) for i in range(3)]
        fo = [k.sb("fo%d" % i, [128, 4, 512], BF16) for i in range(2)]
        scale = float(1.0 / np.sqrt(SEQ * 128.0))
        npan = 0
        for ft in range(8):
            ps_ = []
            for (tab, q_) in ((dc, "sp"), (dsn, "pool")):
                p_ = pan[npan % 3]
                npan += 1
                for hlf in range(2):
                    k.load(p_, p_[:, hlf * 16:(hlf + 1) * 16, :], tab[hlf * 2048:(hlf + 1) * 2048, ft * 512:(ft + 1) * 512].rearrange("(c p) n -> p c n", p=128), q=q_)
                ps_.append(p_)
            c_, s_ = ps_
            f = fo[ft % 2]
            for g in range(4):
                bank = k.bank()
                steps = []
                for tc in range(32):
                    steps.append((bank[:, :], gc[:, tc, g * 128:(g + 1) * 128], c_[:, tc, :], tc == 0, False))
                for tc in range(32):
                    steps.append((bank[:, :], gs[:, tc, g * 128:(g + 1) * 128], s_[:, tc, :], False, tc == 31))
                k.mm(bank, steps, [gc, gs, c_, s_])
                k.op("act", lambda e, f=f, g=g, bank=bank: e.activation(out=f[:, g, :], in_=bank[:, :], func=AF.Copy, scale=scale), [bank], [f])
            k.store(f, FN[:, ft * 512:(ft + 1) * 512].rearrange("(g p) n -> p g n", p=128), f[:, :, :])
        k.end()

    def stage_mix(self):
        k = self.k
        k.begin()
        XT = self.S("XT", [D, SEQ])
        DN = self.S("DN_T", [512, SEQ], BF16)
        FN = self.S("FN_T", [512, SEQ], BF16)
        stg = [k.sb("stg%d" % i, [128, 8, 256], F32) for i in range(2)]
        wo = k.sb("wo", [128, 8, D], BF16)
        self.load_w16(wo, self.I("w_out", [D, D]), 8, D, stg)
        mx = [k.sb("mx%d" % i, [128, 8, T], BF16) for i in range(2)]
        xt = [k.sb("xt%d" % i, [128, 8, T], F32) for i in range(2)]
        cf = self.coef
        for i in range(NT):
            m, x_ = mx[i % 2], xt[i % 2]
            k.load(m, m[:, 0:4, :], DN[:, i * T:(i + 1) * T].rearrange("(c p) t -> p c t", p=128), q="sp")
            k.load(m, m[:, 4:8, :], FN[:, i * T:(i + 1) * T].rearrange("(c p) t -> p c t", p=128), q="sp")
            k.load(x_, x_[:, :, :], XT[:, i * T:(i + 1) * T].rearrange("(c p) t -> p c t", p=128), q="pool")
            for oc in range(8):
                bank = k.bank()
                k.mm(bank, [(bank[:, :], wo[:, kc, oc * 128:(oc + 1) * 128], m[:, kc, :], kc == 0, kc == 7) for kc in range(8)], [wo, m])
                k.op("dve", lambda e, x_=x_, oc=oc, bank=bank: e.scalar_tensor_tensor(out=x_[:, oc, :], in0=bank[:, :], scalar=cf[:, 0, 2, oc:oc + 1],
                                                                                     in1=x_[:, oc, :], op0=ALU.mult, op1=ALU.add), [bank, cf, x_], [x_])
            k.store(x_, XT[:, i * T:(i + 1) * T].rearrange("(c p) t -> p c t", p=128), x_[:, :, :])
        k.end()

    def stage_moe(self, l):
        k = self.k
        k.begin()
        XT = self.S("XT", [D, SEQ])
        wg_d = self.I("moe_wg", [2, NEXP, D, FF])
        wu_d = self.I("moe_wu", [2, NEXP, D, FF])
        wd_d = self.I("moe_wd", [2, NEXP, FF, D])
        cf = self.coef
        ST = 2048
        wr = k.sb("wr", [128, 8, 32], F32)
        k.load(wr, wr[:, :, :], self.I("router_w", [D, 32]).rearrange("(k p) n -> p k n", p=128))
        rb = k.sb("rb", [128, 128], F32)
        k.load(rb, rb[:, :], self.I("rbias", [128, 128]))
        selb = [k.sb("selm%d" % i, [32, 128], F32) for i in range(2)]
        tT = k.sb("tT", [128, 8, ST], BF16)
        yacc = k.sb("yacc", [128, 8, ST], F32)
        combT = k.sb("combT", [32, ST], F32)
        wgb = [k.sb("wg%d" % i, [128, 8, FF], BF16) for i in range(2)]
        wub = [k.sb("wu%d" % i, [128, 8, FF], BF16) for i in range(2)]
        wdb = [k.sb("wd%d" % i, [128, 4, D], BF16) for i in range(2)]
        cb = [k.sb("cb%d" % i, [128, T], F32) for i in range(2)]
        sgb = [k.sb("sg%d" % i, [128, T], F32) for i in range(2)]
        t1b = [k.sb("t1%d" % i, [128, T], F32) for i in range(2)]
        hid = [k.sb("hid%d" % i, [128, 4, T], BF16) for i in range(2)]
        rstd = k.sb("rstd", [128, T], F32)
        R = {}
        for nm, shp in (("sc", [128, 128]), ("sel", [128, 128]), ("sel2", [128, 128]), ("eq", [128, 32]), ("m1", [128, 32]), ("m2", [128, 32]),
                        ("gs", [128, 32]), ("gmax", [128, 4]), ("ghot", [128, 32]), ("mask", [128, 128]), ("den", [128, 4]), ("comb", [128, 128])):
            R[nm] = k.sb("r_" + nm, shp, F32)
        xt = Buf(yacc.t, "yv")
        for st in range(SEQ // ST):
            for tt in range(4):
                tok = slice(st * ST + tt * T, st * ST + (tt + 1) * T)
                xv = yacc[:, :, 0:T]
                t32 = yacc[:, :, T:2 * T]
                sq = yacc[:, :, 2 * T:3 * T]
                k.load(yacc, xv, XT[:, tok].rearrange("(c p) t -> p c t", p=128))
                k.op("act", lambda e, sq=sq, xv=xv: e.activation(out=sq, in_=xv, func=AF.Square), [yacc], [yacc])
                bank = k.bank()
                k.mm(bank, [(bank[:, :], self.ones[:, :], yacc[:, c, 2 * T:3 * T], c == 0, c == 7) for c in range(8)], [self.ones, yacc])
                k.op("act", lambda e, bank=bank: e.activation(out=rstd[:, :], in_=bank[:, :], func=AF.Sqrt, bias=self.eps_c[:, 0:1], scale=1.0 / D), [bank, self.eps_c], [rstd])
                k.op("dve", lambda e: e.reciprocal(out=rstd[:, :], in_=rstd[:, :]), [rstd], [rstd])
                for c in range(8):
                    k.op("dve", lambda e, c=c: e.scalar_tensor_tensor(out=yacc[:, c, 2 * T:3 * T], in0=yacc[:, c, 0:T], scalar=cf[:, l, 3, c:c + 1],
                                                                     in1=rstd[:, :], op0=ALU.mult, op1=ALU.mult), [yacc, rstd, cf], [yacc])
                for c in range(8):
                    k.op("act", lambda e, c=c: e.activation(out=yacc[:, c, T:2 * T], in_=yacc[:, c, 2 * T:3 * T], func=AF.Identity,
                                                           bias=cf[:, l, 4, c:c + 1], scale=1.0), [yacc, cf], [yacc])
                k.op("pool", lambda e, tt=tt, t32=t32: e.tensor_copy(out=tT[:, :, tt * T:(tt + 1) * T], in_=t32), [yacc], [tT])
                bank = k.bank()
                steps = []
                for j in range(4):
                    for kc in range(8):
                        steps.append((bank[:, j * 32:(j + 1) * 32], yacc[:, kc, T + j * 128:T + (j + 1) * 128], wr[:, kc, :], kc == 0, kc == 7))
                k.mm(bank, steps, [yacc, wr])
                sc, sel, sel2, eq, m1, m2, gsm, gmax, ghot, mask, den, comb = (R[n] for n in ("sc", "sel", "sel2", "eq", "m1", "m2", "gs", "gmax", "ghot", "mask", "den", "comb"))
                k.op("act", lambda e, bank=bank: e.activation(out=sc[:, :], in_=bank[:, 0:128], func=AF.Sigmoid), [bank], [sc])
                k.op("dve", lambda e: e.tensor_tensor(out=sel[:, :], in0=sc[:, :], in1=rb[:, :], op=ALU.add), [sc, rb], [sel])
                v4 = lambda b_: b_[:, :].rearrange("p (a i) -> p a i", i=4)
                k.op("dve", lambda e: e.tensor_reduce(out=m1[:, :], in_=v4(sel), axis=AX.X, op=ALU.max), [sel], [m1])
                for i4 in range(4):
                    k.op("dve", lambda e, i4=i4: e.tensor_tensor(out=eq[:, :], in0=v4(sel)[:, :, i4], in1=m1[:, :], op=ALU.is_equal), [sel, m1], [eq])
                    k.op("dve", lambda e, i4=i4: e.scalar_tensor_tensor(out=v4(sel2)[:, :, i4], in0=eq[:, :], scalar=-1.0e9, in1=v4(sel)[:, :, i4],
                                                                       op0=ALU.mult, op1=ALU.add), [eq, sel], [sel2])
                k.op("dve", lambda e: e.tensor_reduce(out=m2[:, :], in_=v4(sel2), axis=AX.X, op=ALU.max), [sel2], [m2])
                k.op("dve", lambda e: e.tensor_tensor(out=gsm[:, :], in0=m1[:, :], in1=m2[:, :], op=ALU.add), [m1, m2], [gsm])
                v8 = lambda b_: b_[:, :].rearrange("p (j g) -> p j g", g=8)
                k.op("dve", lambda e: e.tensor_reduce(out=gmax[:, :], in_=v8(gsm), axis=AX.X, op=ALU.max), [gsm], [gmax])
                for g8 in range(8):
                    k.op("dve", lambda e, g8=g8: e.tensor_tensor(out=v8(ghot)[:, :, g8], in0=v8(gsm)[:, :, g8], in1=gmax[:, :], op=ALU.is_equal), [gsm, gmax], [ghot])
                for i4 in range(4):
                    k.op("dve", lambda e, i4=i4: e.tensor_tensor(out=eq[:, :], in0=v4(sel)[:, :, i4], in1=m2[:, :], op=ALU.is_ge), [sel, m2], [eq])
                    k.op("dve", lambda e, i4=i4: e.tensor_tensor(out=v4(mask)[:, :, i4], in0=eq[:, :], in1=ghot[:, :], op=ALU.mult), [eq, ghot], [mask])
                k.op("dve", lambda e: e.tensor_tensor(out=mask[:, :], in0=mask[:, :], in1=sc[:, :], op=ALU.mult), [mask, sc], [mask])
                v32 = lambda b_: b_[:, :].rearrange("p (j e) -> p j e", e=32)
                k.op("dve", lambda e: e.tensor_reduce(out=den[:, :], in_=v32(mask), axis=AX.X, op=ALU.add), [mask], [den])
                k.op("dve", lambda e: e.reciprocal(out=den[:, :], in_=den[:, :]), [den], [den])
                for j in range(4):
                    k.op("dve", lambda e, j=j: e.tensor_scalar(out=comb[:, j * 32:(j + 1) * 32], in0=mask[:, j * 32:(j + 1) * 32], scalar1=den[:, j:j + 1],
                                                              scalar2=None, op0=ALU.mult), [mask, den], [comb])
                bank = k.bank()
                k.tr(bank, [(bank[0:32, j * 128:(j + 1) * 128], comb[:, j * 32:(j + 1) * 32]) for j in range(4)], [comb], self.ident)
                k.op("act", lambda e, bank=bank, tt=tt: e.copy(out=combT[:, tt * T:(tt + 1) * T], in_=bank[0:32, :]), [bank], [combT])
            if "combT" in self.dbgset:
                k.store(combT, self.S("combT", [2, 32, ST])[st], combT[:, :])
            for e_ in range(NEXP):
                wg, wu, wd = wgb[e_ % 2], wub[e_ % 2], wdb[e_ % 2]
                k.load(wg, wg[:, :, :], wg_d[l, e_].rearrange("(k p) n -> p k n", p=128), q="pool")
                k.load(wu, wu[:, :, :], wu_d[l, e_].rearrange("(k p) n -> p k n", p=128), q="pool")
                k.load(wd, wd[:, :, :], wd_d[l, e_].rearrange("(k p) n -> p k n", p=128), q="pool")
                selm = selb[e_ % 2]
                k.op("pool", lambda e, selm=selm, e_=e_: e.tensor_scalar(out=selm[:, :], in0=self.ones[0:32, :], scalar1=self.ident[0:32, e_:e_ + 1],
                                                                        scalar2=None, op0=ALU.mult), [self.ones, self.ident], [selm])
                for tt in range(4):
                    n = e_ * 4 + tt
                    c_ = cb[n % 2]
                    bank = k.bank()
                    k.mm(bank, [(bank[:, :], selm[:, :], combT[:, tt * T:(tt + 1) * T], True, True)], [selm, combT])
                    k.op("act", lambda e, c_=c_, bank=bank: e.copy(out=c_[:, :], in_=bank[:, :]), [bank], [c_])
                    h_ = hid[n % 2]
                    for fc in range(4):
                        m = n * 4 + fc
                        bg = k.bank()
                        k.mm(bg, [(bg[:, :], wg[:, kc, fc * 128:(fc + 1) * 128], tT[:, kc, tt * T:(tt + 1) * T], kc == 0, kc == 7) for kc in range(8)], [wg, tT])
                        bu = k.bank()
                        k.mm(bu, [(bu[:, :], wu[:, kc, fc * 128:(fc + 1) * 128], tT[:, kc, tt * T:(tt + 1) * T], kc == 0, kc == 7) for kc in range(8)], [wu, tT])
                        sg, t1 = sgb[m % 2], t1b[m % 2]
                        k.op("act", lambda e, sg=sg, bg=bg: e.activation(out=sg[:, :], in_=bg[:, :], func=AF.Silu), [bg], [sg])
                        k.op("dve", lambda e, t1=t1, bu=bu, c_=c_: e.tensor_tensor(out=t1[:, :], in0=bu[:, :], in1=c_[:, :], op=ALU.mult), [bu, c_], [t1])
                        k.op("pool", lambda e, h_=h_, fc=fc, sg=sg, t1=t1: e.tensor_tensor(out=h_[:, fc, :], in0=sg[:, :], in1=t1[:, :], op=ALU.mult), [sg, t1], [h_])
                    for oc in range(8):
                        bd = k.bank()
                        k.mm(bd, [(bd[:, :], wd[:, fc, oc * 128:(oc + 1) * 128], h_[:, fc, :], fc == 0, fc == 3) for fc in range(4)], [wd, h_])
                        if e_ == 0:
                            k.op("dve", lambda e, oc=oc, tt=tt, bd=bd: e.tensor_copy(out=yacc[:, oc, tt * T:(tt + 1) * T], in_=bd[:, :]), [bd], [yacc])
                        else:
                            k.op("dve", lambda e, oc=oc, tt=tt, bd=bd: e.tensor_tensor(out=yacc[:, oc, tt * T:(tt + 1) * T], in0=bd[:, :],
                                                                                      in1=yacc[:, oc, tt * T:(tt + 1) * T], op=ALU.add), [bd, yacc], [yacc])
            xr = [k.sb("xr%d_%d" % (st, i), [128, 8, T], F32) for i in range(1)] if st == 0 else xr
            for tt in range(4):
                tok = slice(st * ST + tt * T, st * ST + (tt + 1) * T)
                x_ = xr[0]
                k.load(x_, x_[:, :, :], XT[:, tok].rearrange("(c p) t -> p c t", p=128))
                for oc in range(8):
                    k.op("dve", lambda e, x_=x_, oc=oc, tt=tt: e.scalar_tensor_tensor(out=x_[:, oc, :], in0=yacc[:, oc, tt * T:(tt + 1) * T], scalar=cf[:, l, 5, oc:oc + 1],
                                                                                     in1=x_[:, oc, :], op0=ALU.mult, op1=ALU.add), [yacc, cf, x_], [x_])
                k.store(x_, XT[:, tok].rearrange("(c p) t -> p c t", p=128), x_[:, :, :])
            k.barrier()
        k.end()

    def stage_moe_r(self, l):
        k = self.k
        I32 = mybir.dt.int32
        NS = 8192
        XT = self.S("XT", [D, SEQ])
        TTOK = self.S("T_tok", [SEQ, D])
        TS = self.S("TS", [NS, D])
        YS = self.S("YS", [NS, D])
        W4S = self.S("W4S", [NS, 128])
        wgl, wul, wdl = self.w16
        cf = self.coef
        k.begin()
        wr = k.sb("wr", [128, 8, 32], F32)
        k.load(wr, wr[:, :, :], self.I("router_w", [D, 32]).rearrange("(k p) n -> p k n", p=128))
        rb = k.sb("rb", [128, 128], F32)
        k.load(rb, rb[:, :], self.I("rbias", [128, 128]))
        mc = k.sb("mc", [128, 129], F32)
        k.load(mc, mc[:, :], self.I("mconst", [128, 129]))
        H = k.sb("H", [128, 32, 8], F32)
        W4 = k.sb("W4", [128, 32, 4], F32)
        RK = k.sb("RK", [128, 32, 8], F32)
        TOT = k.sb("TOT", [128, 32, 8], F32)
        PRE = k.sb("PRE", [128, 32, 8], F32)
        sm = k.sb("sm", [128, 64], F32)
        SLOTF = k.sb("SLOTF", [128, 32], F32)
        SLOTI = self.SLOTI_p
        GID = k.sb("GID", [128, 16], F32)
        IDXF = k.sb("IDXF", [128, 16, 4], F32)
        IDXI = self.IDXI_p
        xv = k.sb("xv", [128, 8, T], F32)
        t32 = k.sb("t32", [128, 8, T], F32)
        sq = k.sb("sq", [128, 8, T], F32)
        rstd = k.sb("rstd", [128, T], F32)
        ttok = [k.sb("ttok%d" % i, [128, 4, D], F32) for i in range(2)]
        R = {}
        for nm, shp in (("sc", [128, 128]), ("sel", [128, 128]), ("sel2", [128, 128]), ("eq", [128, 32]), ("m1", [128, 32]), ("m2", [128, 32]),
                        ("gs", [128, 32]), ("gmax", [128, 4]), ("ghot", [128, 32]), ("mask", [128, 128]), ("den", [128, 4]), ("comb", [128, 128])):
            R[nm] = k.sb("r_" + nm, shp, F32)
        sc, sel, sel2, eq, m1, m2, gsm, gmax, ghot, mask, den, comb = (R[n] for n in ("sc", "sel", "sel2", "eq", "m1", "m2", "gs", "gmax", "ghot", "mask", "den", "comb"))
        v4 = lambda b_: b_[:, :].rearrange("p (a i) -> p a i", i=4)
        v8 = lambda b_: b_[:, :].rearrange("p (j g) -> p j g", g=8)
        v32 = lambda b_: b_[:, :].rearrange("p (j e) -> p j e", e=32)
        for tt in range(NT):
            tok = slice(tt * T, (tt + 1) * T)
            k.load(xv, xv[:, :, :], XT[:, tok].rearrange("(c p) t -> p c t", p=128))
            self.rstd_fm(xv, 8, T, sq, rstd, 1.0 / D)
            self.modulate(xv, rstd, cf[:, l, 3, :], cf, cf[:, l, 4, :], cf, sq, t32, T)
            bank = k.bank()
            steps = []
            for j in range(4):
                for kc in range(8):
                    steps.append((bank[:, j * 32:(j + 1) * 32], t32[:, kc, j * 128:(j + 1) * 128], wr[:, kc, :], kc == 0, kc == 7))
            k.mm(bank, steps, [t32, wr])
            k.op("act", lambda e, bank=bank: e.activation(out=sc[:, :], in_=bank[:, 0:128], func=AF.Sigmoid), [bank], [sc])
            k.op("dve", lambda e: e.tensor_tensor(out=sel[:, :], in0=sc[:, :], in1=rb[:, :], op=ALU.add), [sc, rb], [sel])
            k.op("dve", lambda e: e.tensor_reduce(out=m1[:, :], in_=v4(sel), axis=AX.X, op=ALU.max), [sel], [m1])
            for i4 in range(4):
                k.op("dve", lambda e, i4=i4: e.tensor_tensor(out=eq[:, :], in0=v4(sel)[:, :, i4], in1=m1[:, :], op=ALU.is_equal), [sel, m1], [eq])
                k.op("dve", lambda e, i4=i4: e.scalar_tensor_tensor(out=v4(sel2)[:, :, i4], in0=eq[:, :], scalar=-1.0e9, in1=v4(sel)[:, :, i4],
                                                                   op0=ALU.mult, op1=ALU.add), [eq, sel], [sel2])
            k.op("dve", lambda e: e.tensor_reduce(out=m2[:, :], in_=v4(sel2), axis=AX.X, op=ALU.max), [sel2], [m2])
            k.op("dve", lambda e: e.tensor_tensor(out=gsm[:, :], in0=m1[:, :], in1=m2[:, :], op=ALU.add), [m1, m2], [gsm])
            k.op("dve", lambda e: e.tensor_reduce(out=gmax[:, :], in_=v8(gsm), axis=AX.X, op=ALU.max), [gsm], [gmax])
            for g8 in range(8):
                k.op("dve", lambda e, g8=g8: e.tensor_tensor(out=v8(ghot)[:, :, g8], in0=v8(gsm)[:, :, g8], in1=gmax[:, :], op=ALU.is_equal), [gsm, gmax], [ghot])
            for i4 in range(4):
                k.op("dve", lambda e, i4=i4: e.tensor_tensor(out=eq[:, :], in0=v4(sel)[:, :, i4], in1=m2[:, :], op=ALU.is_ge), [sel, m2], [eq])
                k.op("dve", lambda e, i4=i4: e.tensor_tensor(out=v4(mask)[:, :, i4], in0=eq[:, :], in1=ghot[:, :], op=ALU.mult), [eq, ghot], [mask])
            k.op("dve", lambda e: e.tensor_tensor(out=mask[:, :], in0=mask[:, :], in1=sc[:, :], op=ALU.mult), [mask, sc], [mask])
            k.op("dve", lambda e: e.tensor_reduce(out=den[:, :], in_=v32(mask), axis=AX.X, op=ALU.add), [mask], [den])
            k.op("dve", lambda e: e.reciprocal(out=den[:, :], in_=den[:, :]), [den], [den])
            for j in range(4):
                k.op("dve", lambda e, j=j: e.tensor_scalar(out=comb[:, j * 32:(j + 1) * 32], in0=mask[:, j * 32:(j + 1) * 32], scalar1=den[:, j:j + 1],
                                                          scalar2=None, op0=ALU.mult), [mask, den], [comb])
            k.op("dve", lambda e, tt=tt: e.tensor_copy(out=H[:, tt * 4:(tt + 1) * 4, :], in_=v8(ghot)), [ghot], [H])
            for j in range(4):
                k.op("dve", lambda e, tt=tt, j=j: e.tensor_reduce(out=W4[:, tt * 4 + j, :], in_=comb[:, j * 32:(j + 1) * 32].rearrange("p (g i) -> p i g", i=4),
                                                                 axis=AX.X, op=ALU.add), [comb], [W4])
            tk = ttok[tt % 2]
            for j in range(4):
                for hf in range(2):
                    bank = k.bank()
                    k.tr(bank, [(bank[:, cc * 128:(cc + 1) * 128], t32[:, hf * 4 + cc, j * 128:(j + 1) * 128]) for cc in range(4)], [t32], self.ident)
                    k.cp("act" if hf else "dve", tk, tk[:, j, hf * 512:(hf + 1) * 512], bank, bank[:, :])
            k.store(tk, TTOK[tok, :].rearrange("(j p) d -> p j d", p=128), tk[:, :, :])
        Hf = H[:, :, :].rearrange("p b g -> p (b g)")
        bank = k.bank()
        k.mm(bank, [(bank[:, 0:256], mc[:, 0:128], Hf, True, True)], [mc, H])
        k.op("dve", lambda e, bank=bank: e.tensor_copy(out=RK[:, :, :].rearrange("p b g -> p (b g)"), in_=bank[:, 0:256]), [bank], [RK])
        bank = k.bank()
        k.mm(bank, [(bank[:, 0:256], self.ones[:, :], Hf, True, True)], [self.ones, H])
        k.op("dve", lambda e, bank=bank: e.tensor_copy(out=TOT[:, :, :].rearrange("p b g -> p (b g)"), in_=bank[:, 0:256]), [bank], [TOT])
        k.op("dve", lambda e: e.memset(PRE[:, 0, :], 0.0), [], [PRE])
        for b in range(1, 32):
            k.op("dve", lambda e, b=b: e.tensor_tensor(out=PRE[:, b, :], in0=PRE[:, b - 1, :], in1=TOT[:, b - 1, :], op=ALU.add), [PRE, TOT], [PRE])
        k.op("dve", lambda e: e.tensor_tensor(out=sm[:, 0:8], in0=PRE[:, 31, :], in1=TOT[:, 31, :], op=ALU.add), [PRE, TOT], [sm])
        k.op("dve", lambda e: e.memset(sm[:, 8:16], 0.0), [], [sm])
        for m in range(8):
            k.op("dve", lambda e, m=m: e.tensor_scalar(out=sm[:, 32:40], in0=sm[:, 0:8], scalar1=float(512 * m), scalar2=None, op0=ALU.is_gt), [sm], [sm])
            k.op("dve", lambda e: e.tensor_tensor(out=sm[:, 8:16], in0=sm[:, 8:16], in1=sm[:, 32:40], op=ALU.add), [sm], [sm])
        k.op("dve", lambda e: e.memset(sm[:, 16:17], 0.0), [], [sm])
        k.op("dve", lambda e: e.tensor_copy(out=sm[:, 24:25], in_=sm[:, 8:9]), [sm], [sm])
        for g8 in range(1, 8):
            k.op("dve", lambda e, g8=g8: e.scalar_tensor_tensor(out=sm[:, 16 + g8:17 + g8], in0=sm[:, 8 + g8 - 1:9 + g8 - 1], scalar=512.0, in1=sm[:, 16 + g8 - 1:17 + g8 - 1],
                                                               op0=ALU.mult, op1=ALU.add), [sm], [sm])
            k.op("dve", lambda e, g8=g8: e.tensor_tensor(out=sm[:, 24 + g8:25 + g8], in0=sm[:, 24 + g8 - 1:25 + g8 - 1], in1=sm[:, 8 + g8:9 + g8], op=ALU.add), [sm], [sm])
        k.op("dve", lambda e: e.tensor_tensor(out=RK[:, :, :], in0=RK[:, :, :], in1=PRE[:, :, :], op=ALU.add), [RK, PRE], [RK])
        for b in range(32):
            k.op("dve", lambda e, b=b: e.tensor_tensor(out=RK[:, b, :], in0=RK[:, b, :], in1=sm[:, 16:24], op=ALU.add), [RK, sm], [RK])
        k.op("dve", lambda e: e.tensor_tensor(out=RK[:, :, :], in0=RK[:, :, :], in1=H[:, :, :], op=ALU.mult), [RK, H], [RK])
        k.op("dve", lambda e: e.tensor_reduce(out=SLOTF[:, :], in_=RK[:, :, :], axis=AX.X, op=ALU.add), [RK], [SLOTF])
        k.op("dve", lambda e: e.tensor_copy(out=SLOTI[:, :], in_=SLOTF[:, :]), [SLOTF], [SLOTI])
        for i in range(16):
            k.op("dve", lambda e, i=i: e.tensor_scalar(out=sm[:, 32:40], in0=sm[:, 24:32], scalar1=float(i), scalar2=None, op0=ALU.is_le), [sm], [sm])
            k.op("dve", lambda e, i=i: e.tensor_reduce(out=GID[:, i:i + 1], in_=sm[:, 32:40], axis=AX.X, op=ALU.add), [sm], [GID])
        k.op("dve", lambda e: e.tensor_scalar(out=GID[:, :], in0=GID[:, :], scalar1=7.0, scalar2=None, op0=ALU.min), [GID], [GID])
        for j in range(4):
            k.op("dve", lambda e, j=j: e.tensor_scalar(out=IDXF[:, :, j], in0=GID[:, :], scalar1=512.0, scalar2=mc[:, 128:129], op0=ALU.mult, op1=ALU.add), [GID, mc], [IDXF])
            k.op("dve", lambda e, j=j: e.tensor_scalar(out=IDXF[:, :, j], in0=IDXF[:, :, j], scalar1=float((l * NEXP + j) * 128), scalar2=None, op0=ALU.add), [IDXF], [IDXF])
        k.op("dve", lambda e: e.tensor_copy(out=IDXI[:, :], in_=IDXF[:, :, :].rearrange("p i j -> p (i j)")), [IDXF], [IDXI])
        if "slots" in self.dbgset:
            k.store(SLOTF, self.S("slots", [128, 32]), SLOTF[:, :])
            k.store(GID, self.S("gid", [128, 16]), GID[:, :])
        k.barrier()
        tb = [k.sb("tb%d" % i, [128, D], F32) for i in range(3)]
        w4b = [k.sb("w4b%d" % i, [128, 128], F32) for i in range(2)]
        for b_ in w4b:
            k.op("pool", lambda e, b_=b_: e.memset(b_[:, :], 0.0), [], [b_])
        for b in range(32):
            t_ = tb[b % 3]
            k.load(t_, t_[:, :], TTOK[b * 128:(b + 1) * 128, :])
            k.idma(TS[:, :], t_[:, :], SLOTI, SLOTI[:, b:b + 1], t_, False, NS)
            w_ = w4b[b % 2]
            k.op("dve", lambda e, w_=w_, b=b: e.tensor_copy(out=w_[:, 0:4], in_=W4[:, b, :]), [W4], [w_])
            k.idma(W4S[:, :], w_[:, :], SLOTI, SLOTI[:, b:b + 1], w_, False, NS)
        k.end()
        k.begin()
        wg16 = [k.sb("wg%d" % i, [128, 8, FF], BF16) for i in range(2)]
        wu16 = [k.sb("wu%d" % i, [128, 8, FF], BF16) for i in range(2)]
        wd16 = [k.sb("wd%d" % i, [128, 4, D], BF16) for i in range(2)]
        self.wcast_finish()
        tsr = [k.sb("tsr%d" % i, [128, 4, D], F32) for i in range(2)]
        tsT = [k.sb("tsT%d" % i, [128, 8, T], BF16) for i in range(2)]
        w4s = [k.sb("w4s%d" % i, [128, 4, 128], F32) for i in range(2)]
        yac = [k.sb("yac%d" % i, [128, 4, D], F32) for i in range(2)]
        sgb = [k.sb("sg%d" % i, [128, T], F32) for i in range(2)]
        hid = [k.sb("hid%d" % i, [128, 4, T], BF16) for i in range(2)]
        nst = 0
        ncast = 0
        for i in range(15):
            tr_, tT_, w4_, ya = tsr[i % 2], tsT[i % 2], w4s[i % 2], yac[i % 2]
            k.load(tr_, tr_[:, :, :], TS[i * T:(i + 1) * T, :].rearrange("(j p) d -> p j d", p=128))
            k.load(w4_, w4_[:, :, :], W4S[i * T:(i + 1) * T, :].rearrange("(j p) d -> p j d", p=128))
            for c in range(8):
                bank = k.bank()
                k.tr(bank, [(bank[:, j * 128:(j + 1) * 128], tr_[:, j, c * 128:(c + 1) * 128]) for j in range(4)], [tr_], self.ident)
                k.cp("act" if c % 2 else "dve", tT_, tT_[:, c, :], bank, bank[:, :])
            for j in range(4):
                n = i * 4 + j
                wg, wu, wd = wg16[n % 2], wu16[n % 2], wd16[n % 2]
                for (dst, srcw) in ((wg, wgl), (wu, wul), (wd, wdl)):
                    k.idma(dst[:, :, :].rearrange("p a b -> p (a b)"), srcw[:, :], self.IDXI_p, self.IDXI_p[:, n:n + 1], dst, True, 2 * NEXP * 128)
                h_ = hid[n % 2]
                for fc in range(4):
                    m = n * 4 + fc
                    bg = k.bank()
                    k.mm(bg, [(bg[:, :], wg[:, kc, fc * 128:(fc + 1) * 128], tT_[:, kc, :], kc == 0, kc == 7) for kc in range(8)], [wg, tT_])
                    bu = k.bank()
                    k.mm(bu, [(bu[:, :], wu[:, kc, fc * 128:(fc + 1) * 128], tT_[:, kc, :], kc == 0, kc == 7) for kc in range(8)], [wu, tT_])
                    sg = sgb[m % 2]
                    k.op("act", lambda e, sg=sg, bg=bg: e.activation(out=sg[:, :], in_=bg[:, :], func=AF.Silu), [bg], [sg])
                    k.op("dve", lambda e, h_=h_, fc=fc, sg=sg, bu=bu: e.tensor_tensor(out=h_[:, fc, :], in0=bu[:, :], in1=sg[:, :], op=ALU.mult), [bu, sg], [h_])
                for blk in range(4):
                    for hf in range(2):
                        bd = k.bank()
                        k.mm(bd, [(bd[:, :], h_[:, fc, blk * 128:(blk + 1) * 128], wd[:, fc, hf * 512:(hf + 1) * 512], fc == 0, fc == 3) for fc in range(4)], [wd, h_])
                        if j == 0:
                            k.op("dve", lambda e, ya=ya, blk=blk, hf=hf, bd=bd, w4_=w4_: e.tensor_scalar(out=ya[:, blk, hf * 512:(hf + 1) * 512], in0=bd[:, :],
                                                                                                     scalar1=w4_[:, blk, 0:1], scalar2=None, op0=ALU.mult), [bd, w4_], [ya])
                        else:
                            k.op("dve", lambda e, ya=ya, blk=blk, hf=hf, bd=bd, w4_=w4_, j=j: e.scalar_tensor_tensor(
                                out=ya[:, blk, hf * 512:(hf + 1) * 512], in0=bd[:, :], scalar=w4_[:, blk, j:j + 1], in1=ya[:, blk, hf * 512:(hf + 1) * 512],
                                op0=ALU.mult, op1=ALU.add), [bd, w4_, ya], [ya])
            k.store(ya, YS[i * T:(i + 1) * T, :].rearrange("(j p) d -> p j d", p=128), ya[:, :, :])
        k.end()
        k.begin()
        yg = [k.sb("yg%d" % i, [128, D], F32) for i in range(4)]
        xr = [k.sb("xr%d" % i, [128, 8, T], F32) for i in range(2)]
        for tt in range(NT):
            tok = slice(tt * T, (tt + 1) * T)
            x_ = xr[tt % 2]
            k.load(x_, x_[:, :, :], XT[:, tok].rearrange("(c p) t -> p c t", p=128))
            for j in range(4):
                b = tt * 4 + j
                k.idma(yg[j][:, :], YS[:, :], self.SLOTI_p, self.SLOTI_p[:, b:b + 1], yg[j], True, NS)
            for c in range(8):
                bank = k.bank()
                k.tr(bank, [(bank[:, j * 128:(j + 1) * 128], yg[j][:, c * 128:(c + 1) * 128]) for j in range(4)], yg, self.ident)
                k.op("dve", lambda e, x_=x_, c=c, bank=bank: e.scalar_tensor_tensor(out=x_[:, c, :], in0=bank[:, :], scalar=cf[:, l, 5, c:c + 1], in1=x_[:, c, :],
                                                                                   op0=ALU.mult, op1=ALU.add), [bank, cf, x_], [x_])
            k.store(x_, XT[:, tok].rearrange("(c p) t -> p c t", p=128), x_[:, :, :])
        k.end()

    def stage_conf(self):
        k = self.k
        cf = self.coef
        g = self.gains
        XT = self.S("XT", [D, SEQ])
        GLU = self.S("GLU_T", [D, SEQ])
        k.begin()
        stg = [k.sb("stg%d" % i, [128, 8, 256], F32) for i in range(2)]
        w1 = k.sb("w1", [128, 8, 2 * D], BF16)
        self.load_w16(w1, self.I("conf_w1", [D, 2 * D]), 8, 2 * D, stg)
        xt = [k.sb("xt%d" % i, [128, 8, T], F32) for i in range(2)]
        tmp = k.sb("tmp", [128, 8, T], F32)
        rstd = k.sb("rstd", [128, T], F32)
        hT = k.sb("hT", [128, 8, T], BF16)
        sig = [k.sb("sig%d" % i, [128, T], F32) for i in range(2)]
        glu = [k.sb("glu%d" % i, [128, T], F32) for i in range(2)]
        for i in range(NT):
            x_ = xt[i % 2]
            k.load(x_, x_[:, :, :], XT[:, i * T:(i + 1) * T].rearrange("(c p) t -> p c t", p=128))
            self.rstd_fm(x_, 8, T, tmp, rstd, 1.0 / D)
            self.modulate(x_, rstd, cf[:, 1, 0, :], cf, cf[:, 1, 1, :], cf, tmp, hT, T)
            for oc in range(8):
                n = i * 8 + oc
                bgt = k.bank()
                k.mm(bgt, [(bgt[:, :], w1[:, kc, D + oc * 128:D + (oc + 1) * 128], hT[:, kc, :], kc == 0, kc == 7) for kc in range(8)], [w1, hT])
                bv = k.bank()
                k.mm(bv, [(bv[:, :], w1[:, kc, oc * 128:(oc + 1) * 128], hT[:, kc, :], kc == 0, kc == 7) for kc in range(8)], [w1, hT])
                s_, gl = sig[n % 2], glu[n % 2]
                k.op("act", lambda e, s_=s_, bgt=bgt, oc=oc: e.activation(out=s_[:, :], in_=bgt[:, :], func=AF.Sigmoid, bias=g[:, 10, oc:oc + 1], scale=1.0), [bgt, g], [s_])
                k.op("dve", lambda e, gl=gl, bv=bv, s_=s_, oc=oc: e.scalar_tensor_tensor(out=gl[:, :], in0=bv[:, :], scalar=g[:, 9, oc:oc + 1], in1=s_[:, :],
                                                                                        op0=ALU.add, op1=ALU.mult), [bv, g, s_], [gl])
                k.store(gl, GLU[oc * 128:(oc + 1) * 128, i * T:(i + 1) * T], gl[:, :], q=("sp" if n % 2 else "pool"))
        k.end()
        k.begin()
        stg = [k.sb("stg%d" % i, [128, 8, 256], F32) for i in range(2)]
        w2 = k.sb("w2", [128, 8, D], BF16)
        self.load_w16(w2, self.I("conf_w2", [D, D]), 8, D, stg)
        dww = k.sb("dww", [128, 8, 31], F32)
        k.load(dww, dww[:, :, :], self.I("conf_dw", [128, 8, 31]))
        dg = k.sb("dg", [128, 8, 31, 128], BF16)
        for c in range(8):
            for j in range(31):
                if (c * 31 + j) % 2:
                    k.op("dve", lambda e, c=c, j=j: e.tensor_scalar(out=dg[:, c, j, :], in0=self.ident[:, :], scalar1=dww[:, c, j:j + 1],
                                                                    scalar2=None, op0=ALU.mult), [self.ident, dww], [dg])
                else:
                    k.op("act", lambda e, c=c, j=j: e.activation(out=dg[:, c, j, :], in_=self.ident[:, :], func=AF.Copy, scale=dww[:, c, j:j + 1]),
                         [self.ident, dww], [dg])
        HL = T + 30
        gin = [k.sb("gin%d" % i, [128, HL], F32) for i in range(2)]
        g16 = [k.sb("g16%d" % i, [128, HL], BF16) for i in range(2)]
        cv = k.sb("cv", [128, 8, T], F32)
        sq = k.sb("sq", [128, 8, T], F32)
        mean = k.sb("mean", [128, T], F32)
        rstd = k.sb("rstd", [128, T], F32)
        uT = k.sb("uT", [128, 8, T], BF16)
        xt = [k.sb("xt%d" % i, [128, 8, T], F32) for i in range(2)]
        mt = [k.sb("mt%d" % i, [128, T], F32) for i in range(2)]
        nld = 0
        for i in range(NT):
            x_ = xt[i % 2]
            k.load(x_, x_[:, :, :], XT[:, i * T:(i + 1) * T].rearrange("(c p) t -> p c t", p=128), q="pool")
            lo = max(0, i * T - 15)
            hi = min(SEQ, (i + 1) * T + 15)
            off = lo - (i * T - 15)
            for c in range(8):
                gi_, gb_ = gin[nld % 2], g16[nld % 2]
                nld += 1
                if i == 0 or i == NT - 1:
                    k.op("dve", lambda e, gi_=gi_: e.memset(gi_[:, :], 0.0), [], [gi_])
                k.load(gi_, gi_[:, off:off + (hi - lo)], GLU[c * 128:(c + 1) * 128, lo:hi])
                k.cp("dve" if c % 2 else "act", gb_, gb_[:, :], gi_, gi_[:, :])
                bank = k.bank()
                k.mm(bank, [(bank[:, :], dg[:, c, j, :], gb_[:, j:j + T], j == 0, j == 30) for j in range(31)], [dg, gb_])
                k.op("act", lambda e, c=c, bank=bank: e.activation(out=cv[:, c, :], in_=bank[:, :], func=AF.Identity, bias=g[:, 5, c:c + 1], scale=1.0), [bank, g], [cv])
            k.op("act", lambda e: e.activation(out=sq[:, :, :], in_=cv[:, :, :], func=AF.Square), [cv], [sq])
            bm = k.bank()
            k.mm(bm, [(bm[:, :], self.ones[:, :], cv[:, c, :], c == 0, c == 7) for c in range(8)], [self.ones, cv])
            bq = k.bank()
            k.mm(bq, [(bq[:, :], self.ones[:, :], sq[:, c, :], c == 0, c == 7) for c in range(8)], [self.ones, sq])
            k.op("act", lambda e, bm=bm: e.activation(out=mean[:, :], in_=bm[:, :], func=AF.Copy, scale=1.0 / D), [bm], [mean])
            k.op("dve", lambda e: e.tensor_tensor(out=rstd[:, :], in0=mean[:, :], in1=mean[:, :], op=ALU.mult), [mean], [rstd])
            k.op("dve", lambda e, bq=bq: e.scalar_tensor_tensor(out=rstd[:, :], in0=bq[:, :], scalar=1.0 / D, in1=rstd[:, :], op0=ALU.mult, op1=ALU.subtract), [bq, rstd], [rstd])
            k.op("act", lambda e: e.activation(out=rstd[:, :], in_=rstd[:, :], func=AF.Sqrt, bias=self.eps_c[:, 0:1], scale=1.0), [rstd, self.eps_c], [rstd])
            k.op("dve", lambda e: e.reciprocal(out=rstd[:, :], in_=rstd[:, :]), [rstd], [rstd])
            for c in range(8):
                k.op("pool", lambda e, c=c: e.tensor_tensor(out=sq[:, c, :], in0=cv[:, c, :], in1=mean[:, :], op=ALU.subtract), [cv, mean], [sq])
                k.op("dve", lambda e, c=c: e.scalar_tensor_tensor(out=sq[:, c, :], in0=sq[:, c, :], scalar=g[:, 6, c:c + 1], in1=rstd[:, :], op0=ALU.mult, op1=ALU.mult), [sq, g, rstd], [sq])
                k.op("act", lambda e, c=c: e.activation(out=uT[:, c, :], in_=sq[:, c, :], func=AF.Silu, bias=g[:, 7, c:c + 1], scale=1.0), [sq, g], [uT])
            for oc in range(8):
                m_ = mt[oc % 2]
                bank = k.bank()
                k.mm(bank, [(bank[:, :], w2[:, kc, oc * 128:(oc + 1) * 128], uT[:, kc, :], kc == 0, kc == 7) for kc in range(8)], [w2, uT])
                k.op("act", lambda e, m_=m_, bank=bank, oc=oc: e.activation(out=m_[:, :], in_=bank[:, :], func=AF.Identity, bias=g[:, 8, oc:oc + 1], scale=1.0), [bank, g], [m_])
                k.op("dve", lambda e, m_=m_, x_=x_, oc=oc: e.scalar_tensor_tensor(out=x_[:, oc, :], in0=m_[:, :], scalar=cf[:, 1, 2, oc:oc + 1], in1=x_[:, oc, :],
                                                                                 op0=ALU.mult, op1=ALU.add), [m_, cf, x_], [x_])
            k.store(x_, XT[:, i * T:(i + 1) * T].rearrange("(c p) t -> p c t", p=128), x_[:, :, :])
        k.end()

    def stage_final(self):
        k = self.k
        k.begin()
        XT = self.S("XT", [D, SEQ])
        out = self.nc.dram_tensor("out", [SEQ, D], F32, kind="ExternalOutput").ap()
        g = self.gains
        xt = [k.sb("xt%d" % i, [128, 8, T], F32) for i in range(2)]
        sq = k.sb("sq", [128, 8, T], F32)
        rstd = k.sb("rstd", [128, T], F32)
        ot = [k.sb("ot%d" % i, [128, 4, D], F32) for i in range(2)]
        for i in range(NT):
            x_ = xt[i % 2]
            o_ = ot[i % 2]
            k.load(x_, x_[:, :, :], XT[:, i * T:(i + 1) * T].rearrange("(c p) t -> p c t", p=128))
            self.rstd_fm(x_, 8, T, sq, rstd, 1.0 / D)
            for c in range(8):
                k.op("dve", lambda e, c=c, x_=x_: e.scalar_tensor_tensor(out=sq[:, c, :], in0=x_[:, c, :], scalar=g[:, 4, c:c + 1], in1=rstd[:, :],
                                                                        op0=ALU.mult, op1=ALU.mult), [x_, g, rstd], [sq])
            for j in range(4):
                for hf in range(2):
                    bank = k.bank()
                    k.tr(bank, [(bank[:, cc * 128:(cc + 1) * 128], sq[:, hf * 4 + cc, j * 128:(j + 1) * 128]) for cc in range(4)], [sq], self.ident)
                    k.cp("act" if hf else "dve", o_, o_[:, j, hf * 512:(hf + 1) * 512], bank, bank[:, :])
            k.store(o_, out[i * T:(i + 1) * T, :].rearrange("(j p) d -> p j d", p=128), o_[:, :, :])
        k.end()

    def build(self):
        self.consts()
        st = self.stages
        if st is None or "moe0r" in st or "moe1r" in st:
            self.start_wcast()
        if st is None or "mods" in st:
            self.stage_mods()
        if st is None or "inproj" in st:
            self.stage_inproj()
        if st is None or "conv" in st:
            self.stage_conv()
        if st is None or "delta" in st:
            self.stage_delta()
        if st is None or "fnet" in st:
            self.stage_fnet()
        if st is None or "mix" in st:
            self.stage_mix()
        if st is not None and "moe0" in st:
            self.stage_moe(0)
        if st is None or "moe0r" in st:
            self.stage_moe_r(0)
        if st is None or "conf" in st:
            self.stage_conf()
        if st is not None and "moe1" in st:
            self.stage_moe(1)
        if st is None or "moe1r" in st:
            self.stage_moe_r(1)
        if st is None or "final" in st:
            self.stage_final()
        self.k.barrier()
        return self.nc


def _fm(v):
    return np.ascontiguousarray(np.asarray(v, np.float32).reshape(8, 128).T)


def _const_tables():
    t = {}
    quarter = D // 4
    omega = (1.0 / np.power(np.float32(10000.0), np.arange(quarter, dtype=np.float32) / np.float32(quarter))).astype(np.float32)

    def axis_emb(n):
        ang = (np.arange(n, dtype=np.float32)[:, None] * omega[None, :]).astype(np.float32)
        return np.concatenate([np.sin(ang), np.cos(ang)], axis=-1).astype(np.float32)

    rows, cols = SEQ // 64, 64
    er = np.broadcast_to(axis_emb(rows)[:, None, :], (rows, cols, D // 2))
    ec = np.broadcast_to(axis_emb(cols)[None, :, :], (rows, cols, D // 2))
    t["pos"] = np.ascontiguousarray(np.concatenate([er, ec], axis=-1).reshape(SEQ, D).astype(np.float32))
    n = np.arange(SEQ, dtype=np.int64)
    ang = 2.0 * np.pi * ((n[:, None] * n[None, :]) % SEQ).astype(np.float64) / SEQ
    t["dft_c"] = np.cos(ang).astype(ml_dtypes.bfloat16)
    t["dft_s"] = (-np.sin(ang)).astype(ml_dtypes.bfloat16)
    m = np.arange(128, dtype=np.int64)
    a2 = 2.0 * np.pi * ((m[:, None] * m[None, :]) % 128).astype(np.float64) / 128
    t["cs_ch"] = np.concatenate([np.cos(a2), np.sin(a2)], axis=1).astype(ml_dtypes.bfloat16)
    return t


_TABLES = None


def tables():
    global _TABLES
    if _TABLES is None:
        _TABLES = _const_tables()
    return _TABLES


def shared_inputs(inp):
    f = lambda a: np.ascontiguousarray(np.asarray(a, np.float32))
    s = dict(tables())
    s["ada_w"] = f(inp["ada_w"])
    s["ada_b"] = np.ascontiguousarray(f(inp["ada_b"]).reshape(2, 48, 128).transpose(2, 0, 1))
    vecs = [inp["norm1_g"][0], inp["norm1_g"][1], inp["norm2_g"][0], inp["norm2_g"][1], inp["final_g"],
            inp["conf_dw_b"][0], inp["conf_ln_g"][0], inp["conf_ln_b"][0], inp["conf_b2"][0],
            inp["conf_b1"][0][:D], inp["conf_b1"][0][D:]]
    s["gains"] = np.ascontiguousarray(np.stack([_fm(v) for v in vecs], axis=1))
    w_in = f(inp["hyb_w_in"][0])
    s["w_qkv"] = np.ascontiguousarray(w_in[:, 0:1536])
    s["w_ab"] = np.ascontiguousarray(w_in[:, 1536:1552])
    s["w_z"] = np.ascontiguousarray(w_in[:, 1552:2064])
    s["w_f"] = np.ascontiguousarray(w_in[:, 2064:2576])
    s["conv_w"] = np.ascontiguousarray(f(inp["dn_conv_w"][0]).T.reshape(12, 128, 5).transpose(1, 0, 2))
    s["adt"] = np.ascontiguousarray(np.stack([f(inp["dn_a_log"][0]).reshape(8), f(inp["dn_dt_bias"][0]).reshape(8)], axis=1))
    s["onorm_g"] = np.ascontiguousarray(np.tile(f(inp["dn_onorm_g"][0])[None, :], (128, 1)))
    s["w_out"] = f(inp["hyb_w_out"][0])
    s["router_w"] = f(inp["router_w"])
    s["rbias"] = np.ascontiguousarray(np.tile(f(inp["router_bias"])[None, :], (128, 4)))
    s["moe_wg"] = f(inp["moe_w_gate"]); s["moe_wu"] = f(inp["moe_w_up"]); s["moe_wd"] = f(inp["moe_w_down"])
    s["moe_wgl"] = np.ascontiguousarray(s["moe_wg"].reshape(2, NEXP, 8, 128, FF).transpose(0, 1, 3, 2, 4)).reshape(2 * NEXP * 128, 8 * FF)
    s["moe_wul"] = np.ascontiguousarray(s["moe_wu"].reshape(2, NEXP, 8, 128, FF).transpose(0, 1, 3, 2, 4)).reshape(2 * NEXP * 128, 8 * FF)
    s["moe_wdl"] = np.ascontiguousarray(s["moe_wd"].reshape(2, NEXP, 4, 128, D).transpose(0, 1, 3, 2, 4)).reshape(2 * NEXP * 128, 4 * D)
    tt_ = np.arange(128)
    s["mconst"] = np.ascontiguousarray(np.concatenate([(tt_[:, None] < tt_[None, :]).astype(np.float32), tt_[:, None].astype(np.float32)], axis=1))
    s["conf_w1"] = f(inp["conf_w1"][0]); s["conf_w2"] = f(inp["conf_w2"][0])
    s["conf_dw"] = np.ascontiguousarray(f(inp["conf_dw_w"][0]).T.reshape(8, 128, 31).transpose(1, 0, 2))
    j = np.arange(64)
    s["dmask"] = np.ascontiguousarray(np.stack([(j[:, None] <= j[None, :]), (j[:, None] < j[None, :]),
                                                (j[:, None] >= j[None, :]), (j[:, None] > j[None, :])], axis=1).astype(np.float32))
    return s


def core_inputs(inp, b):
    f = lambda a: np.ascontiguousarray(np.asarray(a, np.float32))
    c = {}
    c["x"] = f(inp["x"][b])
    c["ctx"] = f(inp["ctx"][b])
    cc = np.stack([f(inp["c"][b]), f(inp["c_ctx"])], axis=-1)
    c["cc"] = np.ascontiguousarray(cc.reshape(8, 128, 2).transpose(1, 0, 2))
    return c


_PROG = None


def kernel(**inputs):
    global _PROG
    if _PROG is None:
        import os
        st = os.environ.get("KSTAGES")
        P = Prog(stages=(st.split(",") if st else None))
        P.build()
        _PROG = P
    P = _PROG
    shared = shared_inputs(inputs)
    in_maps = []
    for b in range(8):
        allin = dict(shared)
        allin.update(core_inputs(inputs, b))
        in_maps.append({n: allin[n] for n in P.inp})
    res = run_bass_kernel_spmd(P.nc, in_maps, core_ids=list(range(8)))
    return np.stack([np.asarray(r["out"], np.float32) for r in res.results], axis=0)
```
